# Optimizing a Trainium2 kernel written in Bass

```python
import jax, jax.numpy as jnp
from jax import lax
import numpy as np

D_MODEL = 2048
BATCH = 8
SEQ = 4096
DEPTH = 4

NORM_EPS = 1e-6
D_FF = 5632
D_MIX = D_MODEL

LRU_WIDTH = D_MIX // 4
LRU_BLOCKS = 8
LRU_BLOCK_SIZE = LRU_WIDTH // LRU_BLOCKS
CONV_WIDTH = 4
LRU_C = 8.0

HEAD_DIM = 128
NSA_HEADS = (D_MIX // 2) // HEAD_DIM
NSA_KV_HEADS = 2
NSA_GROUP = NSA_HEADS // NSA_KV_HEADS
NSA_WIDTH = NSA_HEADS * HEAD_DIM
NSA_KV_WIDTH = NSA_KV_HEADS * HEAD_DIM
ROPE_DIM = HEAD_DIM // 4
ROPE_THETA = 500000.0
CMP_BLOCK = 32
CMP_STRIDE = 16
SEL_BLOCK = 64
N_SELECT = 16
WINDOW = 512
Q_BLOCK = 128
MASK_VALUE = 1e30

GLA_DV = 128
GLA_HEADS = (D_MIX // 4) // GLA_DV
GLA_DK = GLA_DV // 2
GLA_WIDTH = GLA_HEADS * GLA_DV
GLA_QK_WIDTH = GLA_HEADS * GLA_DK
GLA_RANK = 16
GLA_TAU = 16.0
GLA_CHUNK = 64

IN_SPLITS = (LRU_WIDTH, LRU_WIDTH, NSA_WIDTH) + (NSA_KV_WIDTH,) * 6 + (
    NSA_HEADS * 3, GLA_QK_WIDTH, GLA_QK_WIDTH, GLA_WIDTH, GLA_WIDTH, GLA_RANK)
D_IN = sum(IN_SPLITS)

kernel_name = 'hymba_style_rglru_nsa_gla_macaron'


def rms_norm(x, g):
    xf = x.astype(jnp.float32)
    y = xf * lax.rsqrt(jnp.mean(xf * xf, axis=-1, keepdims=True) + NORM_EPS)
    return (y * g.astype(jnp.float32)).astype(x.dtype)


def swiglu(h, w_gate, w_up, w_down):
    return (jax.nn.silu(h @ w_gate) * (h @ w_up)) @ w_down


def rope_tables(T):
    inv = 1.0 / (ROPE_THETA ** (jnp.arange(0, ROPE_DIM, 2, dtype=jnp.float32) / ROPE_DIM))
    ang = jnp.arange(T, dtype=jnp.float32)[:, None] * inv[None, :]
    return jnp.cos(ang), jnp.sin(ang)


def partial_rope(x, cos, sin):
    half = ROPE_DIM // 2
    x1, x2, xp = x[..., :half], x[..., half:ROPE_DIM], x[..., ROPE_DIM:]
    rot = jnp.concatenate([x1 * cos - x2 * sin, x2 * cos + x1 * sin], axis=-1)
    return jnp.concatenate([rot.astype(x.dtype), xp], axis=-1)


def split_cols(p):
    idx = np.cumsum(np.array(IN_SPLITS))[:-1].tolist()
    return jnp.split(p, idx, axis=-1)


def block_diag_linear(x, w, b):
    B, T, W = x.shape
    xb = x.reshape(B, T, LRU_BLOCKS, LRU_BLOCK_SIZE)
    return jnp.einsum('btni,nio->btno', xb, w).reshape(B, T, W) + b


def rg_lru(x, ga_w, ga_b, gx_w, gx_b, lam):
    f32 = jnp.float32
    r = jax.nn.sigmoid(block_diag_linear(x, ga_w, ga_b).astype(f32))
    i = jax.nn.sigmoid(block_diag_linear(x, gx_w, gx_b).astype(f32))
    log_a = -LRU_C * r * jax.nn.softplus(-lam.astype(f32))
    a = jnp.exp(log_a)
    mult = jnp.sqrt(-jnp.expm1(2.0 * log_a))
    bx = mult * i * x.astype(f32)

    def step(h, inp):
        a_t, b_t = inp
        h = a_t * h + b_t
        return h, h

    h0 = jnp.zeros((x.shape[0], x.shape[2]), f32)
    _, hs = lax.scan(step, h0, (a.swapaxes(0, 1), bx.swapaxes(0, 1)))
    return hs.swapaxes(0, 1).astype(x.dtype)


def recurrent_block(u, y_in, conv_w, conv_b, ga_w, ga_b, gx_w, gx_b, lam):
    xc = lax.conv_general_dilated(
        u, conv_w[:, None, :], window_strides=(1,), padding=[(CONV_WIDTH - 1, 0)],
        dimension_numbers=('NWC', 'WIO', 'NWC'), feature_group_count=u.shape[-1]) + conv_b
    h = rg_lru(xc, ga_w, ga_b, gx_w, gx_b, lam)
    return h * jax.nn.gelu(y_in)


def compress(k, pe, w1, w2):
    T = k.shape[2]
    nc = (T - CMP_BLOCK) // CMP_STRIDE + 1
    idx = np.arange(nc)[:, None] * CMP_STRIDE + np.arange(CMP_BLOCK)[None, :]
    blocks = k[:, :, idx] + pe
    flat = blocks.reshape(blocks.shape[0], blocks.shape[1], nc, CMP_BLOCK * HEAD_DIM)
    return jax.nn.gelu(flat @ w1) @ w2


def gather_blocks(blocks, idx):
    return jax.vmap(jax.vmap(lambda bl, ix: bl[ix]))(blocks, idx)


def masked_softmax(s, mask):
    return jax.nn.softmax(jnp.where(mask, s, -MASK_VALUE), axis=-1)


def nsa_mixer(q, k_cmp, v_cmp, k_sel, v_sel, k_win, v_win, gate_logits, gate_b,
              q_norm, kc_norm, ks_norm, kw_norm, pe_k, w1_k, w2_k, pe_v, w1_v, w2_v, cos, sin):
    B, T, _ = q.shape
    KV, G, HD = NSA_KV_HEADS, NSA_GROUP, HEAD_DIM
    f32 = jnp.float32
    scale = HEAD_DIM ** -0.5

    def to_heads(t, n):
        return t.reshape(B, T, n, HD).transpose(0, 2, 1, 3)

    qh = rms_norm(to_heads(q, NSA_HEADS), q_norm)
    q_c = qh.reshape(B, KV, G, T, HD)
    q_r = partial_rope(qh, cos, sin).reshape(B, KV, G, T, HD)

    kc = rms_norm(compress(to_heads(k_cmp, KV), pe_k, w1_k, w2_k), kc_norm)
    vc = compress(to_heads(v_cmp, KV), pe_v, w1_v, w2_v)
    nc = kc.shape[2]

    ns = T // SEL_BLOCK
    n_sel = min(N_SELECT, ns)
    ks = partial_rope(rms_norm(to_heads(k_sel, KV), ks_norm), cos, sin)
    ks_blocks = ks.reshape(B, KV, ns, SEL_BLOCK, HD)
    vs_blocks = to_heads(v_sel, KV).reshape(B, KV, ns, SEL_BLOCK, HD)

    pad = ((0, 0), (0, 0), (WINDOW, 0), (0, 0))
    kw_pad = jnp.pad(partial_rope(rms_norm(to_heads(k_win, KV), kw_norm), cos, sin), pad)
    vw_pad = jnp.pad(to_heads(v_win, KV), pad)

    gates = jax.nn.sigmoid((gate_logits + gate_b).astype(f32)).astype(q.dtype)
    gates = gates.reshape(B, T, KV, G, 3).transpose(0, 2, 3, 1, 4)

    cmp_start = np.arange(nc) * CMP_STRIDE
    sel_start = np.arange(ns) * SEL_BLOCK
    cmp_end = jnp.asarray(cmp_start + CMP_BLOCK - 1)
    overlap = jnp.asarray(((cmp_start[:, None] < sel_start[None, :] + SEL_BLOCK) &
                           (cmp_start[:, None] + CMP_BLOCK > sel_start[None, :])).astype(np.float32))
    sel_ids = jnp.arange(ns)

    def q_block(qb):
        s = qb * Q_BLOCK
        t = s + jnp.arange(Q_BLOCK)
        qc = lax.dynamic_slice_in_dim(q_c, s, Q_BLOCK, axis=3)
        qr = lax.dynamic_slice_in_dim(q_r, s, Q_BLOCK, axis=3)
        g = lax.dynamic_slice_in_dim(gates, s, Q_BLOCK, axis=3)

        sc = jnp.einsum('bkgqd,bknd->bkgqn', qc, kc).astype(f32) * scale
        cmask = cmp_end[None, :] <= t[:, None]
        pc = jnp.where(cmask, masked_softmax(sc, cmask), 0.0)
        o_cmp = jnp.einsum('bkgqn,bknd->bkgqd', pc.astype(vc.dtype), vc)

        imp = jnp.einsum('bkgqn,ns->bkqs', pc, overlap)
        cur = t // SEL_BLOCK
        forced = (sel_ids[None, :] == 0) | (sel_ids[None, :] == cur[:, None]) | (sel_ids[None, :] == cur[:, None] - 1)
        valid = sel_ids[None, :] * SEL_BLOCK <= t[:, None]
        imp = jnp.where(forced, MASK_VALUE, jnp.where(valid, imp, -MASK_VALUE))
        _, sel = lax.top_k(imp, n_sel)
        k_g = gather_blocks(ks_blocks, sel)
        v_g = gather_blocks(vs_blocks, sel)
        ss = jnp.einsum('bkgqd,bkqnjd->bkgqnj', qr, k_g).astype(f32) * scale
        kpos_sel = sel[..., None] * SEL_BLOCK + jnp.arange(SEL_BLOCK)
        smask = (kpos_sel <= t[:, None, None])[:, :, None]
        ss = jnp.where(smask, ss, -MASK_VALUE)
        ps = jax.nn.softmax(ss.reshape(ss.shape[:4] + (n_sel * SEL_BLOCK,)), axis=-1).reshape(ss.shape)
        o_sel = jnp.einsum('bkgqnj,bkqnjd->bkgqd', ps.astype(v_g.dtype), v_g)

        k_w = lax.dynamic_slice_in_dim(kw_pad, s, WINDOW + Q_BLOCK, axis=2)
        v_w = lax.dynamic_slice_in_dim(vw_pad, s, WINDOW + Q_BLOCK, axis=2)
        kpos = s - WINDOW + jnp.arange(WINDOW + Q_BLOCK)
        wmask = (kpos[None, :] <= t[:, None]) & (kpos[None, :] > t[:, None] - WINDOW) & (kpos[None, :] >= 0)
        sw = jnp.einsum('bkgqd,bkjd->bkgqj', qr, k_w).astype(f32) * scale
        pw = masked_softmax(sw, wmask)
        o_win = jnp.einsum('bkgqj,bkjd->bkgqd', pw.astype(v_w.dtype), v_w)

        return g[..., 0:1] * o_cmp + g[..., 1:2] * o_sel + g[..., 2:3] * o_win

    outs = lax.map(q_block, jnp.arange(T // Q_BLOCK))
    return outs.transpose(1, 0, 4, 2, 3, 5).reshape(B, T, NSA_WIDTH)


def gla_mixer(q, k, v, g, a_lr, a_w2, a_b, out_norm):
    B, T, _ = q.shape
    n = T // GLA_CHUNK
    f32 = jnp.float32
    log_alpha = jax.nn.log_sigmoid((a_lr @ a_w2 + a_b).astype(f32)) / GLA_TAU

    def chunks(t, d):
        return t.reshape(B, n, GLA_CHUNK, GLA_HEADS, d).transpose(1, 0, 3, 2, 4).astype(f32)

    qc = chunks(q, GLA_DK) * GLA_DK ** -0.5
    kc = chunks(k, GLA_DK)
    vc = chunks(v, GLA_DV)
    lc = chunks(log_alpha, GLA_DK)
    causal = jnp.tril(jnp.ones((GLA_CHUNK, GLA_CHUNK), dtype=bool))[:, :, None]

    def step(S, inp):
        q_, k_, v_, la = inp
        b = jnp.cumsum(la, axis=2)
        o_inter = jnp.einsum('bhid,bhde->bhie', q_ * jnp.exp(b), S)
        decay = jnp.exp(jnp.where(causal, b[:, :, :, None] - b[:, :, None], -jnp.inf))
        A = jnp.einsum('bhid,bhjd,bhijd->bhij', q_, k_, decay)
        o = o_inter + jnp.einsum('bhij,bhje->bhie', A, v_)
        b_last = b[:, :, -1]
        S = jnp.exp(b_last)[..., None] * S + jnp.einsum(
            'bhjd,bhje->bhde', k_ * jnp.exp(b_last[:, :, None] - b), v_)
        return S, o

    S0 = jnp.zeros((B, GLA_HEADS, GLA_DK, GLA_DV), f32)
    _, o = lax.scan(step, S0, (qc, kc, vc, lc))
    o = o.transpose(1, 0, 3, 2, 4).reshape(B, T, GLA_HEADS, GLA_DV)
    o = rms_norm(o, out_norm).reshape(B, T, GLA_WIDTH)
    return (o * jax.nn.silu(g.astype(f32))).astype(q.dtype)


def setup_inputs(seed: int = 0) -> dict:
    key = jax.random.key(seed)
    keys = iter(jax.random.split(key, 64))
    L = DEPTH

    def nrm(shape, scale):
        return scale * jax.random.normal(next(keys), shape, jnp.float32)

    def gain(shape):
        return 1.0 + 0.02 * jax.random.normal(next(keys), shape, jnp.float32)

    u = jax.random.uniform(next(keys), (L, LRU_WIDTH), jnp.float32, 0.9 ** 2, 0.999 ** 2)
    lru_lambda = -jnp.log(jnp.expm1(-0.5 * jnp.log(u)))
    return {
        'x': nrm((BATCH, SEQ, D_MODEL), 1.0),
        'ffn1_norm': gain((L, D_MODEL)),
        'ffn1_w_gate': nrm((L, D_MODEL, D_FF), D_MODEL ** -0.5),
        'ffn1_w_up': nrm((L, D_MODEL, D_FF), D_MODEL ** -0.5),
        'ffn1_w_down': nrm((L, D_FF, D_MODEL), D_FF ** -0.5),
        'mix_norm': gain((L, D_MODEL)),
        'w_in': nrm((L, D_MODEL, D_IN), D_MODEL ** -0.5),
        'lru_conv_w': nrm((L, CONV_WIDTH, LRU_WIDTH), CONV_WIDTH ** -0.5),
        'lru_conv_b': nrm((L, LRU_WIDTH), 0.02),
        'lru_gate_a_w': nrm((L, LRU_BLOCKS, LRU_BLOCK_SIZE, LRU_BLOCK_SIZE), LRU_BLOCK_SIZE ** -0.5),
        'lru_gate_a_b': nrm((L, LRU_WIDTH), 0.02),
        'lru_gate_x_w': nrm((L, LRU_BLOCKS, LRU_BLOCK_SIZE, LRU_BLOCK_SIZE), LRU_BLOCK_SIZE ** -0.5),
        'lru_gate_x_b': nrm((L, LRU_WIDTH), 0.02),
        'lru_lambda': lru_lambda,
        'lru_out_norm': gain((L, LRU_WIDTH)),
        'nsa_q_norm': gain((L, HEAD_DIM)),
        'nsa_k_cmp_norm': gain((L, HEAD_DIM)),
        'nsa_k_sel_norm': gain((L, HEAD_DIM)),
        'nsa_k_win_norm': gain((L, HEAD_DIM)),
        'nsa_cmp_pe_k': nrm((L, CMP_BLOCK, HEAD_DIM), 0.1),
        'nsa_cmp_w1_k': nrm((L, CMP_BLOCK * HEAD_DIM, HEAD_DIM), (CMP_BLOCK * HEAD_DIM) ** -0.5),
        'nsa_cmp_w2_k': nrm((L, HEAD_DIM, HEAD_DIM), HEAD_DIM ** -0.5),
        'nsa_cmp_pe_v': nrm((L, CMP_BLOCK, HEAD_DIM), 0.1),
        'nsa_cmp_w1_v': nrm((L, CMP_BLOCK * HEAD_DIM, HEAD_DIM), (CMP_BLOCK * HEAD_DIM) ** -0.5),
        'nsa_cmp_w2_v': nrm((L, HEAD_DIM, HEAD_DIM), HEAD_DIM ** -0.5),
        'nsa_gate_b': nrm((L, NSA_HEADS * 3), 0.02),
        'nsa_out_norm': gain((L, NSA_WIDTH)),
        'gla_a_w2': nrm((L, GLA_RANK, GLA_QK_WIDTH), GLA_RANK ** -0.5),
        'gla_a_b': nrm((L, GLA_QK_WIDTH), 0.02),
        'gla_out_norm': gain((L, GLA_DV)),
        'w_out': nrm((L, D_MIX, D_MODEL), D_MIX ** -0.5),
        'ffn2_norm': gain((L, D_MODEL)),
        'ffn2_w_gate': nrm((L, D_MODEL, D_FF), D_MODEL ** -0.5),
        'ffn2_w_up': nrm((L, D_MODEL, D_FF), D_MODEL ** -0.5),
        'ffn2_w_down': nrm((L, D_FF, D_MODEL), D_FF ** -0.5),
    }


def reference(x, ffn1_norm, ffn1_w_gate, ffn1_w_up, ffn1_w_down, mix_norm, w_in,
              lru_conv_w, lru_conv_b, lru_gate_a_w, lru_gate_a_b, lru_gate_x_w, lru_gate_x_b,
              lru_lambda, lru_out_norm, nsa_q_norm, nsa_k_cmp_norm, nsa_k_sel_norm, nsa_k_win_norm,
              nsa_cmp_pe_k, nsa_cmp_w1_k, nsa_cmp_w2_k, nsa_cmp_pe_v, nsa_cmp_w1_v, nsa_cmp_w2_v,
              nsa_gate_b, nsa_out_norm, gla_a_w2, gla_a_b, gla_out_norm, w_out,
              ffn2_norm, ffn2_w_gate, ffn2_w_up, ffn2_w_down):
    T = x.shape[1]
    cos, sin = rope_tables(T)
    for l in range(DEPTH):
        x = x + 0.5 * swiglu(rms_norm(x, ffn1_norm[l]), ffn1_w_gate[l], ffn1_w_up[l], ffn1_w_down[l])

        h = rms_norm(x, mix_norm[l])
        (lru_x, lru_y, nq, nkc, nvc, nks, nvs, nkw, nvw, ngate,
         gq, gk, gv, gg, ga) = split_cols(h @ w_in[l])
        y_a = recurrent_block(lru_x, lru_y, lru_conv_w[l], lru_conv_b[l], lru_gate_a_w[l],
                              lru_gate_a_b[l], lru_gate_x_w[l], lru_gate_x_b[l], lru_lambda[l])
        y_b = nsa_mixer(nq, nkc, nvc, nks, nvs, nkw, nvw, ngate, nsa_gate_b[l],
                        nsa_q_norm[l], nsa_k_cmp_norm[l], nsa_k_sel_norm[l], nsa_k_win_norm[l],
                        nsa_cmp_pe_k[l], nsa_cmp_w1_k[l], nsa_cmp_w2_k[l],
                        nsa_cmp_pe_v[l], nsa_cmp_w1_v[l], nsa_cmp_w2_v[l], cos, sin)
        y_c = gla_mixer(gq, gk, gv, gg, ga, gla_a_w2[l], gla_a_b[l], gla_out_norm[l])
        mix = jnp.concatenate([rms_norm(y_a, lru_out_norm[l]), rms_norm(y_b, nsa_out_norm[l]), y_c], axis=-1)
        x = x + mix @ w_out[l]

        x = x + 0.5 * swiglu(rms_norm(x, ffn2_norm[l]), ffn2_w_gate[l], ffn2_w_up[l], ffn2_w_down[l])
    return x
```

```python
from contextlib import ExitStack
import numpy as np
import concourse.bass as bass
import concourse.mybir as mybir
from concourse.bass_utils import run_bass_kernel_spmd

F32 = mybir.dt.float32
BF16 = mybir.dt.bfloat16
AF = mybir.ActivationFunctionType
ALU = mybir.AluOpType
AX = mybir.AxisListType

D = 2048
DFF = 5632
NFF = DFF // 128
KC = D // 128
EPS = 1e-6
NTOK = 512


class Res:
    __slots__ = ("w", "r")

    def __init__(self):
        self.w = None
        self.r = {}


class Sched:
    def __init__(self, nc, es, ndma=40):
        self.nc = nc
        self.eng = {"pe": nc.tensor, "act": nc.scalar, "dve": nc.vector, "pool": nc.gpsimd, "sp": nc.sync}
        self.sem = {}
        self.cnt = {}
        self.seen = {e: {} for e in self.eng}
        for e in ("pe", "act", "dve", "pool"):
            self.sem[("E", e)] = es.enter_context(nc.semaphore("sem_" + e))
            self.cnt[e] = 0
        self.ndma = {"sp": ndma, "pool": 8, "act": 8}
        for q, n in self.ndma.items():
            for i in range(n):
                self.sem[("D", q, i)] = es.enter_context(nc.semaphore("dsem_%s%d" % (q, i)))
        self.dma_i = {"sp": 0, "pool": 0, "act": 0}
        self.dlast = {}

    def _waits(self, eng, reads, writes):
        need = {}
        seen = self.seen[eng]
        own = ("E", eng)

        def add(k, v):
            if seen.get(k, 0) >= v:
                return
            if need.get(k, 0) < v:
                need[k] = v

        for r in reads:
            if r.w is not None:
                k, v = r.w
                if not (k == own and eng == "pe"):
                    add(k, v)
        pe_own = (eng == "pe")
        for w in writes:
            if w.w is not None and not (pe_own and w.w[0] == own):
                add(*w.w)
            for k, v in w.r.items():
                if not (pe_own and k == own):
                    add(k, v)
        return need

    def _emit_waits(self, eng, need):
        e = self.eng[eng]
        for k, v in need.items():
            e.wait_ge(self.sem[k], v)
            self.seen[eng][k] = v

    def op(self, eng, fn, reads=(), writes=()):
        need = self._waits(eng, reads, writes)
        self._emit_waits(eng, need)
        ins = fn(self.eng[eng])
        self.cnt[eng] += 1
        k = ("E", eng)
        v = self.cnt[eng]
        ins.then_inc(self.sem[k], 1)
        for r in reads:
            r.r[k] = v
        for w in writes:
            w.w = (k, v)
            w.r = {}

    def dma(self, out, in_, reads=(), writes=(), q="sp"):
        i = self.dma_i[q]
        self.dma_i[q] += 1
        slot = i % self.ndma[q]
        rnd = i // self.ndma[q]
        need = self._waits(q, reads, writes)
        k = ("D", q, slot)
        if rnd > 0 and self.seen[q].get(k, 0) < 16 * rnd:
            need[k] = max(need.get(k, 0), 16 * rnd)
        self._emit_waits(q, need)
        ins = self.eng[q].dma_start(out=out, in_=in_)
        v = 16 * (rnd + 1)
        ins.then_inc(self.sem[k], 16)
        self.dlast[k] = v
        for r in reads:
            r.r[k] = v
        for w in writes:
            w.w = (k, v)
            w.r = {}

    def barrier(self, include_pool=False):
        for eng in self.eng:
            need = {}
            for e2 in ("pe", "act", "dve", "pool"):
                k = ("E", e2)
                if e2 != eng and self.cnt[e2] > self.seen[eng].get(k, 0):
                    need[k] = self.cnt[e2]
            for k, v in self.dlast.items():
                if k[1] == "pool" and not include_pool:
                    continue
                if v > self.seen[eng].get(k, 0):
                    need[k] = v
            self._emit_waits(eng, need)


class Rot:
    def __init__(self, items):
        self.items = items
        self.i = 0

    def next(self):
        it = self.items[self.i % len(self.items)]
        self.i += 1
        return it


def mm_group(s, out, pairs, reads, writes, **kw):
    n = len(pairs)

    def fn(e):
        ins = None
        for i, (a, b) in enumerate(pairs):
            ins = e.matmul(out, a, b, start=(i == 0), stop=(i == n - 1), **kw)
        return ins

    s.op("pe", fn, reads=reads, writes=writes)


class Prog:
    def __init__(self, T, L, debug=()):
        self.T = T
        self.L = L
        self.debug = set(debug)
        self.nc = bass.Bass("TRN2", target_bir_lowering=False)
        self.dram = {}
        self.res = {}

    def din(self, name, shape, dt=F32):
        t = self.nc.dram_tensor(name, list(shape), dt, kind="ExternalInput")
        self.dram[name] = t
        return t

    def dout(self, name, shape, dt=F32):
        t = self.nc.dram_tensor(name, list(shape), dt, kind="ExternalOutput")
        self.dram[name] = t
        return t

    def dscr(self, name, shape, dt=F32):
        kind = "ExternalOutput" if name in self.debug else "Internal"
        t = self.nc.dram_tensor(name, list(shape), dt, kind=kind)
        self.dram[name] = t
        return t

    def R(self, *key):
        r = self.res.get(key)
        if r is None:
            r = Res()
            self.res[key] = r
        return r


WIN_CM = 17
WIN_TM = 7


def build(T, L, debug=(), stop_after=None, only_mix=None, skip_tt0=False, stage=99):
    P = Prog(T, L, debug)
    nc = P.nc
    NT = T // NTOK
    es = ExitStack()
    with es:
        s = Sched(nc, es)
        xT_in = P.din("xT", [D, T])
        outT = P.dout("outT", [D, T])
        gains = P.din("gains", [128, L * 3 * KC])
        w32 = {}
        w16 = {}
        wspec = {
            "wgu1": (NFF, 2 * KC * 128), "wd1": (KC, NFF * 128),
            "wgu2": (NFF, 2 * KC * 128), "wd2": (KC, NFF * 128),
            "wincm": (WIN_CM, KC * 128), "wintm": (WIN_TM, KC * 512), "wout": (KC, KC * 128),
            "cmpw1": (2, 4096), "cmpw2": (2, 128), "cmppe": (2, 32),
        }
        for nm, (npc, el) in wspec.items():
            w32[nm] = P.din(nm, [L, npc, 128, el])
            w16[nm] = P.dscr(nm + "_bf", [L, npc, 128, el], BF16)
        xT = P.dscr("xT_s", [D, T])
        mixT = P.dscr("mixT", [D, T], BF16)
        cm_rows = {"lru": 1024, "kvc": 512, "gqk": 512}
        lruT = P.dscr("lruT", [1024, T])
        kvcT = P.dscr("kvcT", [512, T], BF16)
        gqkT = P.dscr("gqkT", [512, T])
        gaT = P.dscr("gaT", [16, T], BF16)
        q_tm = P.dscr("q_tm", [T, 1024])
        ksw_tm = P.dscr("ksw_tm", [T, 1024])
        gkg_tm = P.dscr("gkg_tm", [T, 512])
        gv_tm = P.dscr("gv_tm", [T, 512], BF16)
        gg_tm = P.dscr("gg_tm", [T, 512])

        lruv = P.din("lruv", [L, 128, 4, 9])
        lrug = P.din("lrug", [L, 2, 4, 128, 128])
        glaw = P.din("glaw", [L, 17, 256])
        glan = P.din("glan", [L, 128])
        c128_d = P.din("c128", [128, 6, 128])
        NCBp = 256
        cmaskT = P.din("cmaskT", [256, T])
        Efull = P.din("Efull", [64, T])
        ropecs = P.din("ropecs", [T, 32])
        fmvm = P.din("fmvm", [T, 128])
        ovl = P.din("ovl", [256, 64])
        nsan = P.din("nsan", [L, 4, 128])
        nsaon = P.din("nsaon", [L, 1024])
        nsagb = P.din("nsagb", [L, 24])
        cast_order = ["wgu1", "wd1", "wincm", "wintm", "cmpw1", "cmpw2", "cmppe", "wout", "wgu2", "wd2"]

        def cast_jobs(l):
            jobs = []
            for nm in cast_order:
                npc, el = wspec[nm]
                grp = max(1, (1 << 19) // (128 * el))
                for p0 in range(0, npc, grp):
                    jobs.append((nm, l, p0, min(npc, p0 + grp)))
            return jobs

        def cast_job(job):
            nm, l, p0, p1 = job
            src = w32[nm].ap()[l, p0:p1].rearrange("c p e -> (c p) e")
            dst = w16[nm].ap()[l, p0:p1].rearrange("c p e -> (c p) e")
            s.dma(dst, src, writes=[P.R("w", nm, l, pc) for pc in range(p0, p1)], q="pool")

        def cast_layer(l):
            for job in cast_jobs(l):
                cast_job(job)

        def cast_layer_old(l):
            for nm, (npc, el) in wspec.items():
                grp = max(1, (1 << 20) // (128 * el))
                for p0 in range(0, npc, grp):
                    p1 = min(npc, p0 + grp)
                    src = w32[nm].ap()[l, p0:p1].rearrange("c p e -> (c p) e")
                    dst = w16[nm].ap()[l, p0:p1].rearrange("c p e -> (c p) e")
                    ws = [P.R("w", nm, l, pc) for pc in range(p0, p1)]
                    s.dma(dst, src, writes=ws, q="pool")

        cmask_bf = P.dscr("cmask_bf", [256, T], BF16)
        Efull_bf = P.dscr("Efull_bf", [64, T], BF16)
        ovl_bf = P.dscr("ovl_bf", [256, 64], BF16)
        s.dma(cmask_bf.ap(), cmaskT.ap(), writes=[P.R("cmask_bf")], q="pool")
        s.dma(Efull_bf.ap(), Efull.ap(), writes=[P.R("Efull_bf")], q="pool")
        s.dma(ovl_bf.ap(), ovl.ap(), writes=[P.R("ovl_bf")], q="pool")
        cast_layer(0)

        def sb(name, shape, dt):
            return es.enter_context(nc.sbuf_tensor(name, list(shape), dt))

        psall = es.enter_context(nc.psum_tensor("psall", [128, 4096], F32))
        psb = psall.bitcast(BF16)
        psum = [psall[:, i * 512:(i + 1) * 512] for i in range(8)]
        psR = [P.R("ps", i) for i in range(8)]
        ones32 = sb("ones32", [128, 128], F32)
        gains_sb = sb("gains_sb", [128, L * 3 * KC], F32)
        s.op("dve", lambda e: e.memset(ones32[:], 1.0), writes=[P.R("ones32")])
        s.dma(gains_sb[:], gains.ap(), writes=[P.R("gains")])
        cst = sb("cst", [128, 4], F32)
        s.op("dve", lambda e: e.memset(cst[:, 0:1], 1.0), writes=[P.R("cst")])
        s.op("dve", lambda e: e.memset(cst[:, 1:2], EPS), writes=[P.R("cst")])
        c128 = sb("c128_sb", [128, 6, 128], F32)
        s.dma(c128[:], c128_d.ap(), writes=[P.R("c128")])
        identb = sb("identb", [128, 128], BF16)
        s.op("dve", lambda e: e.tensor_copy(out=identb[:], in_=c128[:, 0, :]), reads=[P.R("c128")], writes=[P.R("identb")])

        g = dict(locals())
        orchestrate(P, s, g)
        s.barrier(include_pool=True)
    return nc


def orchestrate(P, s, g):
    T, L = P.T, P.L
    NT = T // NTOK
    stop_after = g.get("stop_after")

    def cast_slice(lc, t):
        if lc >= L:
            return
        jobs = g["cast_jobs"](lc)
        per = (len(jobs) + NT - 1) // NT
        for job in jobs[t * per:(t + 1) * per]:
            g["cast_job"](job)

    def body0(f):
        for t in range(NT):
            cast_slice(1, t)
            f["load_x"](g["xT_in"], t, "xT_in")
            f["ffn"](0, 1)
            f["proj"](0, t)
            f["store_x"](g["xT"] if stop_after != "tt0" else g["outT"], t, "xT")
    if not g.get("skip_tt0"):
        tt_phase(P, s, g, body0, "a")
        s.barrier()
    if stop_after == "tt0":
        return
    for l in range(L):
        mix_phase(P, s, g, l)
        s.barrier()
        if stop_after == "mix%d" % l:
            return

        def body(f, l=l):
            for t in range(NT):
                cast_slice(l + 2, t)
                f["load_x"](g["xT"], t, "xT")
                f["wout"](l, t)
                f["ffn"](l, 2)
                if l + 1 < L:
                    f["ffn"](l + 1, 1)
                    f["proj"](l + 1, t)
                    f["store_x"](g["xT"], t, "xT")
                else:
                    f["store_x"](g["outT"], t, "outT")
        tt_phase(P, s, g, body, "b%d" % l)
        s.barrier()


def mix_phase(P, s, g, l):
    only = g.get("only_mix")
    if only is None or "lru" in only:
        lru_mix(P, s, g, l)
        s.barrier()
    if only is None or "gla" in only:
        gla_mix(P, s, g, l)
        s.barrier()
    if only is None or "nsa" in only:
        nsa_mix(P, s, g, l)
        s.barrier()


def gelu2(s, dst, src, tmp, rsrc, rtmp, rdst):
    s.op("dve", lambda e: e.tensor_tensor(out=tmp, in0=src, in1=src, op=ALU.mult), reads=[rsrc], writes=[rtmp])
    s.op("dve", lambda e: e.tensor_scalar(out=tmp, in0=tmp, scalar1=0.044715, scalar2=1.0, op0=ALU.mult, op1=ALU.add),
         reads=[rtmp], writes=[rtmp])
    s.op("dve", lambda e: e.tensor_tensor(out=tmp, in0=tmp, in1=src, op=ALU.mult), reads=[rtmp, rsrc], writes=[rtmp])
    s.op("act", lambda e: e.activation(out=tmp, in_=tmp, func=AF.Tanh, scale=0.7978845608028654), reads=[rtmp], writes=[rtmp])
    s.op("dve", lambda e: e.scalar_tensor_tensor(out=dst, in0=tmp, scalar=1.0, in1=src, op0=ALU.add, op1=ALU.mult),
         reads=[rtmp, rsrc], writes=[rdst])


def lru_mix(P, s, g, l):
    nc = P.nc
    T = P.T
    NTT = T // 512
    psum, ones32, cst = g["psum"], g["ones32"], g["cst"]
    lruT, mixT = g["lruT"], g["mixT"]
    R = P.R
    with ExitStack() as ms:
        def sb(name, shape, dt):
            return ms.enter_context(nc.sbuf_tensor("lru_%s_%d" % (name, l), list(shape), dt))
        lv = sb("lv", [128, 4, 9], F32)
        gw = sb("gw", [128, 2, 4, 128], F32)
        c12 = sb("c12", [128, 2, 4], F32)
        u_sb = sb("u", [128, 4, 515], F32)
        y_sb = sb("y", [128, 4, 512], F32)
        xc = sb("xc", [128, 4, 512], F32)
        r_sb = sb("r", [128, 4, 512], F32)
        i_sb = sb("i", [128, 4, 512], F32)
        a_sb = sb("a", [128, 4, 512], F32)
        m_sb = sb("m", [128, 4, 512], F32)
        h_sb = sb("h", [128, 4, 512], F32)
        gl = sb("gl", [128, 4, 512], F32)
        tmp = sb("tmp", [128, 4, 512], F32)
        ya = sb("ya", [128, 4, 512], F32)
        sq = sb("sq", [128, 2, 512], F32)
        rstd = sb("rstd", [128, 512], F32)
        outb = sb("outb", [128, 4, 512], BF16)
        hprev = sb("hprev", [128, 4], F32)
        s.dma(lv[:], g["lruv"].ap()[l], writes=[R("lv")])
        s.dma(gw[:], g["lrug"].ap()[l].rearrange("a c p o -> p a c o"), writes=[R("gw")])
        s.op("act", lambda e: e.activation(out=c12[:, 0, :], in_=lv[:, :, 7], func=AF.Exp, scale=-1.0), reads=[R("lv")], writes=[R("c12")])
        s.op("act", lambda e: e.activation(out=c12[:, 0, :], in_=c12[:, 0, :], func=AF.Ln, bias=cst[:, 0:1]), reads=[R("c12"), R("cst")], writes=[R("c12")])
        s.op("dve", lambda e: e.tensor_scalar(out=c12[:, 1, :], in0=c12[:, 0, :], scalar1=-16.0, scalar2=None, op0=ALU.mult), reads=[R("c12")], writes=[R("c12")])
        s.op("dve", lambda e: e.tensor_scalar(out=c12[:, 0, :], in0=c12[:, 0, :], scalar1=-8.0, scalar2=None, op0=ALU.mult), reads=[R("c12")], writes=[R("c12")])
        sqrot = Rot([(0, R("lsq", 0)), (1, R("lsq", 1))])
        C4 = range(4)
        for tt in range(NTT):
            t0 = tt * 512
            for c in C4:
                rows = slice(c * 128, (c + 1) * 128)
                uR, yR = R("lu", c), R("ly", c)
                if tt == 0:
                    s.op("dve", lambda e: e.memset(u_sb[:, c, 0:3], 0.0), writes=[uR])
                    s.dma(u_sb[:, c, 3:515], lruT.ap()[rows, 0:512], reads=[R("lruT", 0)], writes=[uR])
                else:
                    s.dma(u_sb[:, c, :], lruT.ap()[rows, t0 - 3:t0 + 512], reads=[R("lruT", tt - 1), R("lruT", tt)], writes=[uR])
                s.dma(y_sb[:, c, :], lruT.ap()[512 + c * 128:512 + (c + 1) * 128, t0:t0 + 512], reads=[R("lruT", tt)], writes=[yR])
            for c in C4:
                s.op("dve", lambda e: e.tensor_scalar(out=xc[:, c, :], in0=u_sb[:, c, 3:515], scalar1=lv[:, c, 3:4], scalar2=lv[:, c, 4:5],
                                                      op0=ALU.mult, op1=ALU.add), reads=[R("lu", c), R("lv")], writes=[R("lxc", c)])
            for k in (2, 1, 0):
                for c in C4:
                    s.op("dve", lambda e: e.scalar_tensor_tensor(out=xc[:, c, :], in0=u_sb[:, c, k:k + 512], scalar=lv[:, c, k:k + 1],
                                                                  in1=xc[:, c, :], op0=ALU.mult, op1=ALU.add), reads=[R("lu", c), R("lxc", c)], writes=[R("lxc", c)])
            for c in C4:
                for a, (dst, nm, bcol) in enumerate(((r_sb, "lr", 5), (i_sb, "li", 6))):
                    bk = a + 2 * (c % 2)
                    pr = g["psR"][bk]
                    s.op("pe", lambda e: e.matmul(psum[bk][:], gw[:, a, c, :], xc[:, c, :], start=True, stop=True), reads=[R("gw"), R("lxc", c)], writes=[pr])
                    s.op("act", lambda e: e.activation(out=dst[:, c, :], in_=psum[bk][:], func=AF.Sigmoid, bias=lv[:, c, bcol:bcol + 1]),
                         reads=[pr, R("lv")], writes=[R(nm, c)])
            for c in C4:
                s.op("act", lambda e: e.activation(out=a_sb[:, c, :], in_=r_sb[:, c, :], func=AF.Exp, scale=c12[:, 0, c:c + 1]), reads=[R("lr", c), R("c12")], writes=[R("la", c)])
                s.op("act", lambda e: e.activation(out=m_sb[:, c, :], in_=r_sb[:, c, :], func=AF.Exp, scale=c12[:, 1, c:c + 1]), reads=[R("lr", c), R("c12")], writes=[R("lm", c)])
            for c in C4:
                s.op("act", lambda e: e.activation(out=m_sb[:, c, :], in_=m_sb[:, c, :], func=AF.Sqrt, scale=-1.0, bias=cst[:, 0:1]), reads=[R("lm", c), R("cst")], writes=[R("lm", c)])
            for c in C4:
                s.op("dve", lambda e: e.tensor_tensor(out=m_sb[:, c, :], in0=m_sb[:, c, :], in1=i_sb[:, c, :], op=ALU.mult), reads=[R("lm", c), R("li", c)], writes=[R("lm", c)])
            for c in C4:
                s.op("dve", lambda e: e.tensor_tensor(out=m_sb[:, c, :], in0=m_sb[:, c, :], in1=xc[:, c, :], op=ALU.mult), reads=[R("lm", c), R("lxc", c)], writes=[R("lm", c)])
            for c in C4:
                init = 0.0 if tt == 0 else hprev[:, c:c + 1]
                s.op("dve", lambda e: e.tensor_tensor_scan(out=h_sb[:, c, :], data0=a_sb[:, c, :], data1=m_sb[:, c, :], initial=init,
                                                           op0=ALU.mult, op1=ALU.add), reads=[R("la", c), R("lm", c), R("lhp", c)], writes=[R("lh", c)])
            for c in C4:
                s.op("dve", lambda e: e.tensor_copy(out=hprev[:, c:c + 1], in_=h_sb[:, c, 511:512]), reads=[R("lh", c)], writes=[R("lhp", c)])
            for c in C4:
                s.op("dve", lambda e: e.tensor_tensor(out=tmp[:, c, :], in0=y_sb[:, c, :], in1=y_sb[:, c, :], op=ALU.mult), reads=[R("ly", c)], writes=[R("ltmp", c)])
            for c in C4:
                s.op("dve", lambda e: e.tensor_scalar(out=tmp[:, c, :], in0=tmp[:, c, :], scalar1=0.044715, scalar2=1.0, op0=ALU.mult, op1=ALU.add),
                     reads=[R("ltmp", c)], writes=[R("ltmp", c)])
            for c in C4:
                s.op("dve", lambda e: e.tensor_tensor(out=tmp[:, c, :], in0=tmp[:, c, :], in1=y_sb[:, c, :], op=ALU.mult), reads=[R("ltmp", c), R("ly", c)], writes=[R("ltmp", c)])
            for c in C4:
                s.op("act", lambda e: e.activation(out=tmp[:, c, :], in_=tmp[:, c, :], func=AF.Tanh, scale=0.7978845608028654), reads=[R("ltmp", c)], writes=[R("ltmp", c)])
            for c in C4:
                s.op("dve", lambda e: e.scalar_tensor_tensor(out=gl[:, c, :], in0=tmp[:, c, :], scalar=1.0, in1=y_sb[:, c, :], op0=ALU.add, op1=ALU.mult),
                     reads=[R("ltmp", c), R("ly", c)], writes=[R("lgl", c)])
            for c in C4:
                s.op("dve", lambda e: e.scalar_tensor_tensor(out=ya[:, c, :], in0=gl[:, c, :], scalar=0.5, in1=h_sb[:, c, :], op0=ALU.mult, op1=ALU.mult),
                     reads=[R("lgl", c), R("lh", c)], writes=[R("lya", c)])
            for c in C4:
                qi, qR = sqrot.next()
                s.op("act", lambda e: e.activation(out=sq[:, qi, :], in_=ya[:, c, :], func=AF.Square), reads=[R("lya", c)], writes=[qR])
                s.op("pe", lambda e: e.matmul(psum[6][:], ones32[:], sq[:, qi, :], start=(c == 0), stop=(c == 3)), reads=[qR, R("ones32")], writes=[g["psR"][6]])
            s.op("act", lambda e: e.activation(out=rstd[:], in_=psum[6][:], func=AF.Sqrt, scale=1.0 / 512, bias=cst[:, 1:2]),
                 reads=[g["psR"][6], R("cst")], writes=[R("lrstd")])
            s.op("dve", lambda e: e.reciprocal(out=rstd[:], in_=rstd[:]), reads=[R("lrstd")], writes=[R("lrstd")])
            for c in C4:
                s.op("dve", lambda e: e.scalar_tensor_tensor(out=outb[:, c, :], in0=ya[:, c, :], scalar=lv[:, c, 8:9], in1=rstd[:], op0=ALU.mult, op1=ALU.mult),
                     reads=[R("lya", c), R("lrstd"), R("lv")], writes=[R("loutb", c)])
                s.dma(mixT.ap()[c * 128:(c + 1) * 128, t0:t0 + 512], outb[:, c, :], reads=[R("loutb", c)], writes=[R("mixT", tt)], q="act")


def gla_mix(P, s, g, l):
    nc = P.nc
    T = P.T
    NG = T // 512
    psum, psb, psR, ones32, cst, c128 = g["psum"], g["psb"], g["psR"], g["ones32"], g["cst"], g["c128"]
    identb = g["identb"]
    R = P.R
    with ExitStack() as ms:
        def sb(name, shape, dt):
            return ms.enter_context(nc.sbuf_tensor("gla_%s_%d" % (name, l), list(shape), dt))
        aw32 = sb("aw32", [17, 256], F32)
        aw = sb("aw", [17, 256], BF16)
        gout = sb("gout", [128, 128], F32)
        qT = sb("qT", [64, 4, 512], F32)
        kT = sb("kT", [64, 4, 512], F32)
        gaT = sb("gaT", [17, 512], BF16)
        ktm = sb("ktm", [128, 4, 256], F32)
        vtm = sb("vtm", [128, 4, 512], BF16)
        ggt = sb("ggt", [128, 4, 512], F32)
        lsp = sb("lsp", [128, 256], F32)
        ekb = sb("ekb", [128, 256], F32)
        kp = sb("kp", [128, 2, 256], BF16)
        eb = sb("eb", [64, 2, 4, 128], F32)
        enb = sb("enb", [64, 4, 128], F32)
        qt_b = sb("qtb", [64, 2, 4, 128], BF16)
        kt_b = sb("ktb", [64, 2, 4, 128], BF16)
        atm = sb("atm", [128, 2, 4, 128], BF16)
        S32 = sb("S32", [64, 4, 128], F32)
        Sb = sb("Sb", [64, 4, 128], BF16)
        osq = sb("osq", [128, 4, 128], F32)
        on = sb("on", [128, 4, 128], F32)
        ssum = sb("ssum", [128, 4], F32)
        sgg = sb("sgg", [128, 512], F32)
        yc = sb("yc", [128, 512], BF16)
        stg = sb("stg", [128, 4, 512], BF16)
        s.dma(aw32[:], g["glaw"].ap()[l], writes=[R("aw32")])
        s.op("dve", lambda e: e.tensor_copy(out=aw[:], in_=aw32[:]), reads=[R("aw32")], writes=[R("aw")])
        s.dma(gout[:], g["glan"].ap()[l:l + 1, :].to_broadcast([128, 128]), writes=[R("gout")])
        s.op("dve", lambda e: e.memset(gaT[:], 1.0), writes=[R("gaT")])
        s.op("dve", lambda e: e.memset(S32[:], 0.0), writes=[R("S32")])
        s.op("dve", lambda e: e.memset(Sb[:], 0.0), writes=[R("Sb")])
        for gi in range(NG):
            tok = slice(gi * 512, (gi + 1) * 512)
            s.dma(qT[:], g["gqkT"].ap()[0:256, tok].rearrange("(a p) n -> p a n", p=64), reads=[R("gqkT", gi)], writes=[R("gqT")])
            s.dma(kT[:], g["gqkT"].ap()[256:512, tok].rearrange("(a p) n -> p a n", p=64), reads=[R("gqkT", gi)], writes=[R("gkT")])
            s.dma(gaT[0:16, :], g["gaT"].ap()[:, tok], reads=[R("gaT", gi)], writes=[R("gaT")])
            s.dma(ktm[:], g["gkg_tm"].ap()[tok, 0:256].rearrange("(c p) n -> p c n", p=128), reads=[R("gkg_tm", gi)], writes=[R("gktm")])
            s.dma(vtm[:], g["gv_tm"].ap()[tok, :].rearrange("(c p) n -> p c n", p=128), reads=[R("gv_tm", gi)], writes=[R("gvtm")])
            s.dma(ggt[:], g["gg_tm"].ap()[tok, :].rearrange("(c p) n -> p c n", p=128), reads=[R("gg_tm", gi)], writes=[R("gggt")])
            def s1(cc):
                pb = cc % 2
                ct = slice(cc * 128, (cc + 1) * 128)
                s.op("pe", lambda e: e.matmul(psum[0][:, 0:256], gaT[:, ct], aw[:], start=True, stop=True), reads=[R("gaT"), R("aw")], writes=[psR[0]])
                s.op("act", lambda e: e.activation(out=lsp[:], in_=psum[0][:, 0:256], func=AF.Exp, scale=-1.0), reads=[psR[0]], writes=[R("lsp")])
                s.op("act", lambda e: e.activation(out=lsp[:], in_=lsp[:], func=AF.Ln, bias=cst[:, 0:1]), reads=[R("lsp"), R("cst")], writes=[R("lsp")])
                s.op("pe", lambda e: e.matmul(psum[1][:, 0:256], c128[:, 2, :], lsp[:], start=True, stop=True), reads=[R("lsp"), R("c128")], writes=[psR[1]])
                for a in range(4):
                    s.op("pe", lambda e: e.matmul(psum[2][0:64, a * 128:(a + 1) * 128], lsp[:, a * 64:(a + 1) * 64], c128[:, 1, :], start=True, stop=True),
                         reads=[R("lsp"), R("c128")], writes=[psR[2]])
                s.op("act", lambda e: e.activation(out=ekb[:], in_=psum[1][:, 0:256], func=AF.Exp), reads=[psR[1]], writes=[R("ekb")])
                s.op("dve", lambda e: e.tensor_tensor(out=kp[:, pb, :], in0=ktm[:, cc, :], in1=ekb[:], op=ALU.mult), reads=[R("gktm"), R("ekb")], writes=[R("kp", pb)])
                s.op("act", lambda e: e.activation(out=eb[:, pb].rearrange("p a n -> p (a n)"), in_=psum[2][0:64, :], func=AF.Exp), reads=[psR[2]], writes=[R("eb", pb)])
                s.op("act", lambda e: e.activation(out=enb[:].rearrange("p a n -> p (a n)"), in_=psum[2][0:64, :], func=AF.Exp, scale=-1.0), reads=[psR[2]], writes=[R("enb")])
                s.op("dve", lambda e: e.scalar_tensor_tensor(out=qt_b[:, pb], in0=qT[:, :, ct], scalar=0.125, in1=eb[:, pb], op0=ALU.mult, op1=ALU.mult),
                     reads=[R("gqT"), R("eb", pb)], writes=[R("qtb", pb)])
                s.op("dve", lambda e: e.tensor_tensor(out=kt_b[:, pb], in0=kT[:, :, ct], in1=enb[:], op=ALU.mult), reads=[R("gkT"), R("enb")], writes=[R("ktb", pb)])
                def at_fn(e):
                    ins = None
                    for h in range(4):
                        ins = e.matmul(psum[3][:, h * 128:(h + 1) * 128], kt_b[:, pb, h, :], qt_b[:, pb, h, :], start=True, stop=True)
                    return ins
                s.op("pe", at_fn, reads=[R("ktb", pb), R("qtb", pb)], writes=[psR[3]])
                s.op("dve", lambda e: e.tensor_tensor(out=atm[:, pb], in0=psum[3][:].rearrange("p (h n) -> p h n", h=4),
                                                      in1=c128[:, 3, :].unsqueeze(1).to_broadcast([128, 4, 128]), op=ALU.mult),
                     reads=[psR[3], R("c128")], writes=[R("atm", pb)])
            def s2(cc):
                pb = cc % 2
                ct = slice(cc * 128, (cc + 1) * 128)
                def o_fn(e):
                    ins = None
                    for h in range(4):
                        e.matmul(psum[4][:, h * 128:(h + 1) * 128], qt_b[:, pb, h, :], Sb[:, h, :], start=True, stop=False)
                        ins = e.matmul(psum[4][:, h * 128:(h + 1) * 128], atm[:, pb, h, :], vtm[:, cc, h * 128:(h + 1) * 128], start=False, stop=True)
                    return ins
                s.op("pe", o_fn, reads=[R("qtb", pb), R("Sb"), R("atm", pb), R("gvtm")], writes=[psR[4]])
                def kv_fn(e):
                    ins = None
                    for h in range(4):
                        ins = e.matmul(psum[5][0:64, h * 128:(h + 1) * 128], kp[:, pb, h * 64:(h + 1) * 64], vtm[:, cc, h * 128:(h + 1) * 128], start=True, stop=True)
                    return ins
                s.op("pe", kv_fn, reads=[R("kp", pb), R("gvtm")], writes=[psR[5]])
                s.op("dve", lambda e: e.tensor_tensor(out=S32[:], in0=S32[:], in1=eb[:, pb, :, 127:128].to_broadcast([64, 4, 128]), op=ALU.mult),
                     reads=[R("S32"), R("eb", pb)], writes=[R("S32")])
                s.op("dve", lambda e: e.tensor_tensor(out=S32[:], in0=S32[:], in1=psum[5][0:64, :].rearrange("p (h n) -> p h n", h=4), op=ALU.add),
                     reads=[R("S32"), psR[5]], writes=[R("S32")])
                s.op("act", lambda e: e.activation(out=Sb[:], in_=S32[:], func=AF.Copy), reads=[R("S32")], writes=[R("Sb")])
                o3 = psum[4][:].rearrange("p (h n) -> p h n", h=4)
                s.op("act", lambda e: e.activation(out=osq[:], in_=o3, func=AF.Square), reads=[psR[4]], writes=[R("osq")])
                s.op("dve", lambda e: e.tensor_reduce(out=ssum[:], in_=osq[:], axis=AX.X, op=ALU.add), reads=[R("osq")], writes=[R("ssum")])
                s.op("act", lambda e: e.activation(out=ssum[:], in_=ssum[:], func=AF.Sqrt, scale=1.0 / 128, bias=cst[:, 1:2]), reads=[R("ssum"), R("cst")], writes=[R("ssum")])
                s.op("dve", lambda e: e.reciprocal(out=ssum[:], in_=ssum[:]), reads=[R("ssum")], writes=[R("ssum")])
                s.op("dve", lambda e: e.tensor_tensor(out=on[:], in0=o3, in1=ssum[:].unsqueeze(2).to_broadcast([128, 4, 128]), op=ALU.mult),
                     reads=[psR[4], R("ssum")], writes=[R("on")])
                s.op("dve", lambda e: e.tensor_tensor(out=on[:], in0=on[:], in1=gout[:].unsqueeze(1).to_broadcast([128, 4, 128]), op=ALU.mult),
                     reads=[R("on"), R("gout")], writes=[R("on")])
                s.op("act", lambda e: e.activation(out=sgg[:], in_=ggt[:, cc, :], func=AF.Silu), reads=[R("gggt")], writes=[R("sgg")])
                s.op("dve", lambda e: e.tensor_tensor(out=yc[:], in0=on[:].rearrange("p h n -> p (h n)"), in1=sgg[:], op=ALU.mult),
                     reads=[R("on"), R("sgg")], writes=[R("yc")])
                def tr_fn(e):
                    ins = None
                    for h in range(4):
                        ins = e.transpose(psb[:, 6 * 1024 + h * 128:6 * 1024 + (h + 1) * 128], yc[:, h * 128:(h + 1) * 128], identb[:])
                    return ins
                s.op("pe", tr_fn, reads=[R("yc"), R("identb")], writes=[psR[6]])
                s.op("act", lambda e: e.activation(out=stg[:, :, ct], in_=psb[:, 6 * 1024:6 * 1024 + 512].rearrange("p (h n) -> p h n", h=4), func=AF.Copy),
                     reads=[psR[6]], writes=[R("gstg")])
            s1(0)
            for cc in range(4):
                if cc + 1 < 4:
                    s1(cc + 1)
                s2(cc)
            s.dma(g["mixT"].ap()[1536:2048, tok].rearrange("(h p) n -> p h n", p=128), stg[:], reads=[R("gstg")], writes=[R("mixT", gi)], q="act")


def nsa_mix(P, s, g, l):
    nc = P.nc
    T = P.T
    NQ = T // 128
    NCB = (T - 32) // 16 + 1
    SCALE = 128 ** -0.5
    psum, psb, psall, psR, ones32, cst, c128, identb = (g["psum"], g["psb"], g["psall"], g["psR"], g["ones32"], g["cst"],
                                                        g["c128"], g["identb"])
    w16 = g["w16"]
    R = P.R
    with ExitStack() as ms:
        def sbuf(name, shape, dt, st=ms):
            return st.enter_context(nc.sbuf_tensor("nsa_%s_%d" % (name, l), list(shape), dt))
        ksT = sbuf("ksT", [128, 2, T], BF16)
        kwT = sbuf("kwT", [128, 2, T], BF16)
        vsa = sbuf("vsa", [128, NQ, 2, 129], BF16)
        vwa = sbuf("vwa", [128, NQ, 2, 129], BF16)
        cmask = sbuf("cmask", [128, 2, T], BF16)
        Efull = sbuf("Efull", [64, T], BF16)
        cs_sb = sbuf("cs", [128, NQ, 32], F32)
        fmvm = sbuf("fmvm", [128, NQ, 128], F32)
        gates = sbuf("gates", [128, NQ, 24], F32)
        outn = sbuf("outn", [128, 1024], F32)
        nrm_bc = sbuf("nrm_bc", [128, 4, 128], F32)
        kcg = sbuf("kcg", [128, 1], F32)
        gb_bc = sbuf("gb_bc", [128, 24], F32)
        kcn = sbuf("kcn", [128, 2, 256], BF16)
        vca = sbuf("vca", [128, 2, 2, 193], BF16)
        negc4 = sbuf("negc4", [128, 4, 128], BF16)
        negu4 = sbuf("negu4", [128, 4, 128], BF16)
        w2 = sbuf("w2", [128, 2, 128], BF16)
        pe_bf = sbuf("pe_bf", [128, 2, 32], BF16)

        s.dma(cmask[:], g["cmask_bf"].ap().rearrange("(c p) t -> p c t", p=128), reads=[R("cmask_bf")], writes=[R("cmask")])
        s.dma(Efull[:], g["Efull_bf"].ap(), reads=[R("Efull_bf")], writes=[R("Efull")])
        s.dma(cs_sb[:], g["ropecs"].ap().rearrange("(i p) c -> p i c", p=128), writes=[R("cs")])
        s.dma(fmvm[:], g["fmvm"].ap().rearrange("(i p) c -> p i c", p=128), writes=[R("fmvm")])
        s.dma(outn[:], g["nsaon"].ap()[l:l + 1, :].to_broadcast([128, 1024]), writes=[R("outn")])
        for k in range(4):
            s.dma(nrm_bc[:, k, :], g["nsan"].ap()[l, k:k + 1, :].to_broadcast([128, 128]), writes=[R("nrm_bc")])
        s.dma(kcg[:], g["nsan"].ap()[l, 1, :].rearrange("(p o) -> p o", o=1), writes=[R("kcg")])
        s.dma(gb_bc[:], g["nsagb"].ap()[l:l + 1, :].to_broadcast([128, 24]), writes=[R("gb_bc")])
        s.op("dve", lambda e: e.memset(kcn[:], 0.0), writes=[R("kcn")])
        s.op("dve", lambda e: e.memset(vca[:], 0.0), writes=[R("vca")])
        s.op("dve", lambda e: e.memset(vca[:, :, :, 128:129], 1.0), writes=[R("vca")])
        for kv in range(2):
            s.dma(vca[:, :, kv, 129:193], g["ovl_bf"].ap().rearrange("(c p) s -> p c s", p=128), reads=[R("vca"), R("ovl_bf")], writes=[R("vca")])
        s.op("dve", lambda e: e.memset(vsa[:, :, :, 128:129], 1.0), writes=[R("vsa1")])
        s.op("dve", lambda e: e.memset(vwa[:, :, :, 128:129], 1.0), writes=[R("vwa1")])
        s.op("dve", lambda e: e.tensor_copy(out=negc4[:], in_=c128[:, 4, :].unsqueeze(1).to_broadcast([128, 4, 128])), reads=[R("c128")], writes=[R("negc4")])
        s.op("dve", lambda e: e.tensor_copy(out=negu4[:], in_=c128[:, 5, :].unsqueeze(1).to_broadcast([128, 4, 128])), reads=[R("c128")], writes=[R("negu4")])
        s.dma(w2[:], w16["cmpw2"].ap()[l].rearrange("k p o -> p k o"), reads=[R("w", "cmpw2", l, 0), R("w", "cmpw2", l, 1)], writes=[R("w2")])
        s.dma(pe_bf[:], w16["cmppe"].ap()[l].rearrange("k p o -> p k o"), reads=[R("w", "cmppe", l, 0), R("w", "cmppe", l, 1)], writes=[R("pe_bf")])

        with ExitStack() as cs_:
            xT_sb = sbuf("xT", [128, 4, T], BF16, cs_)
            w1 = sbuf("w1", [128, 2, 4096], BF16, cs_)
            hid = sbuf("hid", [128, 4, 256], BF16, cs_)
            hx = sbuf("hx", [128, 256], F32, cs_)
            htmp = sbuf("htmp", [128, 256], F32, cs_)
            hg = sbuf("hg", [128, 256], F32, cs_)
            cvec = sbuf("cvec", [128, 2], F32, cs_)
            ksq = sbuf("ksq", [128, 256], F32, cs_)
            krs = sbuf("krs", [128, 256], F32, cs_)
            s.dma(xT_sb[:], g["kvcT"].ap().rearrange("(a p) t -> p a t", p=128), reads=[R("kvcT", t) for t in range(T // NTOK)], writes=[R("nxT")])
            s.dma(w1[:], w16["cmpw1"].ap()[l].rearrange("k p o -> p k o"), reads=[R("w", "cmpw1", l, 0), R("w", "cmpw1", l, 1)], writes=[R("nw1")])
            s.op("dve", lambda e: e.memset(hid[:], 0.0), writes=[R("nhid")])
            for kvi in range(2):
                mm_group(s, psum[7][:, 0:1], [(w1[:, kvi, ll * 128:(ll + 1) * 128], pe_bf[:, kvi, ll:ll + 1]) for ll in range(32)],
                         reads=[R("nw1"), R("pe_bf")], writes=[psR[7]])
                s.op("dve", lambda e: e.tensor_copy(out=cvec[:, kvi:kvi + 1], in_=psum[7][:, 0:1]), reads=[psR[7]], writes=[R("ncvec")])
                for hd in range(2):
                    a = kvi * 2 + hd
                    mm_group(s, psum[0][:, 0:NCB], [(w1[:, kvi, ll * 128:(ll + 1) * 128], xT_sb[:, a, ll:ll + 16 * (NCB - 1) + 1:16]) for ll in range(32)],
                             reads=[R("nw1"), R("nxT")], writes=[psR[0]])
                    s.op("dve", lambda e: e.tensor_scalar(out=hx[:, 0:NCB], in0=psum[0][:, 0:NCB], scalar1=cvec[:, kvi:kvi + 1], scalar2=None, op0=ALU.add),
                         reads=[psR[0], R("ncvec")], writes=[R("nhx")])
                    gelu2(s, hg[:, 0:NCB], hx[:, 0:NCB], htmp[:, 0:NCB], R("nhx"), R("nhtmp"), R("nhg"))
                    s.op("act", lambda e: e.activation(out=hid[:, a, 0:NCB], in_=hg[:, 0:NCB], func=AF.Copy, scale=0.5), reads=[R("nhg")], writes=[R("nhid")])
                    if kvi == 0:
                        s.op("pe", lambda e: e.matmul(psum[1][:, 0:NCB], w2[:, 0, :], hid[:, a, 0:NCB], start=True, stop=True), reads=[R("w2"), R("nhid")], writes=[psR[1]])
                        s.op("act", lambda e: e.activation(out=ksq[:, 0:NCB], in_=psum[1][:, 0:NCB], func=AF.Square), reads=[psR[1]], writes=[R("nksq")])
                        s.op("pe", lambda e: e.matmul(psum[2][:, 0:NCB], ones32[:], ksq[:, 0:NCB], start=True, stop=True), reads=[R("nksq"), R("ones32")], writes=[psR[2]])
                        s.op("act", lambda e: e.activation(out=krs[:, 0:NCB], in_=psum[2][:, 0:NCB], func=AF.Sqrt, scale=1.0 / 128, bias=cst[:, 1:2]),
                             reads=[psR[2], R("cst")], writes=[R("nkrs")])
                        s.op("dve", lambda e: e.reciprocal(out=krs[:, 0:NCB], in_=krs[:, 0:NCB]), reads=[R("nkrs")], writes=[R("nkrs")])
                        s.op("dve", lambda e: e.scalar_tensor_tensor(out=kcn[:, hd, 0:NCB], in0=psum[1][:, 0:NCB], scalar=kcg[:, 0:1], in1=krs[:, 0:NCB],
                                                                      op0=ALU.mult, op1=ALU.mult), reads=[psR[1], R("kcg"), R("nkrs")], writes=[R("kcn")])
                    else:
                        for c in range((NCB + 127) // 128):
                            s.op("pe", lambda e: e.matmul(psum[3][:, 0:128], hid[:, a, c * 128:(c + 1) * 128], w2[:, 1, :], start=True, stop=True),
                                 reads=[R("w2"), R("nhid")], writes=[psR[3]])
                            s.op("act", lambda e: e.activation(out=vca[:, c, hd, 0:128], in_=psum[3][:, 0:128], func=AF.Copy), reads=[psR[3]], writes=[R("vca")])
            s.barrier()

        q_in = sbuf("q_in", [128, 8, 128], F32)
        ksw_in = sbuf("ksw_in", [128, 8, 128], F32)
        gl_in = sbuf("gl_in", [128, 24], F32)
        sqb = sbuf("sqb", [128, 8, 128], F32)
        xn = sbuf("xn", [128, 8, 128], F32)
        ss = sbuf("ss", [128, 8], F32)
        rt = sbuf("rt", [128, 4, 8, 16], F32)
        qc_bf = sbuf("qc_bf", [128, 8, 128], BF16)
        qr_bf = sbuf("qr_bf", [128, 8, 128], BF16)
        kr_bf = sbuf("kr_bf", [128, 4, 128], BF16)
        qcT = sbuf("qcT", [128, 8, 128], BF16)
        qrT = sbuf("qrT", [128, 8, 128], BF16)
        pT = sbuf("pT", [128, 3, 512], BF16)
        yb = sbuf("yb", [128, 8, 128], F32)
        tmpo = sbuf("tmpo", [128, 4, 128], F32)
        tmpu = sbuf("tmpu", [128, 4, 64], F32)
        imp = sbuf("imp", [128, 64], F32)
        imp2 = sbuf("imp2", [128, 64], F32)
        m8 = sbuf("m8", [128, 2, 8], F32)
        negsel = sbuf("negsel", [128, 2, 64], F32)
        sqy = sbuf("sqy", [128, 8, 128], F32)
        nsT4 = sbuf("nsT4", [64, 2, 4, 128], BF16)
        rden = sbuf("rden", [128, 3, 4], F32)
        coef = sbuf("coef", [128, 3, 4], F32)
        ss1 = sbuf("ss1", [128, 2], F32)
        yn = sbuf("yn", [128, 8, 128], BF16)
        stg = sbuf("stg", [128, 8, 512], BF16)
        pTrot = Rot([(k, R("npT", k)) for k in range(3)])
        SC = Rot([(0, psR[0]), (1, psR[1])])
        OA = Rot([(2, [psR[2], psR[3]]), (4, [psR[4], psR[5]])])

        def oview(b):
            return psall[:, b * 512:(b + 2) * 512].rearrange("p (g c) -> p g c", g=4)

        def norm_rope(i, src, H, gain, out_c, out_r, rsrc, rout):
            s.op("dve", lambda e: e.tensor_tensor(out=sqb[:, 0:H, :], in0=src, in1=src, op=ALU.mult), reads=[rsrc], writes=[R("nsqb")])
            s.op("dve", lambda e: e.tensor_reduce(out=ss[:, 0:H], in_=sqb[:, 0:H, :], axis=AX.X, op=ALU.add), reads=[R("nsqb")], writes=[R("nss")])
            s.op("act", lambda e: e.activation(out=ss[:, 0:H], in_=ss[:, 0:H], func=AF.Sqrt, scale=1.0 / 128, bias=cst[:, 1:2]), reads=[R("nss"), R("cst")], writes=[R("nss")])
            s.op("dve", lambda e: e.reciprocal(out=ss[:, 0:H], in_=ss[:, 0:H]), reads=[R("nss")], writes=[R("nss")])
            s.op("dve", lambda e: e.tensor_tensor(out=xn[:, 0:H, :], in0=src, in1=ss[:, 0:H].unsqueeze(2).to_broadcast([128, H, 128]), op=ALU.mult),
                 reads=[rsrc, R("nss")], writes=[R("nxn")])
            s.op("dve", lambda e: e.tensor_tensor(out=xn[:, 0:H, :], in0=xn[:, 0:H, :], in1=gain.unsqueeze(1).to_broadcast([128, H, 128]), op=ALU.mult),
                 reads=[R("nxn"), R("nrm_bc")], writes=[R("nxn")])
            if out_c is not None:
                s.op("act", lambda e: e.activation(out=out_c, in_=xn[:, 0:H, :], func=AF.Copy), reads=[R("nxn")], writes=[rout])
            cosb = cs_sb[:, i, 0:16].unsqueeze(1).to_broadcast([128, H, 16])
            sinb = cs_sb[:, i, 16:32].unsqueeze(1).to_broadcast([128, H, 16])
            x1 = xn[:, 0:H, 0:16]
            x2 = xn[:, 0:H, 16:32]
            for k, (a, b) in enumerate(((x1, cosb), (x2, sinb), (x2, cosb), (x1, sinb))):
                s.op("dve", lambda e: e.tensor_tensor(out=rt[:, k, 0:H, :], in0=a, in1=b, op=ALU.mult), reads=[R("nxn"), R("cs")], writes=[R("nrt")])
            s.op("dve", lambda e: e.tensor_tensor(out=out_r[:, :, 0:16], in0=rt[:, 0, 0:H, :], in1=rt[:, 1, 0:H, :], op=ALU.subtract), reads=[R("nrt")], writes=[rout])
            s.op("dve", lambda e: e.tensor_tensor(out=out_r[:, :, 16:32], in0=rt[:, 2, 0:H, :], in1=rt[:, 3, 0:H, :], op=ALU.add), reads=[R("nrt")], writes=[rout])
            s.op("act", lambda e: e.activation(out=out_r[:, :, 32:128], in_=xn[:, 0:H, 32:128], func=AF.Copy), reads=[R("nxn")], writes=[rout])

        def transposes(bank, srcs, rsrc):
            def fn(e):
                ins = None
                for k, a in enumerate(srcs):
                    ins = e.transpose(psb[:, bank * 1024 + k * 128:bank * 1024 + (k + 1) * 128], a, identb[:])
                return ins
            s.op("pe", fn, reads=rsrc + [R("identb")], writes=[psR[bank]])

        def branch(i, kv, kind):
            if kind == "sel":
                js = list(range(0, i + 1))
                kT_, va, bi = ksT, vsa, 1
            else:
                js = list(range(max(0, i - 4), i + 1))
                kT_, va, bi = kwT, vwa, 2
            ob, oR = OA.next()
            ov = oview(ob)
            q4 = qrT[:, kv * 4:(kv + 1) * 4, :].rearrange("p g n -> p (g n)")
            def score(j):
                si, sR = SC.next()
                pairs = [(kT_[:, kv, j * 128:(j + 1) * 128], q4)]
                rd = [R("nkT", kind, j), R("nqrT")]
                if kind == "sel":
                    pairs.append((Efull[:, j * 128:(j + 1) * 128], nsT4[:, kv, :, :].rearrange("p g n -> p (g n)")))
                    rd += [R("Efull"), R("nnsT4", kv)]
                if j == i:
                    pairs.append((identb[:], negc4[:].rearrange("p g n -> p (g n)")))
                    rd += [R("identb"), R("negc4")]
                if kind == "win" and j == i - 4:
                    pairs.append((identb[:], negu4[:].rearrange("p g n -> p (g n)")))
                    rd += [R("identb"), R("negu4")]
                mm_group(s, psum[si][:], pairs, reads=rd, writes=[sR])
                return si, sR

            def finish(j, si, sR):
                pi, pR = pTrot.next()
                s.op("act", lambda e: e.activation(out=pT[:, pi, :], in_=psum[si][:], func=AF.Exp, scale=SCALE), reads=[sR], writes=[pR])

                def pv(e):
                    ins = None
                    for gq in range(4):
                        ins = e.matmul(ov[:, gq, 0:129], pT[:, pi, gq * 128:(gq + 1) * 128], va[:, j, kv, :], start=(j == js[0] and gq % 2 == 0), stop=(j == js[-1] and gq % 2 == 1))
                    return ins
                s.op("pe", pv, reads=[pR, R("nva", kind, j), R("vsa1" if kind == "sel" else "vwa1")], writes=oR)

            nxt = score(js[0])
            for idx, j in enumerate(js):
                cur = nxt
                if idx + 1 < len(js):
                    nxt = score(js[idx + 1])
                finish(j, *cur)
            s.op("dve", lambda e: e.reciprocal(out=rden[:, bi, :], in_=ov[:, :, 128]), reads=oR, writes=[R("nrden", bi)])
            s.op("dve", lambda e: e.tensor_tensor(out=coef[:, bi, :], in0=gates[:, i, kv * 12 + bi:kv * 12 + 12:3], in1=rden[:, bi, :], op=ALU.mult),
                 reads=[R("ngates", i), R("nrden", bi)], writes=[R("ncoef", bi)])
            s.op("dve", lambda e: e.tensor_tensor(out=tmpo[:], in0=ov[:, :, 0:128], in1=coef[:, bi, :].unsqueeze(2).to_broadcast([128, 4, 128]), op=ALU.mult),
                 reads=oR + [R("ncoef", bi)], writes=[R("ntmpo")])
            s.op("dve", lambda e: e.tensor_tensor(out=yb[:, kv * 4:(kv + 1) * 4, :], in0=yb[:, kv * 4:(kv + 1) * 4, :], in1=tmpo[:], op=ALU.add),
                 reads=[R("ntmpo"), R("nyb")], writes=[R("nyb")])

        def prep_dve(i):
            rows = slice(i * 128, (i + 1) * 128)
            tt_i = i // 4
            s.dma(q_in[:].rearrange("p h d -> p (h d)"), g["q_tm"].ap()[rows, :], reads=[R("q_tm", tt_i)], writes=[R("nq_in")])
            s.dma(ksw_in[:].rearrange("p h d -> p (h d)"), g["ksw_tm"].ap()[rows, :], reads=[R("ksw_tm", tt_i)], writes=[R("nksw_in")])
            s.dma(gl_in[:], g["gkg_tm"].ap()[rows, 256:280], reads=[R("gkg_tm", tt_i)], writes=[R("ngl_in")])
            s.op("dve", lambda e: e.tensor_tensor(out=gl_in[:], in0=gl_in[:], in1=gb_bc[:], op=ALU.add), reads=[R("ngl_in"), R("gb_bc")], writes=[R("ngl_in")])
            s.op("act", lambda e: e.activation(out=gates[:, i, :], in_=gl_in[:], func=AF.Sigmoid), reads=[R("ngl_in")], writes=[R("ngates", i)])
            norm_rope(i, q_in[:], 8, nrm_bc[:, 0, :], qc_bf[:], qr_bf[:], R("nq_in"), R("nqbf"))
            norm_rope(i, ksw_in[:, 0:2, :], 2, nrm_bc[:, 2, :], None, kr_bf[:, 0:2, :], R("nksw_in"), R("nkrbf"))
            norm_rope(i, ksw_in[:, 4:6, :], 2, nrm_bc[:, 3, :], None, kr_bf[:, 2:4, :], R("nksw_in"), R("nkrbf"))
            s.op("dve", lambda e: e.tensor_copy(out=vsa[:, i, :, 0:128], in_=ksw_in[:, 2:4, :]), reads=[R("nksw_in")], writes=[R("nva", "sel", i)])
            s.op("dve", lambda e: e.tensor_copy(out=vwa[:, i, :, 0:128], in_=ksw_in[:, 6:8, :]), reads=[R("nksw_in")], writes=[R("nva", "win", i)])

        def prep_pe(i):
            rows = slice(i * 128, (i + 1) * 128)
            transposes(6, [qc_bf[:, h, :] for h in range(8)], [R("nqbf")])
            s.op("act", lambda e: e.activation(out=qcT[:].rearrange("p h n -> p (h n)"), in_=psb[:, 6 * 1024:7 * 1024], func=AF.Copy), reads=[psR[6]], writes=[R("nqcT")])
            transposes(7, [qr_bf[:, h, :] for h in range(8)], [R("nqbf")])
            s.op("dve", lambda e: e.tensor_copy(out=qrT[:].rearrange("p h n -> p (h n)"), in_=psb[:, 7 * 1024:8 * 1024]), reads=[psR[7]], writes=[R("nqrT")])
            transposes(6, [kr_bf[:, h, :] for h in range(4)], [R("nkrbf")])
            s.op("act", lambda e: e.activation(out=ksT[:, :, rows], in_=psb[:, 6 * 1024:6 * 1024 + 256].rearrange("p (h n) -> p h n", h=2), func=AF.Copy),
                 reads=[psR[6]], writes=[R("nkT", "sel", i)])
            s.op("act", lambda e: e.activation(out=kwT[:, :, rows], in_=psb[:, 6 * 1024 + 256:6 * 1024 + 512].rearrange("p (h n) -> p h n", h=2), func=AF.Copy),
                 reads=[psR[6]], writes=[R("nkT", "win", i)])

        def cmp_topk(i, kv):
            rows = slice(i * 128, (i + 1) * 128)
            n_max = min(NCB - 1, 8 * i + 6)
            ncc = n_max // 128 + 1
            ob, oR = OA.next()
            ov = oview(ob)
            q4c = qcT[:, kv * 4:(kv + 1) * 4, :].rearrange("p g n -> p (g n)")
            for c in range(ncc):
                si, sR = SC.next()
                s.op("pe", lambda e: e.matmul(psum[si][:], kcn[:, kv, c * 128:(c + 1) * 128], q4c, start=True, stop=True), reads=[R("kcn"), R("nqcT")], writes=[sR])
                pi, pR = pTrot.next()
                s.op("act", lambda e: e.activation(out=pT[:, pi, :], in_=psum[si][:], func=AF.Exp, scale=SCALE), reads=[sR], writes=[pR])
                s.op("dve", lambda e: e.tensor_tensor(out=pT[:, pi, :].rearrange("p (g n) -> p g n", g=4), in0=pT[:, pi, :].rearrange("p (g n) -> p g n", g=4),
                                                      in1=cmask[:, c, rows].unsqueeze(1).to_broadcast([128, 4, 128]), op=ALU.mult),
                     reads=[pR, R("cmask")], writes=[pR])

                def pvc(e):
                    ins = None
                    for gq in range(4):
                        ins = e.matmul(ov[:, gq, 0:193], pT[:, pi, gq * 128:(gq + 1) * 128], vca[:, c, kv, :], start=(c == 0 and gq % 2 == 0), stop=(c == ncc - 1 and gq % 2 == 1))
                    return ins
                s.op("pe", pvc, reads=[pR, R("vca")], writes=oR)
            s.op("dve", lambda e: e.tensor_scalar(out=rden[:, 0, :], in0=ov[:, :, 128], scalar1=1e-30, scalar2=None, op0=ALU.max), reads=oR, writes=[R("nrden", 0)])
            s.op("dve", lambda e: e.reciprocal(out=rden[:, 0, :], in_=rden[:, 0, :]), reads=[R("nrden", 0)], writes=[R("nrden", 0)])
            s.op("dve", lambda e: e.tensor_tensor(out=tmpu[:], in0=ov[:, :, 129:193], in1=rden[:, 0, :].unsqueeze(2).to_broadcast([128, 4, 64]), op=ALU.mult),
                 reads=oR + [R("nrden", 0)], writes=[R("ntmpu")])
            s.op("dve", lambda e: e.tensor_reduce(out=imp[:], in_=tmpu[:].rearrange("p g s -> p s g"), axis=AX.X, op=ALU.add), reads=[R("ntmpu")], writes=[R("nimp")])
            s.op("dve", lambda e: e.tensor_tensor(out=coef[:, 0, :], in0=gates[:, i, kv * 12:kv * 12 + 12:3], in1=rden[:, 0, :], op=ALU.mult),
                 reads=[R("ngates", i), R("nrden", 0)], writes=[R("ncoef", 0)])
            s.op("dve", lambda e: e.tensor_tensor(out=yb[:, kv * 4:(kv + 1) * 4, :], in0=ov[:, :, 0:128], in1=coef[:, 0, :].unsqueeze(2).to_broadcast([128, 4, 128]), op=ALU.mult),
                 reads=oR + [R("ncoef", 0)], writes=[R("nyb")])
            s.op("dve", lambda e: e.tensor_tensor(out=imp[:], in0=imp[:], in1=fmvm[:, i, 64:128], op=ALU.mult), reads=[R("nimp"), R("fmvm")], writes=[R("nimp")])
            s.op("dve", lambda e: e.tensor_tensor(out=imp[:], in0=imp[:], in1=fmvm[:, i, 0:64], op=ALU.add), reads=[R("nimp"), R("fmvm")], writes=[R("nimp")])
            s.op("dve", lambda e: e.max(out=m8[:, 0, :], in_=imp[:]), reads=[R("nimp")], writes=[R("nm8")])
            s.op("dve", lambda e: e.match_replace(out=imp2[:], in_to_replace=m8[:, 0, :], in_values=imp[:], imm_value=-3.0e38), reads=[R("nimp"), R("nm8")], writes=[R("nimp2")])
            s.op("dve", lambda e: e.max(out=m8[:, 1, :], in_=imp2[:]), reads=[R("nimp2")], writes=[R("nm8")])
            s.op("dve", lambda e: e.tensor_scalar(out=negsel[:, kv, :], in0=imp[:], scalar1=m8[:, 1, 7:8], scalar2=1.0, op0=ALU.is_ge, op1=ALU.subtract),
                 reads=[R("nimp"), R("nm8")], writes=[R("nnegsel", kv)])

        def sel_mask_T(kv):
            s.op("pe", lambda e: e.transpose(psum[7][0:64, 0:128], negsel[:, kv, :], c128[:, 0, :]), reads=[R("nnegsel", kv), R("c128")], writes=[psR[7]])
            s.op("act", lambda e: e.activation(out=nsT4[:, kv, :, :], in_=psum[7][0:64, 0:128].unsqueeze(1).to_broadcast([64, 4, 128]), func=AF.Copy),
                 reads=[psR[7]], writes=[R("nnsT4", kv)])

        def out_tile(i):
            tt_i = i // 4
            s.op("dve", lambda e: e.tensor_tensor(out=sqy[:], in0=yb[:], in1=yb[:], op=ALU.mult), reads=[R("nyb")], writes=[R("nsqy")])
            s.op("dve", lambda e: e.tensor_reduce(out=ss1[:, 0:1], in_=sqy[:].rearrange("p h d -> p (h d)"), axis=AX.X, op=ALU.add), reads=[R("nsqy")], writes=[R("nss1")])
            s.op("act", lambda e: e.activation(out=ss1[:, 0:1], in_=ss1[:, 0:1], func=AF.Sqrt, scale=1.0 / 1024, bias=cst[:, 1:2]), reads=[R("nss1"), R("cst")], writes=[R("nss1")])
            s.op("dve", lambda e: e.reciprocal(out=ss1[:, 0:1], in_=ss1[:, 0:1]), reads=[R("nss1")], writes=[R("nss1")])
            s.op("dve", lambda e: e.scalar_tensor_tensor(out=yn[:].rearrange("p h d -> p (h d)"), in0=yb[:].rearrange("p h d -> p (h d)"), scalar=ss1[:, 0:1], in1=outn[:],
                                                          op0=ALU.mult, op1=ALU.mult), reads=[R("nyb"), R("nss1"), R("outn")], writes=[R("nyn")])
            transposes(6, [yn[:, h, :] for h in range(8)], [R("nyn")])
            ci = i % 4
            s.op("act", lambda e: e.activation(out=stg[:, :, ci * 128:(ci + 1) * 128], in_=psb[:, 6 * 1024:7 * 1024].rearrange("p (h n) -> p h n", h=8), func=AF.Copy),
                 reads=[psR[6]], writes=[R("nstg")])
            if ci == 3:
                tok = slice(tt_i * 512, (tt_i + 1) * 512)
                s.dma(g["mixT"].ap()[512:1536, tok].rearrange("(h p) n -> p h n", p=128), stg[:], reads=[R("nstg")], writes=[R("mixT", tt_i)], q="act")

        jobs = g["cast_jobs"](l + 1) if l + 1 < P.L else []
        per_tile = (len(jobs) + NQ - 1) // NQ if jobs else 0
        prep_dve(0)
        prep_pe(0)
        for i in range(NQ):
            for kv in range(2):
                cmp_topk(i, kv)
            if i + 1 < NQ:
                prep_dve(i + 1)
            for kv in range(2):
                branch(i, kv, "win")
                sel_mask_T(kv)
                branch(i, kv, "sel")
            out_tile(i)
            if i + 1 < NQ:
                prep_pe(i + 1)


def tt_phase(P, s, g, body, sfx):
    nc = P.nc
    T, L = P.T, P.L
    NT = T // NTOK
    psum, psR = g["psum"], g["psR"]
    w16 = g["w16"]
    ones32, gains_sb = g["ones32"], g["gains_sb"]

    with ExitStack() as ts:
        def sb(name, shape, dt):
            return ts.enter_context(nc.sbuf_tensor(name + sfx, list(shape), dt))

        x_sb = sb("x_sb", [128, KC, NTOK], F32)
        h_sb = sb("h_sb", [128, KC, NTOK], BF16)
        act_sb = sb("act_sb", [128, NFF, NTOK], BF16)
        wbuf = sb("wbuf", [128, 4, 8192], BF16)
        sq_sb = sb("sq_sb", [128, 2, NTOK], F32)
        rstd_sb = sb("rstd_sb", [128, NTOK], F32)
        sg_sb = sb("sg_sb", [128, 2, NTOK], F32)
        stg_sb = sb("stg_sb", [128, 3, NTOK], F32)
        stgb_sb = sb("stgb_sb", [128, 3, NTOK], BF16)
        xR = [P.R("x_sb", i) for i in range(KC)]
        hR = P.R("h_sb")
        actR = [P.R("act_sb", i) for i in range(NFF)]
        wrot = Rot([(i, P.R("wbuf", i)) for i in range(4)])
        sqrot = Rot([(i, P.R("sq", i)) for i in range(2)])
        sgrot = Rot([(i, P.R("sg", i)) for i in range(2)])
        stgrot = Rot([(i, P.R("stg", i)) for i in range(3)])
        stgbrot = Rot([(i, P.R("stgb", i)) for i in range(3)])
        rstdR = P.R("rstd")
        PG = Rot([(0, psR[0]), (1, psR[1])])
        PU = Rot([(2, psR[2]), (3, psR[3])])
        PO = Rot([(4, psR[4]), (5, psR[5])])
        PSTAT = (6, psR[6])

        def load_w(nm, l, pc, nel):
            bi, bR = wrot.next()
            s.dma(wbuf[:, bi, 0:nel], w16[nm].ap()[l, pc], reads=[P.R("w", nm, l, pc)], writes=[bR])
            return bi, bR

        def norm(l, which):
            pi, pR = PSTAT
            for kc in range(KC):
                qi, qR = sqrot.next()
                s.op("act", lambda e: e.activation(out=sq_sb[:, qi, :], in_=x_sb[:, kc, :], func=AF.Square),
                     reads=[xR[kc]], writes=[qR])
                s.op("pe", lambda e: e.matmul(psum[pi][:], ones32[:], sq_sb[:, qi, :], start=(kc == 0), stop=(kc == KC - 1)),
                     reads=[qR, P.R("ones32")], writes=[pR])
            s.op("act", lambda e: e.activation(out=rstd_sb[:], in_=psum[pi][:], func=AF.Sqrt, scale=1.0 / D, bias=eps_sb[:, 0:1]),
                 reads=[pR, P.R("eps")], writes=[rstdR])
            s.op("dve", lambda e: e.reciprocal(out=rstd_sb[:], in_=rstd_sb[:]), reads=[rstdR], writes=[rstdR])
            gbase = (l * 3 + which) * KC
            for kc in range(KC):
                s.op("dve", lambda e: e.scalar_tensor_tensor(out=h_sb[:, kc, :], in0=x_sb[:, kc, :],
                                                              scalar=gains_sb[:, gbase + kc:gbase + kc + 1], in1=rstd_sb[:],
                                                              op0=ALU.mult, op1=ALU.mult),
                     reads=[xR[kc], rstdR, P.R("gains")], writes=[hR])

        eps_sb = sb("eps_sb", [128, 1], F32)
        s.op("dve", lambda e: e.memset(eps_sb[:], EPS), writes=[P.R("eps")])

        def ffn(l, which):
            nm_gu = "wgu%d" % which
            nm_d = "wd%d" % which
            norm(l, 0 if which == 1 else 2)
            for f in range(NFF):
                bi, bR = load_w(nm_gu, l, f, 2 * KC * 128)
                gi, gR = PG.next()
                ui, uR = PU.next()
                mm_group(s, psum[gi][:], [(wbuf[:, bi, kc * 128:(kc + 1) * 128], h_sb[:, kc, :]) for kc in range(KC)],
                         reads=[bR, hR], writes=[gR])
                mm_group(s, psum[ui][:], [(wbuf[:, bi, (KC + kc) * 128:(KC + kc + 1) * 128], h_sb[:, kc, :]) for kc in range(KC)],
                         reads=[bR, hR], writes=[uR])
                si, sR = sgrot.next()
                s.op("act", lambda e: e.activation(out=sg_sb[:, si, :], in_=psum[gi][:], func=AF.Silu), reads=[gR], writes=[sR])
                s.op("dve", lambda e: e.tensor_tensor(out=act_sb[:, f, :], in0=sg_sb[:, si, :], in1=psum[ui][:], op=ALU.mult),
                     reads=[sR, uR], writes=[actR[f]])
            for m in range(KC):
                bi, bR = load_w(nm_d, l, m, NFF * 128)
                oi, oR = PO.next()
                mm_group(s, psum[oi][:], [(wbuf[:, bi, fc * 128:(fc + 1) * 128], act_sb[:, fc, :]) for fc in range(NFF)],
                         reads=[bR] + actR, writes=[oR])
                s.op("dve", lambda e: e.scalar_tensor_tensor(out=x_sb[:, m, :], in0=psum[oi][:], scalar=0.5, in1=x_sb[:, m, :],
                                                              op0=ALU.mult, op1=ALU.add),
                     reads=[oR, xR[m]], writes=[xR[m]])

        def wout(l, t):
            tok = slice(t * NTOK, (t + 1) * NTOK)
            s.dma(h_sb[:], g["mixT"].ap()[:, tok].rearrange("(kc p) n -> p kc n", p=128),
                  reads=[P.R("mixT", t)], writes=[hR])
            for m in range(KC):
                bi, bR = load_w("wout", l, m, KC * 128)
                oi, oR = PO.next()
                mm_group(s, psum[oi][:], [(wbuf[:, bi, kc * 128:(kc + 1) * 128], h_sb[:, kc, :]) for kc in range(KC)],
                         reads=[bR, hR], writes=[oR])
                s.op("dve", lambda e: e.tensor_tensor(out=x_sb[:, m, :], in0=psum[oi][:], in1=x_sb[:, m, :], op=ALU.add),
                     reads=[oR, xR[m]], writes=[xR[m]])

        cm_dst = ([("lruT", 128 * i, 128, F32) for i in range(8)] + [("kvcT", 128 * i, 128, BF16) for i in range(4)]
                  + [("gqkT", 128 * i, 128, F32) for i in range(4)] + [("gaT", 0, 16, BF16)])
        tm_dst = [("q_tm", 0, F32), ("q_tm", 512, F32), ("ksw_tm", 0, F32), ("ksw_tm", 512, F32), ("gkg_tm", 0, F32),
                  ("gv_tm", 0, BF16), ("gg_tm", 0, F32)]

        def proj(l, t):
            tok = slice(t * NTOK, (t + 1) * NTOK)
            norm(l, 1)
            pieces = [("wincm", c, KC * 128) for c in range(WIN_CM)] + [("wintm", pc, KC * 512) for pc in range(WIN_TM)]
            loaded = {}

            def ensure(k):
                if k < len(pieces) and k not in loaded:
                    loaded[k] = load_w(pieces[k][0], l, pieces[k][1], pieces[k][2])
            ensure(0)
            ensure(1)
            for c in range(WIN_CM):
                ensure(c + 2)
                bi, bR = loaded[c]
                name, r0, nr, dt = cm_dst[c]
                oi, oR = PO.next()
                mm_group(s, psum[oi][0:nr, :], [(wbuf[:, bi, kc * 128:kc * 128 + nr], h_sb[:, kc, :]) for kc in range(KC)],
                         reads=[bR, hR], writes=[oR])
                if dt == F32:
                    gi, gR = stgrot.next()
                    dst_sb = stg_sb[0:nr, gi, :]
                else:
                    gi, gR = stgbrot.next()
                    dst_sb = stgb_sb[0:nr, gi, :]
                s.op("act", lambda e: e.activation(out=dst_sb, in_=psum[oi][0:nr, :], func=AF.Copy), reads=[oR], writes=[gR])
                s.dma(g[name].ap()[r0:r0 + nr, tok], dst_sb, reads=[gR], writes=[P.R(name, t)], q="act")
            for pc in range(WIN_TM):
                ensure(WIN_CM + pc + 2)
                bi, bR = loaded[WIN_CM + pc]
                name, c0, dt = tm_dst[pc]
                for sub in range(NTOK // 128):
                    oi, oR = PO.next()
                    mm_group(s, psum[oi][:], [(h_sb[:, kc, sub * 128:(sub + 1) * 128], wbuf[:, bi, kc * 512:(kc + 1) * 512])
                                               for kc in range(KC)], reads=[bR, hR], writes=[oR])
                    if dt == F32:
                        gi, gR = stgrot.next()
                        dst_sb = stg_sb[:, gi, :]
                    else:
                        gi, gR = stgbrot.next()
                        dst_sb = stgb_sb[:, gi, :]
                    eng = "act" if sub % 2 == 0 else "dve"
                    if eng == "act":
                        s.op("act", lambda e: e.activation(out=dst_sb, in_=psum[oi][:], func=AF.Copy), reads=[oR], writes=[gR])
                    else:
                        s.op("dve", lambda e: e.tensor_copy(out=dst_sb, in_=psum[oi][:]), reads=[oR], writes=[gR])
                    r0 = t * NTOK + sub * 128
                    s.dma(g[name].ap()[r0:r0 + 128, c0:c0 + 512], dst_sb, reads=[gR], writes=[P.R(name, t)], q="act")

        def load_x(src, t, srcname):
            tok = slice(t * NTOK, (t + 1) * NTOK)
            for kc in range(KC):
                s.dma(x_sb[:, kc, :], src.ap()[kc * 128:(kc + 1) * 128, tok], reads=[P.R(srcname, t, kc)], writes=[xR[kc]])

        def store_x(dst, t, dstname):
            tok = slice(t * NTOK, (t + 1) * NTOK)
            for kc in range(KC):
                s.dma(dst.ap()[kc * 128:(kc + 1) * 128, tok], x_sb[:, kc, :], reads=[xR[kc]], writes=[P.R(dstname, t, kc)], q="act")

        body(dict(load_x=load_x, store_x=store_x, ffn=ffn, proj=proj, wout=wout))


CM_COLS = ([list(range(128 * i, 128 * (i + 1))) for i in range(8)]
           + [list(range(2048 + 128 * i, 2048 + 128 * (i + 1))) for i in range(4)]
           + [list(range(3608 + 128 * i, 3608 + 128 * (i + 1))) for i in range(4)]
           + [list(range(5144, 5160)) + [-1] * 112])
TM_COLS = [list(range(1024, 1536)), list(range(1536, 2048)), list(range(2560, 3072)), list(range(3072, 3584)),
           list(range(3864, 4120)) + list(range(3584, 3608)) + [-1] * 232,
           list(range(4120, 4632)), list(range(4632, 5144))]


def _tile_cols(W, cols):
    cols = np.asarray(cols)
    Wz = np.concatenate([W, np.zeros((W.shape[0], 1), W.dtype)], axis=1)
    sel = Wz[:, cols]
    return sel.reshape(KC, 128, len(cols)).transpose(1, 0, 2)


def prep_weights(inp, L):
    out = {}
    g = np.stack([inp["ffn1_norm"][:L], inp["mix_norm"][:L], inp["ffn2_norm"][:L]], axis=1)
    out["gains"] = np.ascontiguousarray(g.reshape(L, 3, KC, 128).transpose(3, 0, 1, 2).reshape(128, L * 3 * KC))
    ffn_w = {1: (inp["ffn1_w_gate"], inp["ffn1_w_up"], inp["ffn1_w_down"]),
             2: (inp["ffn2_w_gate"], inp["ffn2_w_up"], inp["ffn2_w_down"])}
    for which in (1, 2):
        wg = ffn_w[which][0][:L].reshape(L, KC, 128, NFF, 128)
        wu = ffn_w[which][1][:L].reshape(L, KC, 128, NFF, 128)
        gu = np.stack([wg, wu], axis=1)
        out["wgu%d" % which] = np.ascontiguousarray(gu.transpose(0, 4, 3, 1, 2, 5)).reshape(L, NFF, 128, 2 * KC * 128)
        wd = ffn_w[which][2][:L].reshape(L, NFF, 128, KC, 128)
        out["wd%d" % which] = np.ascontiguousarray(wd.transpose(0, 3, 2, 1, 4)).reshape(L, KC, 128, NFF * 128)
    win = inp["w_in"][:L]
    out["wincm"] = np.stack([np.stack([_tile_cols(win[l], c) for c in CM_COLS]) for l in range(L)]).reshape(L, WIN_CM, 128, KC * 128)
    out["wintm"] = np.stack([np.stack([_tile_cols(win[l], c) for c in TM_COLS]) for l in range(L)]).reshape(L, WIN_TM, 128, KC * 512)
    wo = inp["w_out"][:L].reshape(L, KC, 128, KC, 128)
    out["wout"] = np.ascontiguousarray(wo.transpose(0, 3, 2, 1, 4)).reshape(L, KC, 128, KC * 128)
    return {k: np.ascontiguousarray(v, dtype=np.float32) for k, v in out.items()}


def make_consts(T):
    c = np.zeros((128, 6, 128), np.float32)
    i = np.arange(128)
    c[:, 0, :] = np.eye(128)
    c[:, 1, :] = np.where(i[:, None] <= i[None, :], -1.0 / 16.0, 0.0)
    c[:, 2, :] = np.where(i[:, None] > i[None, :], -1.0 / 16.0, 0.0)
    c[:, 3, :] = np.where(i[:, None] <= i[None, :], 1.0, 0.0)
    c[:, 4, :] = np.where(i[:, None] <= i[None, :], 0.0, -30000.0)
    c[:, 5, :] = np.where(i[:, None] > i[None, :], 0.0, -30000.0)
    out = {"c128": c}
    NCB = (T - 32) // 16 + 1
    NS = T // 64
    t = np.arange(T)
    n = np.arange(256)
    out["cmaskT"] = ((n[:, None] < NCB) & (16 * n[:, None] + 31 <= t[None, :])).astype(np.float32)
    sidx = np.arange(64)
    out["Efull"] = np.where((t[None, :] // 64) == sidx[:, None], 30000.0, 0.0).astype(np.float32)
    inv = (np.float32(1.0) / (np.float32(500000.0) ** (np.arange(0, 32, 2, dtype=np.float32) / np.float32(32)))).astype(np.float32)
    ang = (t.astype(np.float32)[:, None] * inv[None, :]).astype(np.float32)
    out["ropecs"] = np.concatenate([np.cos(ang), np.sin(ang)], axis=1).astype(np.float32)
    cur = t // 64
    forced = (sidx[None, :] == 0) | (sidx[None, :] == cur[:, None]) | (sidx[None, :] == cur[:, None] - 1)
    valid = (sidx[None, :] * 64 <= t[:, None]) & (sidx[None, :] < NS)
    forced = forced & valid
    fm = np.where(forced, 1e30, np.where(valid, 0.0, -1e30))
    vm = (valid & ~forced).astype(np.float32)
    out["fmvm"] = np.concatenate([fm, vm], axis=1).astype(np.float32)
    cstart = n * 16
    sstart = sidx * 64
    ov = (n[:, None] < NCB) & (sidx[None, :] < NS) & (cstart[:, None] < sstart[None, :] + 64) & (cstart[:, None] + 32 > sstart[None, :])
    out["ovl"] = ov.astype(np.float32)
    return out


def prep_small(inp, L):
    out = {}
    lv = np.zeros((L, 128, 4, 9), np.float32)

    def cp(v):
        return v.reshape(L, 4, 128).transpose(0, 2, 1)
    for k in range(4):
        lv[..., k] = cp(inp["lru_conv_w"][:L, k])
    lv[..., 4] = cp(inp["lru_conv_b"][:L])
    lv[..., 5] = cp(inp["lru_gate_a_b"][:L])
    lv[..., 6] = cp(inp["lru_gate_x_b"][:L])
    lv[..., 7] = cp(inp["lru_lambda"][:L])
    lv[..., 8] = cp(inp["lru_out_norm"][:L])
    out["lruv"] = lv
    gw = np.zeros((L, 2, 4, 128, 128), np.float32)
    for a, nm in enumerate(("lru_gate_a_w", "lru_gate_x_w")):
        w = inp[nm][:L]
        for c in range(4):
            gw[:, a, c, 0:64, 0:64] = w[:, 2 * c]
            gw[:, a, c, 64:128, 64:128] = w[:, 2 * c + 1]
    out["lrug"] = gw
    out["glaw"] = np.concatenate([inp["gla_a_w2"][:L], inp["gla_a_b"][:L, None, :]], axis=1)
    out["glan"] = inp["gla_out_norm"][:L]
    out["nsan"] = np.stack([inp["nsa_q_norm"][:L], inp["nsa_k_cmp_norm"][:L], inp["nsa_k_sel_norm"][:L], inp["nsa_k_win_norm"][:L]], axis=1)
    out["nsaon"] = inp["nsa_out_norm"][:L]
    out["nsagb"] = inp["nsa_gate_b"][:L]
    out["cmpw1"] = np.stack([inp[k][:L].reshape(L, 32, 128, 128).transpose(0, 2, 1, 3).reshape(L, 128, 4096)
                             for k in ("nsa_cmp_w1_k", "nsa_cmp_w1_v")], axis=1)
    out["cmpw2"] = np.stack([inp["nsa_cmp_w2_k"][:L], inp["nsa_cmp_w2_v"][:L]], axis=1)
    out["cmppe"] = np.stack([inp["nsa_cmp_pe_k"][:L].transpose(0, 2, 1), inp["nsa_cmp_pe_v"][:L].transpose(0, 2, 1)], axis=1)
    return {k: np.ascontiguousarray(v, dtype=np.float32) for k, v in out.items()}


_CACHE = {}


def kernel(**inputs):
    inp = {k: np.asarray(v) for k, v in inputs.items()}
    x = inp["x"]
    B, T, _ = x.shape
    L = inp["w_in"].shape[0]
    key = (T, L)
    if key not in _CACHE:
        _CACHE[key] = build(T, L)
    nc = _CACHE[key]
    shared = {}
    shared.update(prep_weights(inp, L))
    shared.update(prep_small(inp, L))
    shared.update(make_consts(T))
    in_maps = []
    for b in range(B):
        m = dict(shared)
        m["xT"] = np.ascontiguousarray(x[b].T)
        in_maps.append(m)
    res = run_bass_kernel_spmd(nc, in_maps, core_ids=list(range(B)))
    out = np.stack([np.asarray(r["outT"]).T for r in res.results], axis=0)
    return np.ascontiguousarray(out.astype(np.float32))
```

```python
from contextlib import ExitStack
import numpy as np
import concourse.bass as bass
import concourse.mybir as mybir
from concourse.bass_utils import run_bass_kernel_spmd

F32 = mybir.dt.float32
BF16 = mybir.dt.bfloat16
AF = mybir.ActivationFunctionType
ALU = mybir.AluOpType
AX = mybir.AxisListType

D = 2048
DFF = 5632
NFF = DFF // 128
KC = D // 128
EPS = 1e-6
NTOK = 512


class Res:
    __slots__ = ("w", "r")

    def __init__(self):
        self.w = None
        self.r = {}


class Sched:
    def __init__(self, nc, es, ndma=40):
        self.nc = nc
        self.eng = {"pe": nc.tensor, "act": nc.scalar, "dve": nc.vector, "pool": nc.gpsimd, "sp": nc.sync}
        self.sem = {}
        self.cnt = {}
        self.seen = {e: {} for e in self.eng}
        for e in ("pe", "act", "dve", "pool"):
            self.sem[("E", e)] = es.enter_context(nc.semaphore("sem_" + e))
            self.cnt[e] = 0
        self.ndma = {"sp": ndma, "pool": 8, "act": 8}
        for q, n in self.ndma.items():
            for i in range(n):
                self.sem[("D", q, i)] = es.enter_context(nc.semaphore("dsem_%s%d" % (q, i)))
        self.dma_i = {"sp": 0, "pool": 0, "act": 0}
        self.dlast = {}

    def _waits(self, eng, reads, writes):
        need = {}
        seen = self.seen[eng]
        own = ("E", eng)

        def add(k, v):
            if seen.get(k, 0) >= v:
                return
            if need.get(k, 0) < v:
                need[k] = v

        for r in reads:
            if r.w is not None:
                k, v = r.w
                if not (k == own and eng == "pe"):
                    add(k, v)
        pe_own = (eng == "pe")
        for w in writes:
            if w.w is not None and not (pe_own and w.w[0] == own):
                add(*w.w)
            for k, v in w.r.items():
                if not (pe_own and k == own):
                    add(k, v)
        return need

    def _emit_waits(self, eng, need):
        e = self.eng[eng]
        for k, v in need.items():
            e.wait_ge(self.sem[k], v)
            self.seen[eng][k] = v

    def op(self, eng, fn, reads=(), writes=()):
        need = self._waits(eng, reads, writes)
        self._emit_waits(eng, need)
        ins = fn(self.eng[eng])
        self.cnt[eng] += 1
        k = ("E", eng)
        v = self.cnt[eng]
        ins.then_inc(self.sem[k], 1)
        for r in reads:
            r.r[k] = v
        for w in writes:
            w.w = (k, v)
            w.r = {}

    def dma(self, out, in_, reads=(), writes=(), q="sp"):
        i = self.dma_i[q]
        self.dma_i[q] += 1
        slot = i % self.ndma[q]
        rnd = i // self.ndma[q]
        need = self._waits(q, reads, writes)
        k = ("D", q, slot)
        if rnd > 0 and self.seen[q].get(k, 0) < 16 * rnd:
            need[k] = max(need.get(k, 0), 16 * rnd)
        self._emit_waits(q, need)
        ins = self.eng[q].dma_start(out=out, in_=in_)
        v = 16 * (rnd + 1)
        ins.then_inc(self.sem[k], 16)
        self.dlast[k] = v
        for r in reads:
            r.r[k] = v
        for w in writes:
            w.w = (k, v)
            w.r = {}

    def barrier(self, include_pool=False):
        for eng in self.eng:
            need = {}
            for e2 in ("pe", "act", "dve", "pool"):
                k = ("E", e2)
                if e2 != eng and self.cnt[e2] > self.seen[eng].get(k, 0):
                    need[k] = self.cnt[e2]
            for k, v in self.dlast.items():
                if k[1] == "pool" and not include_pool:
                    continue
                if v > self.seen[eng].get(k, 0):
                    need[k] = v
            self._emit_waits(eng, need)


class Rot:
    def __init__(self, items):
        self.items = items
        self.i = 0

    def next(self):
        it = self.items[self.i % len(self.items)]
        self.i += 1
        return it


def mm_group(s, out, pairs, reads, writes, **kw):
    n = len(pairs)

    def fn(e):
        ins = None
        for i, (a, b) in enumerate(pairs):
            ins = e.matmul(out, a, b, start=(i == 0), stop=(i == n - 1), **kw)
        return ins

    s.op("pe", fn, reads=reads, writes=writes)


class Prog:
    def __init__(self, T, L, debug=()):
        self.T = T
        self.L = L
        self.debug = set(debug)
        self.nc = bass.Bass("TRN2", target_bir_lowering=False)
        self.dram = {}
        self.res = {}

    def din(self, name, shape, dt=F32):
        t = self.nc.dram_tensor(name, list(shape), dt, kind="ExternalInput")
        self.dram[name] = t
        return t

    def dout(self, name, shape, dt=F32):
        t = self.nc.dram_tensor(name, list(shape), dt, kind="ExternalOutput")
        self.dram[name] = t
        return t

    def dscr(self, name, shape, dt=F32):
        kind = "ExternalOutput" if name in self.debug else "Internal"
        t = self.nc.dram_tensor(name, list(shape), dt, kind=kind)
        self.dram[name] = t
        return t

    def R(self, *key):
        r = self.res.get(key)
        if r is None:
            r = Res()
            self.res[key] = r
        return r


WIN_CM = 17
WIN_TM = 7


def build(T, L, debug=(), stop_after=None, only_mix=None, skip_tt0=False, stage=99):
    P = Prog(T, L, debug)
    nc = P.nc
    NT = T // NTOK
    es = ExitStack()
    with es:
        s = Sched(nc, es)
        xT_in = P.din("xT", [D, T])
        outT = P.dout("outT", [D, T])
        gains = P.din("gains", [128, L * 3 * KC])
        w32 = {}
        w16 = {}
        wspec = {
            "wgu1": (NFF, 2 * KC * 128), "wd1": (KC, NFF * 128),
            "wgu2": (NFF, 2 * KC * 128), "wd2": (KC, NFF * 128),
            "wincm": (WIN_CM, KC * 128), "wintm": (WIN_TM, KC * 512), "wout": (KC, KC * 128),
            "cmpw1": (2, 4096), "cmpw2": (2, 128), "cmppe": (2, 32),
        }
        for nm, (npc, el) in wspec.items():
            w32[nm] = P.din(nm, [L, npc, 128, el])
            w16[nm] = P.dscr(nm + "_bf", [L, npc, 128, el], BF16)
        xT = P.dscr("xT_s", [D, T])
        mixT = P.dscr("mixT", [D, T], BF16)
        cm_rows = {"lru": 1024, "kvc": 512, "gqk": 512}
        lruT = P.dscr("lruT", [1024, T])
        kvcT = P.dscr("kvcT", [512, T], BF16)
        gqkT = P.dscr("gqkT", [512, T])
        gaT = P.dscr("gaT", [16, T], BF16)
        q_tm = P.dscr("q_tm", [T, 1024])
        ksw_tm = P.dscr("ksw_tm", [T, 1024])
        gkg_tm = P.dscr("gkg_tm", [T, 512])
        gv_tm = P.dscr("gv_tm", [T, 512], BF16)
        gg_tm = P.dscr("gg_tm", [T, 512])

        lruv = P.din("lruv", [L, 128, 4, 9])
        lrug = P.din("lrug", [L, 2, 4, 128, 128])
        glaw = P.din("glaw", [L, 17, 256])
        glan = P.din("glan", [L, 128])
        c128_d = P.din("c128", [128, 6, 128])
        NCBp = 256
        cmaskT = P.din("cmaskT", [256, T])
        Efull = P.din("Efull", [64, T])
        ropecs = P.din("ropecs", [T, 32])
        fmvm = P.din("fmvm", [T, 128])
        ovl = P.din("ovl", [256, 64])
        nsan = P.din("nsan", [L, 4, 128])
        nsaon = P.din("nsaon", [L, 1024])
        nsagb = P.din("nsagb", [L, 24])
        cast_order = ["wgu1", "wd1", "wincm", "wintm", "cmpw1", "cmpw2", "cmppe", "wout", "wgu2", "wd2"]

        def cast_jobs(l):
            jobs = []
            for nm in cast_order:
                npc, el = wspec[nm]
                grp = max(1, (1 << 20) // (128 * el))
                for p0 in range(0, npc, grp):
                    jobs.append((nm, l, p0, min(npc, p0 + grp)))
            return jobs

        def cast_job(job):
            nm, l, p0, p1 = job
            src = w32[nm].ap()[l, p0:p1].rearrange("c p e -> (c p) e")
            dst = w16[nm].ap()[l, p0:p1].rearrange("c p e -> (c p) e")
            s.dma(dst, src, writes=[P.R("w", nm, l, pc) for pc in range(p0, p1)], q="pool")

        def cast_layer(l):
            for job in cast_jobs(l):
                cast_job(job)

        def cast_layer_old(l):
            for nm, (npc, el) in wspec.items():
                grp = max(1, (1 << 20) // (128 * el))
                for p0 in range(0, npc, grp):
                    p1 = min(npc, p0 + grp)
                    src = w32[nm].ap()[l, p0:p1].rearrange("c p e -> (c p) e")
                    dst = w16[nm].ap()[l, p0:p1].rearrange("c p e -> (c p) e")
                    ws = [P.R("w", nm, l, pc) for pc in range(p0, p1)]
                    s.dma(dst, src, writes=ws, q="pool")

        cmask_bf = P.dscr("cmask_bf", [256, T], BF16)
        Efull_bf = P.dscr("Efull_bf", [64, T], BF16)
        ovl_bf = P.dscr("ovl_bf", [256, 64], BF16)
        s.dma(cmask_bf.ap(), cmaskT.ap(), writes=[P.R("cmask_bf")], q="pool")
        s.dma(Efull_bf.ap(), Efull.ap(), writes=[P.R("Efull_bf")], q="pool")
        s.dma(ovl_bf.ap(), ovl.ap(), writes=[P.R("ovl_bf")], q="pool")
        cast_layer(0)

        def sb(name, shape, dt):
            return es.enter_context(nc.sbuf_tensor(name, list(shape), dt))

        psall = es.enter_context(nc.psum_tensor("psall", [128, 4096], F32))
        psb = psall.bitcast(BF16)
        psum = [psall[:, i * 512:(i + 1) * 512] for i in range(8)]
        psR = [P.R("ps", i) for i in range(8)]
        ones32 = sb("ones32", [128, 128], F32)
        gains_sb = sb("gains_sb", [128, L * 3 * KC], F32)
        s.op("dve", lambda e: e.memset(ones32[:], 1.0), writes=[P.R("ones32")])
        s.dma(gains_sb[:], gains.ap(), writes=[P.R("gains")])
        cst = sb("cst", [128, 4], F32)
        s.op("dve", lambda e: e.memset(cst[:, 0:1], 1.0), writes=[P.R("cst")])
        s.op("dve", lambda e: e.memset(cst[:, 1:2], EPS), writes=[P.R("cst")])
        c128 = sb("c128_sb", [128, 6, 128], F32)
        s.dma(c128[:], c128_d.ap(), writes=[P.R("c128")])
        identb = sb("identb", [128, 128], BF16)
        s.op("dve", lambda e: e.tensor_copy(out=identb[:], in_=c128[:, 0, :]), reads=[P.R("c128")], writes=[P.R("identb")])

        g = dict(locals())
        orchestrate(P, s, g)
        s.barrier(include_pool=True)
    return nc


def orchestrate(P, s, g):
    T, L = P.T, P.L
    NT = T // NTOK
    stop_after = g.get("stop_after")

    def body0(f):
        for t in range(NT):
            f["load_x"](g["xT_in"], t, "xT_in")
            f["ffn"](0, 1)
            f["proj"](0, t)
            f["store_x"](g["xT"] if stop_after != "tt0" else g["outT"], t, "xT")
    if not g.get("skip_tt0"):
        tt_phase(P, s, g, body0, "a")
        s.barrier()
    if stop_after == "tt0":
        return
    for l in range(L):
        mix_phase(P, s, g, l)
        s.barrier()
        if stop_after == "mix%d" % l:
            return

        def body(f, l=l):
            for t in range(NT):
                f["load_x"](g["xT"], t, "xT")
                f["wout"](l, t)
                f["ffn"](l, 2)
                if l + 1 < L:
                    f["ffn"](l + 1, 1)
                    f["proj"](l + 1, t)
                    f["store_x"](g["xT"], t, "xT")
                else:
                    f["store_x"](g["outT"], t, "outT")
        tt_phase(P, s, g, body, "b%d" % l)
        s.barrier()


def mix_phase(P, s, g, l):
    only = g.get("only_mix")
    if only is None or "lru" in only:
        lru_mix(P, s, g, l)
        s.barrier()
    if only is None or "gla" in only:
        gla_mix(P, s, g, l)
        s.barrier()
    if only is None or "nsa" in only:
        nsa_mix(P, s, g, l)
        s.barrier()


def gelu2(s, dst, src, tmp, rsrc, rtmp, rdst):
    s.op("dve", lambda e: e.tensor_tensor(out=tmp, in0=src, in1=src, op=ALU.mult), reads=[rsrc], writes=[rtmp])
    s.op("dve", lambda e: e.tensor_scalar(out=tmp, in0=tmp, scalar1=0.044715, scalar2=1.0, op0=ALU.mult, op1=ALU.add),
         reads=[rtmp], writes=[rtmp])
    s.op("dve", lambda e: e.tensor_tensor(out=tmp, in0=tmp, in1=src, op=ALU.mult), reads=[rtmp, rsrc], writes=[rtmp])
    s.op("act", lambda e: e.activation(out=tmp, in_=tmp, func=AF.Tanh, scale=0.7978845608028654), reads=[rtmp], writes=[rtmp])
    s.op("dve", lambda e: e.scalar_tensor_tensor(out=dst, in0=tmp, scalar=1.0, in1=src, op0=ALU.add, op1=ALU.mult),
         reads=[rtmp, rsrc], writes=[rdst])


def lru_mix(P, s, g, l):
    nc = P.nc
    T = P.T
    NTT = T // 512
    psum, ones32, cst = g["psum"], g["ones32"], g["cst"]
    lruT, mixT = g["lruT"], g["mixT"]
    R = P.R
    with ExitStack() as ms:
        def sb(name, shape, dt):
            return ms.enter_context(nc.sbuf_tensor("lru_%s_%d" % (name, l), list(shape), dt))
        lv = sb("lv", [128, 4, 9], F32)
        gw = sb("gw", [128, 2, 4, 128], F32)
        c12 = sb("c12", [128, 2, 4], F32)
        u_sb = sb("u", [128, 4, 515], F32)
        y_sb = sb("y", [128, 4, 512], F32)
        xc = sb("xc", [128, 4, 512], F32)
        r_sb = sb("r", [128, 4, 512], F32)
        i_sb = sb("i", [128, 4, 512], F32)
        a_sb = sb("a", [128, 4, 512], F32)
        m_sb = sb("m", [128, 4, 512], F32)
        h_sb = sb("h", [128, 4, 512], F32)
        gl = sb("gl", [128, 4, 512], F32)
        tmp = sb("tmp", [128, 4, 512], F32)
        ya = sb("ya", [128, 4, 512], F32)
        sq = sb("sq", [128, 2, 512], F32)
        rstd = sb("rstd", [128, 512], F32)
        outb = sb("outb", [128, 4, 512], BF16)
        hprev = sb("hprev", [128, 4], F32)
        s.dma(lv[:], g["lruv"].ap()[l], writes=[R("lv")])
        s.dma(gw[:], g["lrug"].ap()[l].rearrange("a c p o -> p a c o"), writes=[R("gw")])
        s.op("act", lambda e: e.activation(out=c12[:, 0, :], in_=lv[:, :, 7], func=AF.Exp, scale=-1.0), reads=[R("lv")], writes=[R("c12")])
        s.op("act", lambda e: e.activation(out=c12[:, 0, :], in_=c12[:, 0, :], func=AF.Ln, bias=cst[:, 0:1]), reads=[R("c12"), R("cst")], writes=[R("c12")])
        s.op("dve", lambda e: e.tensor_scalar(out=c12[:, 1, :], in0=c12[:, 0, :], scalar1=-16.0, scalar2=None, op0=ALU.mult), reads=[R("c12")], writes=[R("c12")])
        s.op("dve", lambda e: e.tensor_scalar(out=c12[:, 0, :], in0=c12[:, 0, :], scalar1=-8.0, scalar2=None, op0=ALU.mult), reads=[R("c12")], writes=[R("c12")])
        sqrot = Rot([(0, R("lsq", 0)), (1, R("lsq", 1))])
        C4 = range(4)
        for tt in range(NTT):
            t0 = tt * 512
            for c in C4:
                rows = slice(c * 128, (c + 1) * 128)
                uR, yR = R("lu", c), R("ly", c)
                if tt == 0:
                    s.op("dve", lambda e: e.memset(u_sb[:, c, 0:3], 0.0), writes=[uR])
                    s.dma(u_sb[:, c, 3:515], lruT.ap()[rows, 0:512], reads=[R("lruT", 0)], writes=[uR])
                else:
                    s.dma(u_sb[:, c, :], lruT.ap()[rows, t0 - 3:t0 + 512], reads=[R("lruT", tt - 1), R("lruT", tt)], writes=[uR])
                s.dma(y_sb[:, c, :], lruT.ap()[512 + c * 128:512 + (c + 1) * 128, t0:t0 + 512], reads=[R("lruT", tt)], writes=[yR])
            for c in C4:
                s.op("dve", lambda e: e.tensor_scalar(out=xc[:, c, :], in0=u_sb[:, c, 3:515], scalar1=lv[:, c, 3:4], scalar2=lv[:, c, 4:5],
                                                      op0=ALU.mult, op1=ALU.add), reads=[R("lu", c), R("lv")], writes=[R("lxc", c)])
            for k in (2, 1, 0):
                for c in C4:
                    s.op("dve", lambda e: e.scalar_tensor_tensor(out=xc[:, c, :], in0=u_sb[:, c, k:k + 512], scalar=lv[:, c, k:k + 1],
                                                                  in1=xc[:, c, :], op0=ALU.mult, op1=ALU.add), reads=[R("lu", c), R("lxc", c)], writes=[R("lxc", c)])
            for c in C4:
                for a, (dst, nm, bcol) in enumerate(((r_sb, "lr", 5), (i_sb, "li", 6))):
                    bk = a + 2 * (c % 2)
                    pr = g["psR"][bk]
                    s.op("pe", lambda e: e.matmul(psum[bk][:], gw[:, a, c, :], xc[:, c, :], start=True, stop=True), reads=[R("gw"), R("lxc", c)], writes=[pr])
                    s.op("act", lambda e: e.activation(out=dst[:, c, :], in_=psum[bk][:], func=AF.Sigmoid, bias=lv[:, c, bcol:bcol + 1]),
                         reads=[pr, R("lv")], writes=[R(nm, c)])
            for c in C4:
                s.op("act", lambda e: e.activation(out=a_sb[:, c, :], in_=r_sb[:, c, :], func=AF.Exp, scale=c12[:, 0, c:c + 1]), reads=[R("lr", c), R("c12")], writes=[R("la", c)])
                s.op("act", lambda e: e.activation(out=m_sb[:, c, :], in_=r_sb[:, c, :], func=AF.Exp, scale=c12[:, 1, c:c + 1]), reads=[R("lr", c), R("c12")], writes=[R("lm", c)])
            for c in C4:
                s.op("act", lambda e: e.activation(out=m_sb[:, c, :], in_=m_sb[:, c, :], func=AF.Sqrt, scale=-1.0, bias=cst[:, 0:1]), reads=[R("lm", c), R("cst")], writes=[R("lm", c)])
            for c in C4:
                s.op("dve", lambda e: e.tensor_tensor(out=m_sb[:, c, :], in0=m_sb[:, c, :], in1=i_sb[:, c, :], op=ALU.mult), reads=[R("lm", c), R("li", c)], writes=[R("lm", c)])
            for c in C4:
                s.op("dve", lambda e: e.tensor_tensor(out=m_sb[:, c, :], in0=m_sb[:, c, :], in1=xc[:, c, :], op=ALU.mult), reads=[R("lm", c), R("lxc", c)], writes=[R("lm", c)])
            for c in C4:
                init = 0.0 if tt == 0 else hprev[:, c:c + 1]
                s.op("dve", lambda e: e.tensor_tensor_scan(out=h_sb[:, c, :], data0=a_sb[:, c, :], data1=m_sb[:, c, :], initial=init,
                                                           op0=ALU.mult, op1=ALU.add), reads=[R("la", c), R("lm", c), R("lhp", c)], writes=[R("lh", c)])
            for c in C4:
                s.op("dve", lambda e: e.tensor_copy(out=hprev[:, c:c + 1], in_=h_sb[:, c, 511:512]), reads=[R("lh", c)], writes=[R("lhp", c)])
            for c in C4:
                s.op("dve", lambda e: e.tensor_tensor(out=tmp[:, c, :], in0=y_sb[:, c, :], in1=y_sb[:, c, :], op=ALU.mult), reads=[R("ly", c)], writes=[R("ltmp", c)])
            for c in C4:
                s.op("dve", lambda e: e.tensor_scalar(out=tmp[:, c, :], in0=tmp[:, c, :], scalar1=0.044715, scalar2=1.0, op0=ALU.mult, op1=ALU.add),
                     reads=[R("ltmp", c)], writes=[R("ltmp", c)])
            for c in C4:
                s.op("dve", lambda e: e.tensor_tensor(out=tmp[:, c, :], in0=tmp[:, c, :], in1=y_sb[:, c, :], op=ALU.mult), reads=[R("ltmp", c), R("ly", c)], writes=[R("ltmp", c)])
            for c in C4:
                s.op("act", lambda e: e.activation(out=tmp[:, c, :], in_=tmp[:, c, :], func=AF.Tanh, scale=0.7978845608028654), reads=[R("ltmp", c)], writes=[R("ltmp", c)])
            for c in C4:
                s.op("dve", lambda e: e.scalar_tensor_tensor(out=gl[:, c, :], in0=tmp[:, c, :], scalar=1.0, in1=y_sb[:, c, :], op0=ALU.add, op1=ALU.mult),
                     reads=[R("ltmp", c), R("ly", c)], writes=[R("lgl", c)])
            for c in C4:
                s.op("dve", lambda e: e.scalar_tensor_tensor(out=ya[:, c, :], in0=gl[:, c, :], scalar=0.5, in1=h_sb[:, c, :], op0=ALU.mult, op1=ALU.mult),
                     reads=[R("lgl", c), R("lh", c)], writes=[R("lya", c)])
            for c in C4:
                qi, qR = sqrot.next()
                s.op("act", lambda e: e.activation(out=sq[:, qi, :], in_=ya[:, c, :], func=AF.Square), reads=[R("lya", c)], writes=[qR])
                s.op("pe", lambda e: e.matmul(psum[6][:], ones32[:], sq[:, qi, :], start=(c == 0), stop=(c == 3)), reads=[qR, R("ones32")], writes=[g["psR"][6]])
            s.op("act", lambda e: e.activation(out=rstd[:], in_=psum[6][:], func=AF.Sqrt, scale=1.0 / 512, bias=cst[:, 1:2]),
                 reads=[g["psR"][6], R("cst")], writes=[R("lrstd")])
            s.op("dve", lambda e: e.reciprocal(out=rstd[:], in_=rstd[:]), reads=[R("lrstd")], writes=[R("lrstd")])
            for c in C4:
                s.op("dve", lambda e: e.scalar_tensor_tensor(out=outb[:, c, :], in0=ya[:, c, :], scalar=lv[:, c, 8:9], in1=rstd[:], op0=ALU.mult, op1=ALU.mult),
                     reads=[R("lya", c), R("lrstd"), R("lv")], writes=[R("loutb", c)])
                s.dma(mixT.ap()[c * 128:(c + 1) * 128, t0:t0 + 512], outb[:, c, :], reads=[R("loutb", c)], writes=[R("mixT", tt)], q="act")


def gla_mix(P, s, g, l):
    nc = P.nc
    T = P.T
    NG = T // 512
    psum, psb, psR, ones32, cst, c128 = g["psum"], g["psb"], g["psR"], g["ones32"], g["cst"], g["c128"]
    identb = g["identb"]
    R = P.R
    with ExitStack() as ms:
        def sb(name, shape, dt):
            return ms.enter_context(nc.sbuf_tensor("gla_%s_%d" % (name, l), list(shape), dt))
        aw32 = sb("aw32", [17, 256], F32)
        aw = sb("aw", [17, 256], BF16)
        gout = sb("gout", [128, 128], F32)
        qT = sb("qT", [64, 4, 512], F32)
        kT = sb("kT", [64, 4, 512], F32)
        gaT = sb("gaT", [17, 512], BF16)
        ktm = sb("ktm", [128, 4, 256], F32)
        vtm = sb("vtm", [128, 4, 512], BF16)
        ggt = sb("ggt", [128, 4, 512], F32)
        lsp = sb("lsp", [128, 256], F32)
        ekb = sb("ekb", [128, 256], F32)
        kp = sb("kp", [128, 2, 256], BF16)
        eb = sb("eb", [64, 2, 4, 128], F32)
        enb = sb("enb", [64, 4, 128], F32)
        qt_b = sb("qtb", [64, 2, 4, 128], BF16)
        kt_b = sb("ktb", [64, 2, 4, 128], BF16)
        atm = sb("atm", [128, 2, 4, 128], BF16)
        S32 = sb("S32", [64, 4, 128], F32)
        Sb = sb("Sb", [64, 4, 128], BF16)
        osq = sb("osq", [128, 4, 128], F32)
        on = sb("on", [128, 4, 128], F32)
        ssum = sb("ssum", [128, 4], F32)
        sgg = sb("sgg", [128, 512], F32)
        yc = sb("yc", [128, 512], BF16)
        stg = sb("stg", [128, 4, 512], BF16)
        s.dma(aw32[:], g["glaw"].ap()[l], writes=[R("aw32")])
        s.op("dve", lambda e: e.tensor_copy(out=aw[:], in_=aw32[:]), reads=[R("aw32")], writes=[R("aw")])
        s.dma(gout[:], g["glan"].ap()[l:l + 1, :].to_broadcast([128, 128]), writes=[R("gout")])
        s.op("dve", lambda e: e.memset(gaT[:], 1.0), writes=[R("gaT")])
        s.op("dve", lambda e: e.memset(S32[:], 0.0), writes=[R("S32")])
        s.op("dve", lambda e: e.memset(Sb[:], 0.0), writes=[R("Sb")])
        for gi in range(NG):
            tok = slice(gi * 512, (gi + 1) * 512)
            s.dma(qT[:], g["gqkT"].ap()[0:256, tok].rearrange("(a p) n -> p a n", p=64), reads=[R("gqkT", gi)], writes=[R("gqT")])
            s.dma(kT[:], g["gqkT"].ap()[256:512, tok].rearrange("(a p) n -> p a n", p=64), reads=[R("gqkT", gi)], writes=[R("gkT")])
            s.dma(gaT[0:16, :], g["gaT"].ap()[:, tok], reads=[R("gaT", gi)], writes=[R("gaT")])
            s.dma(ktm[:], g["gkg_tm"].ap()[tok, 0:256].rearrange("(c p) n -> p c n", p=128), reads=[R("gkg_tm", gi)], writes=[R("gktm")])
            s.dma(vtm[:], g["gv_tm"].ap()[tok, :].rearrange("(c p) n -> p c n", p=128), reads=[R("gv_tm", gi)], writes=[R("gvtm")])
            s.dma(ggt[:], g["gg_tm"].ap()[tok, :].rearrange("(c p) n -> p c n", p=128), reads=[R("gg_tm", gi)], writes=[R("gggt")])
            def s1(cc):
                pb = cc % 2
                ct = slice(cc * 128, (cc + 1) * 128)
                s.op("pe", lambda e: e.matmul(psum[0][:, 0:256], gaT[:, ct], aw[:], start=True, stop=True), reads=[R("gaT"), R("aw")], writes=[psR[0]])
                s.op("act", lambda e: e.activation(out=lsp[:], in_=psum[0][:, 0:256], func=AF.Exp, scale=-1.0), reads=[psR[0]], writes=[R("lsp")])
                s.op("act", lambda e: e.activation(out=lsp[:], in_=lsp[:], func=AF.Ln, bias=cst[:, 0:1]), reads=[R("lsp"), R("cst")], writes=[R("lsp")])
                s.op("pe", lambda e: e.matmul(psum[1][:, 0:256], c128[:, 2, :], lsp[:], start=True, stop=True), reads=[R("lsp"), R("c128")], writes=[psR[1]])
                for a in range(4):
                    s.op("pe", lambda e: e.matmul(psum[2][0:64, a * 128:(a + 1) * 128], lsp[:, a * 64:(a + 1) * 64], c128[:, 1, :], start=True, stop=True),
                         reads=[R("lsp"), R("c128")], writes=[psR[2]])
                s.op("act", lambda e: e.activation(out=ekb[:], in_=psum[1][:, 0:256], func=AF.Exp), reads=[psR[1]], writes=[R("ekb")])
                s.op("dve", lambda e: e.tensor_tensor(out=kp[:, pb, :], in0=ktm[:, cc, :], in1=ekb[:], op=ALU.mult), reads=[R("gktm"), R("ekb")], writes=[R("kp", pb)])
                s.op("act", lambda e: e.activation(out=eb[:, pb].rearrange("p a n -> p (a n)"), in_=psum[2][0:64, :], func=AF.Exp), reads=[psR[2]], writes=[R("eb", pb)])
                s.op("act", lambda e: e.activation(out=enb[:].rearrange("p a n -> p (a n)"), in_=psum[2][0:64, :], func=AF.Exp, scale=-1.0), reads=[psR[2]], writes=[R("enb")])
                s.op("dve", lambda e: e.scalar_tensor_tensor(out=qt_b[:, pb], in0=qT[:, :, ct], scalar=0.125, in1=eb[:, pb], op0=ALU.mult, op1=ALU.mult),
                     reads=[R("gqT"), R("eb", pb)], writes=[R("qtb", pb)])
                s.op("dve", lambda e: e.tensor_tensor(out=kt_b[:, pb], in0=kT[:, :, ct], in1=enb[:], op=ALU.mult), reads=[R("gkT"), R("enb")], writes=[R("ktb", pb)])
                def at_fn(e):
                    ins = None
                    for h in range(4):
                        ins = e.matmul(psum[3][:, h * 128:(h + 1) * 128], kt_b[:, pb, h, :], qt_b[:, pb, h, :], start=True, stop=True)
                    return ins
                s.op("pe", at_fn, reads=[R("ktb", pb), R("qtb", pb)], writes=[psR[3]])
                s.op("dve", lambda e: e.tensor_tensor(out=atm[:, pb], in0=psum[3][:].rearrange("p (h n) -> p h n", h=4),
                                                      in1=c128[:, 3, :].unsqueeze(1).to_broadcast([128, 4, 128]), op=ALU.mult),
                     reads=[psR[3], R("c128")], writes=[R("atm", pb)])
            def s2(cc):
                pb = cc % 2
                ct = slice(cc * 128, (cc + 1) * 128)
                def o_fn(e):
                    ins = None
                    for h in range(4):
                        e.matmul(psum[4][:, h * 128:(h + 1) * 128], qt_b[:, pb, h, :], Sb[:, h, :], start=True, stop=False)
                        ins = e.matmul(psum[4][:, h * 128:(h + 1) * 128], atm[:, pb, h, :], vtm[:, cc, h * 128:(h + 1) * 128], start=False, stop=True)
                    return ins
                s.op("pe", o_fn, reads=[R("qtb", pb), R("Sb"), R("atm", pb), R("gvtm")], writes=[psR[4]])
                def kv_fn(e):
                    ins = None
                    for h in range(4):
                        ins = e.matmul(psum[5][0:64, h * 128:(h + 1) * 128], kp[:, pb, h * 64:(h + 1) * 64], vtm[:, cc, h * 128:(h + 1) * 128], start=True, stop=True)
                    return ins
                s.op("pe", kv_fn, reads=[R("kp", pb), R("gvtm")], writes=[psR[5]])
                s.op("dve", lambda e: e.tensor_tensor(out=S32[:], in0=S32[:], in1=eb[:, pb, :, 127:128].to_broadcast([64, 4, 128]), op=ALU.mult),
                     reads=[R("S32"), R("eb", pb)], writes=[R("S32")])
                s.op("dve", lambda e: e.tensor_tensor(out=S32[:], in0=S32[:], in1=psum[5][0:64, :].rearrange("p (h n) -> p h n", h=4), op=ALU.add),
                     reads=[R("S32"), psR[5]], writes=[R("S32")])
                s.op("act", lambda e: e.activation(out=Sb[:], in_=S32[:], func=AF.Copy), reads=[R("S32")], writes=[R("Sb")])
                o3 = psum[4][:].rearrange("p (h n) -> p h n", h=4)
                s.op("act", lambda e: e.activation(out=osq[:], in_=o3, func=AF.Square), reads=[psR[4]], writes=[R("osq")])
                s.op("dve", lambda e: e.tensor_reduce(out=ssum[:], in_=osq[:], axis=AX.X, op=ALU.add), reads=[R("osq")], writes=[R("ssum")])
                s.op("act", lambda e: e.activation(out=ssum[:], in_=ssum[:], func=AF.Sqrt, scale=1.0 / 128, bias=cst[:, 1:2]), reads=[R("ssum"), R("cst")], writes=[R("ssum")])
                s.op("dve", lambda e: e.reciprocal(out=ssum[:], in_=ssum[:]), reads=[R("ssum")], writes=[R("ssum")])
                s.op("dve", lambda e: e.tensor_tensor(out=on[:], in0=o3, in1=ssum[:].unsqueeze(2).to_broadcast([128, 4, 128]), op=ALU.mult),
                     reads=[psR[4], R("ssum")], writes=[R("on")])
                s.op("dve", lambda e: e.tensor_tensor(out=on[:], in0=on[:], in1=gout[:].unsqueeze(1).to_broadcast([128, 4, 128]), op=ALU.mult),
                     reads=[R("on"), R("gout")], writes=[R("on")])
                s.op("act", lambda e: e.activation(out=sgg[:], in_=ggt[:, cc, :], func=AF.Silu), reads=[R("gggt")], writes=[R("sgg")])
                s.op("dve", lambda e: e.tensor_tensor(out=yc[:], in0=on[:].rearrange("p h n -> p (h n)"), in1=sgg[:], op=ALU.mult),
                     reads=[R("on"), R("sgg")], writes=[R("yc")])
                def tr_fn(e):
                    ins = None
                    for h in range(4):
                        ins = e.transpose(psb[:, 6 * 1024 + h * 128:6 * 1024 + (h + 1) * 128], yc[:, h * 128:(h + 1) * 128], identb[:])
                    return ins
                s.op("pe", tr_fn, reads=[R("yc"), R("identb")], writes=[psR[6]])
                s.op("act", lambda e: e.activation(out=stg[:, :, ct], in_=psb[:, 6 * 1024:6 * 1024 + 512].rearrange("p (h n) -> p h n", h=4), func=AF.Copy),
                     reads=[psR[6]], writes=[R("gstg")])
            s1(0)
            for cc in range(4):
                if cc + 1 < 4:
                    s1(cc + 1)
                s2(cc)
            s.dma(g["mixT"].ap()[1536:2048, tok].rearrange("(h p) n -> p h n", p=128), stg[:], reads=[R("gstg")], writes=[R("mixT", gi)], q="act")


def nsa_mix(P, s, g, l):
    nc = P.nc
    T = P.T
    NQ = T // 128
    NCB = (T - 32) // 16 + 1
    SCALE = 128 ** -0.5
    psum, psb, psall, psR, ones32, cst, c128, identb = (g["psum"], g["psb"], g["psall"], g["psR"], g["ones32"], g["cst"],
                                                        g["c128"], g["identb"])
    w16 = g["w16"]
    R = P.R
    with ExitStack() as ms:
        def sbuf(name, shape, dt, st=ms):
            return st.enter_context(nc.sbuf_tensor("nsa_%s_%d" % (name, l), list(shape), dt))
        ksT = sbuf("ksT", [128, 2, T], BF16)
        kwT = sbuf("kwT", [128, 2, T], BF16)
        vsa = sbuf("vsa", [128, NQ, 2, 129], BF16)
        vwa = sbuf("vwa", [128, NQ, 2, 129], BF16)
        cmask = sbuf("cmask", [128, 2, T], BF16)
        Efull = sbuf("Efull", [64, T], BF16)
        cs_sb = sbuf("cs", [128, NQ, 32], F32)
        fmvm = sbuf("fmvm", [128, NQ, 128], F32)
        gates = sbuf("gates", [128, NQ, 24], F32)
        outn = sbuf("outn", [128, 1024], F32)
        nrm_bc = sbuf("nrm_bc", [128, 4, 128], F32)
        kcg = sbuf("kcg", [128, 1], F32)
        gb_bc = sbuf("gb_bc", [128, 24], F32)
        kcn = sbuf("kcn", [128, 2, 256], BF16)
        vca = sbuf("vca", [128, 2, 2, 193], BF16)
        negc4 = sbuf("negc4", [128, 4, 128], BF16)
        negu4 = sbuf("negu4", [128, 4, 128], BF16)
        w2 = sbuf("w2", [128, 2, 128], BF16)
        pe_bf = sbuf("pe_bf", [128, 2, 32], BF16)

        s.dma(cmask[:], g["cmask_bf"].ap().rearrange("(c p) t -> p c t", p=128), reads=[R("cmask_bf")], writes=[R("cmask")])
        s.dma(Efull[:], g["Efull_bf"].ap(), reads=[R("Efull_bf")], writes=[R("Efull")])
        s.dma(cs_sb[:], g["ropecs"].ap().rearrange("(i p) c -> p i c", p=128), writes=[R("cs")])
        s.dma(fmvm[:], g["fmvm"].ap().rearrange("(i p) c -> p i c", p=128), writes=[R("fmvm")])
        s.dma(outn[:], g["nsaon"].ap()[l:l + 1, :].to_broadcast([128, 1024]), writes=[R("outn")])
        for k in range(4):
            s.dma(nrm_bc[:, k, :], g["nsan"].ap()[l, k:k + 1, :].to_broadcast([128, 128]), writes=[R("nrm_bc")])
        s.dma(kcg[:], g["nsan"].ap()[l, 1, :].rearrange("(p o) -> p o", o=1), writes=[R("kcg")])
        s.dma(gb_bc[:], g["nsagb"].ap()[l:l + 1, :].to_broadcast([128, 24]), writes=[R("gb_bc")])
        s.op("dve", lambda e: e.memset(kcn[:], 0.0), writes=[R("kcn")])
        s.op("dve", lambda e: e.memset(vca[:], 0.0), writes=[R("vca")])
        s.op("dve", lambda e: e.memset(vca[:, :, :, 128:129], 1.0), writes=[R("vca")])
        for kv in range(2):
            s.dma(vca[:, :, kv, 129:193], g["ovl_bf"].ap().rearrange("(c p) s -> p c s", p=128), reads=[R("vca"), R("ovl_bf")], writes=[R("vca")])
        s.op("dve", lambda e: e.memset(vsa[:, :, :, 128:129], 1.0), writes=[R("vsa1")])
        s.op("dve", lambda e: e.memset(vwa[:, :, :, 128:129], 1.0), writes=[R("vwa1")])
        s.op("dve", lambda e: e.tensor_copy(out=negc4[:], in_=c128[:, 4, :].unsqueeze(1).to_broadcast([128, 4, 128])), reads=[R("c128")], writes=[R("negc4")])
        s.op("dve", lambda e: e.tensor_copy(out=negu4[:], in_=c128[:, 5, :].unsqueeze(1).to_broadcast([128, 4, 128])), reads=[R("c128")], writes=[R("negu4")])
        s.dma(w2[:], w16["cmpw2"].ap()[l].rearrange("k p o -> p k o"), reads=[R("w", "cmpw2", l, 0), R("w", "cmpw2", l, 1)], writes=[R("w2")])
        s.dma(pe_bf[:], w16["cmppe"].ap()[l].rearrange("k p o -> p k o"), reads=[R("w", "cmppe", l, 0), R("w", "cmppe", l, 1)], writes=[R("pe_bf")])

        with ExitStack() as cs_:
            xT_sb = sbuf("xT", [128, 4, T], BF16, cs_)
            w1 = sbuf("w1", [128, 2, 4096], BF16, cs_)
            hid = sbuf("hid", [128, 4, 256], BF16, cs_)
            hx = sbuf("hx", [128, 256], F32, cs_)
            htmp = sbuf("htmp", [128, 256], F32, cs_)
            hg = sbuf("hg", [128, 256], F32, cs_)
            cvec = sbuf("cvec", [128, 2], F32, cs_)
            ksq = sbuf("ksq", [128, 256], F32, cs_)
            krs = sbuf("krs", [128, 256], F32, cs_)
            s.dma(xT_sb[:], g["kvcT"].ap().rearrange("(a p) t -> p a t", p=128), reads=[R("kvcT", t) for t in range(T // NTOK)], writes=[R("nxT")])
            s.dma(w1[:], w16["cmpw1"].ap()[l].rearrange("k p o -> p k o"), reads=[R("w", "cmpw1", l, 0), R("w", "cmpw1", l, 1)], writes=[R("nw1")])
            s.op("dve", lambda e: e.memset(hid[:], 0.0), writes=[R("nhid")])
            for kvi in range(2):
                mm_group(s, psum[7][:, 0:1], [(w1[:, kvi, ll * 128:(ll + 1) * 128], pe_bf[:, kvi, ll:ll + 1]) for ll in range(32)],
                         reads=[R("nw1"), R("pe_bf")], writes=[psR[7]])
                s.op("dve", lambda e: e.tensor_copy(out=cvec[:, kvi:kvi + 1], in_=psum[7][:, 0:1]), reads=[psR[7]], writes=[R("ncvec")])
                for hd in range(2):
                    a = kvi * 2 + hd
                    mm_group(s, psum[0][:, 0:NCB], [(w1[:, kvi, ll * 128:(ll + 1) * 128], xT_sb[:, a, ll:ll + 16 * (NCB - 1) + 1:16]) for ll in range(32)],
                             reads=[R("nw1"), R("nxT")], writes=[psR[0]])
                    s.op("dve", lambda e: e.tensor_scalar(out=hx[:, 0:NCB], in0=psum[0][:, 0:NCB], scalar1=cvec[:, kvi:kvi + 1], scalar2=None, op0=ALU.add),
                         reads=[psR[0], R("ncvec")], writes=[R("nhx")])
                    gelu2(s, hg[:, 0:NCB], hx[:, 0:NCB], htmp[:, 0:NCB], R("nhx"), R("nhtmp"), R("nhg"))
                    s.op("act", lambda e: e.activation(out=hid[:, a, 0:NCB], in_=hg[:, 0:NCB], func=AF.Copy, scale=0.5), reads=[R("nhg")], writes=[R("nhid")])
                    if kvi == 0:
                        s.op("pe", lambda e: e.matmul(psum[1][:, 0:NCB], w2[:, 0, :], hid[:, a, 0:NCB], start=True, stop=True), reads=[R("w2"), R("nhid")], writes=[psR[1]])
                        s.op("act", lambda e: e.activation(out=ksq[:, 0:NCB], in_=psum[1][:, 0:NCB], func=AF.Square), reads=[psR[1]], writes=[R("nksq")])
                        s.op("pe", lambda e: e.matmul(psum[2][:, 0:NCB], ones32[:], ksq[:, 0:NCB], start=True, stop=True), reads=[R("nksq"), R("ones32")], writes=[psR[2]])
                        s.op("act", lambda e: e.activation(out=krs[:, 0:NCB], in_=psum[2][:, 0:NCB], func=AF.Sqrt, scale=1.0 / 128, bias=cst[:, 1:2]),
                             reads=[psR[2], R("cst")], writes=[R("nkrs")])
                        s.op("dve", lambda e: e.reciprocal(out=krs[:, 0:NCB], in_=krs[:, 0:NCB]), reads=[R("nkrs")], writes=[R("nkrs")])
                        s.op("dve", lambda e: e.scalar_tensor_tensor(out=kcn[:, hd, 0:NCB], in0=psum[1][:, 0:NCB], scalar=kcg[:, 0:1], in1=krs[:, 0:NCB],
                                                                      op0=ALU.mult, op1=ALU.mult), reads=[psR[1], R("kcg"), R("nkrs")], writes=[R("kcn")])
                    else:
                        for c in range((NCB + 127) // 128):
                            s.op("pe", lambda e: e.matmul(psum[3][:, 0:128], hid[:, a, c * 128:(c + 1) * 128], w2[:, 1, :], start=True, stop=True),
                                 reads=[R("w2"), R("nhid")], writes=[psR[3]])
                            s.op("act", lambda e: e.activation(out=vca[:, c, hd, 0:128], in_=psum[3][:, 0:128], func=AF.Copy), reads=[psR[3]], writes=[R("vca")])
            s.barrier()

        q_in = sbuf("q_in", [128, 8, 128], F32)
        ksw_in = sbuf("ksw_in", [128, 8, 128], F32)
        gl_in = sbuf("gl_in", [128, 24], F32)
        sqb = sbuf("sqb", [128, 12, 128], F32)
        xn = sbuf("xn", [128, 12, 128], F32)
        ss = sbuf("ss", [128, 12], F32)
        rt = sbuf("rt", [128, 4, 12, 16], F32)
        qc_bf = sbuf("qc_bf", [128, 8, 128], BF16)
        r12 = sbuf("r12", [128, 12, 128], BF16)
        gain12 = sbuf("gain12", [128, 12, 128], F32)
        qcT = sbuf("qcT", [128, 8, 128], BF16)
        qrT = sbuf("qrT", [128, 8, 128], BF16)
        pT = sbuf("pT", [128, 3, 512], BF16)
        yb = sbuf("yb", [128, 8, 128], F32)
        tmpo = sbuf("tmpo", [128, 4, 128], F32)
        tmpu = sbuf("tmpu", [128, 4, 64], F32)
        imp = sbuf("imp", [128, 64], F32)
        imp2 = sbuf("imp2", [128, 64], F32)
        m8 = sbuf("m8", [128, 2, 8], F32)
        negsel = sbuf("negsel", [128, 2, 64], F32)
        sqy = sbuf("sqy", [128, 8, 128], F32)
        nsT4 = sbuf("nsT4", [64, 2, 4, 128], BF16)
        rden = sbuf("rden", [128, 3, 4], F32)
        coef = sbuf("coef", [128, 3, 4], F32)
        ss1 = sbuf("ss1", [128, 2], F32)
        yn = sbuf("yn", [128, 8, 128], BF16)
        stg = sbuf("stg", [128, 8, 512], BF16)
        pTrot = Rot([(k, R("npT", k)) for k in range(3)])
        SC = Rot([(0, psR[0]), (1, psR[1])])
        OA = Rot([(2, [psR[2], psR[3]]), (4, [psR[4], psR[5]])])

        def oview(b):
            return psall[:, b * 512:(b + 2) * 512].rearrange("p (g c) -> p g c", g=4)

        def transposes(bank, srcs, rsrc):
            def fn(e):
                ins = None
                for k, a in enumerate(srcs):
                    ins = e.transpose(psb[:, bank * 1024 + k * 128:bank * 1024 + (k + 1) * 128], a, identb[:])
                return ins
            s.op("pe", fn, reads=rsrc + [R("identb")], writes=[psR[bank]])

        def branch(i, kv, kind):
            if kind == "sel":
                js = list(range(0, i + 1))
                kT_, va, bi = ksT, vsa, 1
            else:
                js = list(range(max(0, i - 4), i + 1))
                kT_, va, bi = kwT, vwa, 2
            ob, oR = OA.next()
            ov = oview(ob)
            q4 = qrT[:, kv * 4:(kv + 1) * 4, :].rearrange("p g n -> p (g n)")
            def score(j):
                si, sR = SC.next()
                pairs = [(kT_[:, kv, j * 128:(j + 1) * 128], q4)]
                rd = [R("nkT", kind, j), R("nqrT")]
                if kind == "sel":
                    pairs.append((Efull[:, j * 128:(j + 1) * 128], nsT4[:, kv, :, :].rearrange("p g n -> p (g n)")))
                    rd += [R("Efull"), R("nnsT4", kv)]
                if j == i:
                    pairs.append((identb[:], negc4[:].rearrange("p g n -> p (g n)")))
                    rd += [R("identb"), R("negc4")]
                if kind == "win" and j == i - 4:
                    pairs.append((identb[:], negu4[:].rearrange("p g n -> p (g n)")))
                    rd += [R("identb"), R("negu4")]
                mm_group(s, psum[si][:], pairs, reads=rd, writes=[sR])
                return si, sR

            def finish(j, si, sR):
                pi, pR = pTrot.next()
                s.op("act", lambda e: e.activation(out=pT[:, pi, :], in_=psum[si][:], func=AF.Exp, scale=SCALE), reads=[sR], writes=[pR])

                def pv(e):
                    ins = None
                    for gq in range(4):
                        ins = e.matmul(ov[:, gq, 0:129], pT[:, pi, gq * 128:(gq + 1) * 128], va[:, j, kv, :], start=(j == js[0] and gq % 2 == 0), stop=(j == js[-1] and gq % 2 == 1))
                    return ins
                s.op("pe", pv, reads=[pR, R("nva", kind, j), R("vsa1" if kind == "sel" else "vwa1")], writes=oR)

            nxt = score(js[0])
            for idx, j in enumerate(js):
                cur = nxt
                if idx + 1 < len(js):
                    nxt = score(js[idx + 1])
                finish(j, *cur)
            s.op("dve", lambda e: e.reciprocal(out=rden[:, bi, :], in_=ov[:, :, 128]), reads=oR, writes=[R("nrden", bi)])
            s.op("dve", lambda e: e.tensor_tensor(out=coef[:, bi, :], in0=gates[:, i, kv * 12 + bi:kv * 12 + 12:3], in1=rden[:, bi, :], op=ALU.mult),
                 reads=[R("ngates", i), R("nrden", bi)], writes=[R("ncoef", bi)])
            s.op("dve", lambda e: e.tensor_tensor(out=tmpo[:], in0=ov[:, :, 0:128], in1=coef[:, bi, :].unsqueeze(2).to_broadcast([128, 4, 128]), op=ALU.mult),
                 reads=oR + [R("ncoef", bi)], writes=[R("ntmpo")])
            s.op("dve", lambda e: e.tensor_tensor(out=yb[:, kv * 4:(kv + 1) * 4, :], in0=yb[:, kv * 4:(kv + 1) * 4, :], in1=tmpo[:], op=ALU.add),
                 reads=[R("ntmpo"), R("nyb")], writes=[R("nyb")])

        for (h0, H, k) in ((0, 8, 0), (8, 2, 2), (10, 2, 3)):
            s.op("dve", lambda e: e.tensor_copy(out=gain12[:, h0:h0 + H, :], in_=nrm_bc[:, k, :].unsqueeze(1).to_broadcast([128, H, 128])),
                 reads=[R("nrm_bc")], writes=[R("ngain12")])
        srcs = [(q_in[:], 0, 8, "nq_in"), (ksw_in[:, 0:2, :], 8, 2, "nksw_in"), (ksw_in[:, 4:6, :], 10, 2, "nksw_in")]

        def prep_p1(i):
            rows = slice(i * 128, (i + 1) * 128)
            tt_i = i // 4
            s.dma(q_in[:].rearrange("p h d -> p (h d)"), g["q_tm"].ap()[rows, :], reads=[R("q_tm", tt_i)], writes=[R("nq_in")])
            s.dma(ksw_in[:].rearrange("p h d -> p (h d)"), g["ksw_tm"].ap()[rows, :], reads=[R("ksw_tm", tt_i)], writes=[R("nksw_in")])
            s.dma(gl_in[:], g["gkg_tm"].ap()[rows, 256:280], reads=[R("gkg_tm", tt_i)], writes=[R("ngl_in")])
            s.op("dve", lambda e: e.tensor_tensor(out=gl_in[:], in0=gl_in[:], in1=gb_bc[:], op=ALU.add), reads=[R("ngl_in"), R("gb_bc")], writes=[R("ngl_in")])
            for (src, h0, H, rk) in srcs:
                s.op("dve", lambda e: e.tensor_tensor(out=sqb[:, h0:h0 + H, :], in0=src, in1=src, op=ALU.mult), reads=[R(rk)], writes=[R("nsqb")])
            s.op("dve", lambda e: e.tensor_reduce(out=ss[:], in_=sqb[:], axis=AX.X, op=ALU.add), reads=[R("nsqb")], writes=[R("nss")])

        def prep_p2(i):
            s.op("act", lambda e: e.activation(out=gates[:, i, :], in_=gl_in[:], func=AF.Sigmoid), reads=[R("ngl_in")], writes=[R("ngates", i)])
            s.op("act", lambda e: e.activation(out=ss[:], in_=ss[:], func=AF.Sqrt, scale=1.0 / 128, bias=cst[:, 1:2]), reads=[R("nss"), R("cst")], writes=[R("nss")])

        def prep_p3(i):
            s.op("dve", lambda e: e.reciprocal(out=ss[:], in_=ss[:]), reads=[R("nss")], writes=[R("nss")])
            for (src, h0, H, rk) in srcs:
                s.op("dve", lambda e: e.tensor_tensor(out=xn[:, h0:h0 + H, :], in0=src, in1=ss[:, h0:h0 + H].unsqueeze(2).to_broadcast([128, H, 128]), op=ALU.mult),
                     reads=[R(rk), R("nss")], writes=[R("nxn")])
            s.op("dve", lambda e: e.tensor_tensor(out=xn[:], in0=xn[:], in1=gain12[:], op=ALU.mult), reads=[R("nxn"), R("ngain12")], writes=[R("nxn")])
            cosb = cs_sb[:, i, 0:16].unsqueeze(1).to_broadcast([128, 12, 16])
            sinb = cs_sb[:, i, 16:32].unsqueeze(1).to_broadcast([128, 12, 16])
            x1 = xn[:, :, 0:16]
            x2 = xn[:, :, 16:32]
            for k, (a, b) in enumerate(((x1, cosb), (x2, sinb), (x2, cosb), (x1, sinb))):
                s.op("dve", lambda e: e.tensor_tensor(out=rt[:, k, :, :], in0=a, in1=b, op=ALU.mult), reads=[R("nxn"), R("cs")], writes=[R("nrt")])
            s.op("dve", lambda e: e.tensor_tensor(out=r12[:, :, 0:16], in0=rt[:, 0, :, :], in1=rt[:, 1, :, :], op=ALU.subtract), reads=[R("nrt")], writes=[R("nr12")])
            s.op("dve", lambda e: e.tensor_tensor(out=r12[:, :, 16:32], in0=rt[:, 2, :, :], in1=rt[:, 3, :, :], op=ALU.add), reads=[R("nrt")], writes=[R("nr12")])

        def prep_p4(i):
            s.op("act", lambda e: e.activation(out=qc_bf[:], in_=xn[:, 0:8, :], func=AF.Copy), reads=[R("nxn")], writes=[R("nqbf")])
            s.op("act", lambda e: e.activation(out=r12[:, :, 32:128], in_=xn[:, :, 32:128], func=AF.Copy), reads=[R("nxn")], writes=[R("nr12")])
            s.op("act", lambda e: e.activation(out=vsa[:, i, :, 0:128], in_=ksw_in[:, 2:4, :], func=AF.Copy), reads=[R("nksw_in")], writes=[R("nva", "sel", i)])
            s.op("act", lambda e: e.activation(out=vwa[:, i, :, 0:128], in_=ksw_in[:, 6:8, :], func=AF.Copy), reads=[R("nksw_in")], writes=[R("nva", "win", i)])

        def prep_pe(i):
            rows = slice(i * 128, (i + 1) * 128)
            transposes(6, [qc_bf[:, h, :] for h in range(8)], [R("nqbf")])
            s.op("act", lambda e: e.activation(out=qcT[:].rearrange("p h n -> p (h n)"), in_=psb[:, 6 * 1024:7 * 1024], func=AF.Copy), reads=[psR[6]], writes=[R("nqcT")])
            transposes(7, [r12[:, h, :] for h in range(8)], [R("nr12")])
            s.op("dve", lambda e: e.tensor_copy(out=qrT[:].rearrange("p h n -> p (h n)"), in_=psb[:, 7 * 1024:8 * 1024]), reads=[psR[7]], writes=[R("nqrT")])
            transposes(6, [r12[:, 8 + h, :] for h in range(4)], [R("nr12")])
            s.op("act", lambda e: e.activation(out=ksT[:, :, rows], in_=psb[:, 6 * 1024:6 * 1024 + 256].rearrange("p (h n) -> p h n", h=2), func=AF.Copy),
                 reads=[psR[6]], writes=[R("nkT", "sel", i)])
            s.op("act", lambda e: e.activation(out=kwT[:, :, rows], in_=psb[:, 6 * 1024 + 256:6 * 1024 + 512].rearrange("p (h n) -> p h n", h=2), func=AF.Copy),
                 reads=[psR[6]], writes=[R("nkT", "win", i)])

        def cmp_topk(i, kv):
            rows = slice(i * 128, (i + 1) * 128)
            n_max = min(NCB - 1, 8 * i + 6)
            ncc = n_max // 128 + 1
            ob, oR = OA.next()
            ov = oview(ob)
            q4c = qcT[:, kv * 4:(kv + 1) * 4, :].rearrange("p g n -> p (g n)")
            for c in range(ncc):
                si, sR = SC.next()
                s.op("pe", lambda e: e.matmul(psum[si][:], kcn[:, kv, c * 128:(c + 1) * 128], q4c, start=True, stop=True), reads=[R("kcn"), R("nqcT")], writes=[sR])
                pi, pR = pTrot.next()
                s.op("act", lambda e: e.activation(out=pT[:, pi, :], in_=psum[si][:], func=AF.Exp, scale=SCALE), reads=[sR], writes=[pR])
                s.op("dve", lambda e: e.tensor_tensor(out=pT[:, pi, :].rearrange("p (g n) -> p g n", g=4), in0=pT[:, pi, :].rearrange("p (g n) -> p g n", g=4),
                                                      in1=cmask[:, c, rows].unsqueeze(1).to_broadcast([128, 4, 128]), op=ALU.mult),
                     reads=[pR, R("cmask")], writes=[pR])

                def pvc(e):
                    ins = None
                    for gq in range(4):
                        ins = e.matmul(ov[:, gq, 0:193], pT[:, pi, gq * 128:(gq + 1) * 128], vca[:, c, kv, :], start=(c == 0 and gq % 2 == 0), stop=(c == ncc - 1 and gq % 2 == 1))
                    return ins
                s.op("pe", pvc, reads=[pR, R("vca")], writes=oR)
            s.op("dve", lambda e: e.tensor_scalar(out=rden[:, 0, :], in0=ov[:, :, 128], scalar1=1e-30, scalar2=None, op0=ALU.max), reads=oR, writes=[R("nrden", 0)])
            s.op("dve", lambda e: e.reciprocal(out=rden[:, 0, :], in_=rden[:, 0, :]), reads=[R("nrden", 0)], writes=[R("nrden", 0)])
            s.op("dve", lambda e: e.tensor_tensor(out=tmpu[:], in0=ov[:, :, 129:193], in1=rden[:, 0, :].unsqueeze(2).to_broadcast([128, 4, 64]), op=ALU.mult),
                 reads=oR + [R("nrden", 0)], writes=[R("ntmpu")])
            s.op("dve", lambda e: e.tensor_reduce(out=imp[:], in_=tmpu[:].rearrange("p g s -> p s g"), axis=AX.X, op=ALU.add), reads=[R("ntmpu")], writes=[R("nimp")])
            s.op("dve", lambda e: e.tensor_tensor(out=coef[:, 0, :], in0=gates[:, i, kv * 12:kv * 12 + 12:3], in1=rden[:, 0, :], op=ALU.mult),
                 reads=[R("ngates", i), R("nrden", 0)], writes=[R("ncoef", 0)])
            s.op("dve", lambda e: e.tensor_tensor(out=yb[:, kv * 4:(kv + 1) * 4, :], in0=ov[:, :, 0:128], in1=coef[:, 0, :].unsqueeze(2).to_broadcast([128, 4, 128]), op=ALU.mult),
                 reads=oR + [R("ncoef", 0)], writes=[R("nyb")])
            s.op("dve", lambda e: e.tensor_tensor(out=imp[:], in0=imp[:], in1=fmvm[:, i, 64:128], op=ALU.mult), reads=[R("nimp"), R("fmvm")], writes=[R("nimp")])
            s.op("dve", lambda e: e.tensor_tensor(out=imp[:], in0=imp[:], in1=fmvm[:, i, 0:64], op=ALU.add), reads=[R("nimp"), R("fmvm")], writes=[R("nimp")])
            s.op("dve", lambda e: e.max(out=m8[:, 0, :], in_=imp[:]), reads=[R("nimp")], writes=[R("nm8")])
            s.op("dve", lambda e: e.match_replace(out=imp2[:], in_to_replace=m8[:, 0, :], in_values=imp[:], imm_value=-3.0e38), reads=[R("nimp"), R("nm8")], writes=[R("nimp2")])
            s.op("dve", lambda e: e.max(out=m8[:, 1, :], in_=imp2[:]), reads=[R("nimp2")], writes=[R("nm8")])
            s.op("dve", lambda e: e.tensor_scalar(out=negsel[:, kv, :], in0=imp[:], scalar1=m8[:, 1, 7:8], scalar2=1.0, op0=ALU.is_ge, op1=ALU.subtract),
                 reads=[R("nimp"), R("nm8")], writes=[R("nnegsel", kv)])

        def sel_mask_T(kv):
            s.op("pe", lambda e: e.transpose(psum[7][0:64, 0:128], negsel[:, kv, :], c128[:, 0, :]), reads=[R("nnegsel", kv), R("c128")], writes=[psR[7]])
            s.op("act", lambda e: e.activation(out=nsT4[:, kv, :, :], in_=psum[7][0:64, 0:128].unsqueeze(1).to_broadcast([64, 4, 128]), func=AF.Copy),
                 reads=[psR[7]], writes=[R("nnsT4", kv)])

        def out_tile(i):
            tt_i = i // 4
            s.op("dve", lambda e: e.tensor_tensor(out=sqy[:], in0=yb[:], in1=yb[:], op=ALU.mult), reads=[R("nyb")], writes=[R("nsqy")])
            s.op("dve", lambda e: e.tensor_reduce(out=ss1[:, 0:1], in_=sqy[:].rearrange("p h d -> p (h d)"), axis=AX.X, op=ALU.add), reads=[R("nsqy")], writes=[R("nss1")])
            s.op("act", lambda e: e.activation(out=ss1[:, 0:1], in_=ss1[:, 0:1], func=AF.Sqrt, scale=1.0 / 1024, bias=cst[:, 1:2]), reads=[R("nss1"), R("cst")], writes=[R("nss1")])
            s.op("dve", lambda e: e.reciprocal(out=ss1[:, 0:1], in_=ss1[:, 0:1]), reads=[R("nss1")], writes=[R("nss1")])
            s.op("dve", lambda e: e.scalar_tensor_tensor(out=yn[:].rearrange("p h d -> p (h d)"), in0=yb[:].rearrange("p h d -> p (h d)"), scalar=ss1[:, 0:1], in1=outn[:],
                                                          op0=ALU.mult, op1=ALU.mult), reads=[R("nyb"), R("nss1"), R("outn")], writes=[R("nyn")])
            transposes(6, [yn[:, h, :] for h in range(8)], [R("nyn")])
            ci = i % 4
            s.op("act", lambda e: e.activation(out=stg[:, :, ci * 128:(ci + 1) * 128], in_=psb[:, 6 * 1024:7 * 1024].rearrange("p (h n) -> p h n", h=8), func=AF.Copy),
                 reads=[psR[6]], writes=[R("nstg")])
            if ci == 3:
                tok = slice(tt_i * 512, (tt_i + 1) * 512)
                s.dma(g["mixT"].ap()[512:1536, tok].rearrange("(h p) n -> p h n", p=128), stg[:], reads=[R("nstg")], writes=[R("mixT", tt_i)], q="act")

        jobs = g["cast_jobs"](l + 1) if l + 1 < P.L else []
        c_start = NQ // 2
        per_tile = (len(jobs) + (NQ - c_start) - 1) // (NQ - c_start) if jobs else 0
        prep_p1(0)
        prep_p2(0)
        prep_p3(0)
        prep_p4(0)
        prep_pe(0)
        for i in range(NQ):
            if i >= c_start:
                for job in jobs[(i - c_start) * per_tile:(i - c_start + 1) * per_tile]:
                    g["cast_job"](job)
            more = i + 1 < NQ
            for kv in range(2):
                cmp_topk(i, kv)
            if more:
                prep_p1(i + 1)
            branch(i, 0, "win")
            sel_mask_T(0)
            branch(i, 0, "sel")
            if more:
                prep_p2(i + 1)
                prep_p3(i + 1)
            branch(i, 1, "win")
            sel_mask_T(1)
            branch(i, 1, "sel")
            if more:
                prep_p4(i + 1)
            out_tile(i)
            if more:
                prep_pe(i + 1)


def tt_phase(P, s, g, body, sfx):
    nc = P.nc
    T, L = P.T, P.L
    NT = T // NTOK
    psum, psR = g["psum"], g["psR"]
    w16 = g["w16"]
    ones32, gains_sb = g["ones32"], g["gains_sb"]

    with ExitStack() as ts:
        def sb(name, shape, dt):
            return ts.enter_context(nc.sbuf_tensor(name + sfx, list(shape), dt))

        x_sb = sb("x_sb", [128, KC, NTOK], F32)
        h_sb = sb("h_sb", [128, KC, NTOK], BF16)
        act_sb = sb("act_sb", [128, NFF, NTOK], BF16)
        wbuf = sb("wbuf", [128, 4, 8192], BF16)
        sq_sb = sb("sq_sb", [128, 2, NTOK], F32)
        rstd_sb = sb("rstd_sb", [128, NTOK], F32)
        sg_sb = sb("sg_sb", [128, 2, NTOK], F32)
        stg_sb = sb("stg_sb", [128, 3, NTOK], F32)
        stgb_sb = sb("stgb_sb", [128, 3, NTOK], BF16)
        xR = [P.R("x_sb", i) for i in range(KC)]
        hR = P.R("h_sb")
        actR = [P.R("act_sb", i) for i in range(NFF)]
        wrot = Rot([(i, P.R("wbuf", i)) for i in range(4)])
        sqrot = Rot([(i, P.R("sq", i)) for i in range(2)])
        sgrot = Rot([(i, P.R("sg", i)) for i in range(2)])
        stgrot = Rot([(i, P.R("stg", i)) for i in range(3)])
        stgbrot = Rot([(i, P.R("stgb", i)) for i in range(3)])
        rstdR = P.R("rstd")
        PG = Rot([(0, psR[0]), (1, psR[1])])
        PU = Rot([(2, psR[2]), (3, psR[3])])
        PO = Rot([(4, psR[4]), (5, psR[5])])
        PSTAT = (6, psR[6])

        def load_w(nm, l, pc, nel):
            bi, bR = wrot.next()
            s.dma(wbuf[:, bi, 0:nel], w16[nm].ap()[l, pc], reads=[P.R("w", nm, l, pc)], writes=[bR])
            return bi, bR

        def norm(l, which):
            pi, pR = PSTAT
            for kc in range(KC):
                qi, qR = sqrot.next()
                s.op("act", lambda e: e.activation(out=sq_sb[:, qi, :], in_=x_sb[:, kc, :], func=AF.Square),
                     reads=[xR[kc]], writes=[qR])
                s.op("pe", lambda e: e.matmul(psum[pi][:], ones32[:], sq_sb[:, qi, :], start=(kc == 0), stop=(kc == KC - 1)),
                     reads=[qR, P.R("ones32")], writes=[pR])
            s.op("act", lambda e: e.activation(out=rstd_sb[:], in_=psum[pi][:], func=AF.Sqrt, scale=1.0 / D, bias=eps_sb[:, 0:1]),
                 reads=[pR, P.R("eps")], writes=[rstdR])
            s.op("dve", lambda e: e.reciprocal(out=rstd_sb[:], in_=rstd_sb[:]), reads=[rstdR], writes=[rstdR])
            gbase = (l * 3 + which) * KC
            for kc in range(KC):
                s.op("dve", lambda e: e.scalar_tensor_tensor(out=h_sb[:, kc, :], in0=x_sb[:, kc, :],
                                                              scalar=gains_sb[:, gbase + kc:gbase + kc + 1], in1=rstd_sb[:],
                                                              op0=ALU.mult, op1=ALU.mult),
                     reads=[xR[kc], rstdR, P.R("gains")], writes=[hR])

        eps_sb = sb("eps_sb", [128, 1], F32)
        s.op("dve", lambda e: e.memset(eps_sb[:], EPS), writes=[P.R("eps")])

        def ffn(l, which):
            nm_gu = "wgu%d" % which
            nm_d = "wd%d" % which
            norm(l, 0 if which == 1 else 2)
            for f in range(NFF):
                bi, bR = load_w(nm_gu, l, f, 2 * KC * 128)
                gi, gR = PG.next()
                ui, uR = PU.next()
                mm_group(s, psum[gi][:], [(wbuf[:, bi, kc * 128:(kc + 1) * 128], h_sb[:, kc, :]) for kc in range(KC)],
                         reads=[bR, hR], writes=[gR])
                mm_group(s, psum[ui][:], [(wbuf[:, bi, (KC + kc) * 128:(KC + kc + 1) * 128], h_sb[:, kc, :]) for kc in range(KC)],
                         reads=[bR, hR], writes=[uR])
                si, sR = sgrot.next()
                s.op("act", lambda e: e.activation(out=sg_sb[:, si, :], in_=psum[gi][:], func=AF.Silu), reads=[gR], writes=[sR])
                s.op("dve", lambda e: e.tensor_tensor(out=act_sb[:, f, :], in0=sg_sb[:, si, :], in1=psum[ui][:], op=ALU.mult),
                     reads=[sR, uR], writes=[actR[f]])
            for m in range(KC):
                bi, bR = load_w(nm_d, l, m, NFF * 128)
                oi, oR = PO.next()
                mm_group(s, psum[oi][:], [(wbuf[:, bi, fc * 128:(fc + 1) * 128], act_sb[:, fc, :]) for fc in range(NFF)],
                         reads=[bR] + actR, writes=[oR])
                s.op("dve", lambda e: e.scalar_tensor_tensor(out=x_sb[:, m, :], in0=psum[oi][:], scalar=0.5, in1=x_sb[:, m, :],
                                                              op0=ALU.mult, op1=ALU.add),
                     reads=[oR, xR[m]], writes=[xR[m]])

        def wout(l, t):
            tok = slice(t * NTOK, (t + 1) * NTOK)
            s.dma(h_sb[:], g["mixT"].ap()[:, tok].rearrange("(kc p) n -> p kc n", p=128),
                  reads=[P.R("mixT", t)], writes=[hR])
            for m in range(KC):
                bi, bR = load_w("wout", l, m, KC * 128)
                oi, oR = PO.next()
                mm_group(s, psum[oi][:], [(wbuf[:, bi, kc * 128:(kc + 1) * 128], h_sb[:, kc, :]) for kc in range(KC)],
                         reads=[bR, hR], writes=[oR])
                s.op("dve", lambda e: e.tensor_tensor(out=x_sb[:, m, :], in0=psum[oi][:], in1=x_sb[:, m, :], op=ALU.add),
                     reads=[oR, xR[m]], writes=[xR[m]])

        cm_dst = ([("lruT", 128 * i, 128, F32) for i in range(8)] + [("kvcT", 128 * i, 128, BF16) for i in range(4)]
                  + [("gqkT", 128 * i, 128, F32) for i in range(4)] + [("gaT", 0, 16, BF16)])
        tm_dst = [("q_tm", 0, F32), ("q_tm", 512, F32), ("ksw_tm", 0, F32), ("ksw_tm", 512, F32), ("gkg_tm", 0, F32),
                  ("gv_tm", 0, BF16), ("gg_tm", 0, F32)]

        def proj(l, t):
            tok = slice(t * NTOK, (t + 1) * NTOK)
            norm(l, 1)
            pieces = [("wincm", c, KC * 128) for c in range(WIN_CM)] + [("wintm", pc, KC * 512) for pc in range(WIN_TM)]
            loaded = {}

            def ensure(k):
                if k < len(pieces) and k not in loaded:
                    loaded[k] = load_w(pieces[k][0], l, pieces[k][1], pieces[k][2])
            ensure(0)
            ensure(1)
            for c in range(WIN_CM):
                ensure(c + 2)
                bi, bR = loaded[c]
                name, r0, nr, dt = cm_dst[c]
                oi, oR = PO.next()
                mm_group(s, psum[oi][0:nr, :], [(wbuf[:, bi, kc * 128:kc * 128 + nr], h_sb[:, kc, :]) for kc in range(KC)],
                         reads=[bR, hR], writes=[oR])
                if dt == F32:
                    gi, gR = stgrot.next()
                    dst_sb = stg_sb[0:nr, gi, :]
                else:
                    gi, gR = stgbrot.next()
                    dst_sb = stgb_sb[0:nr, gi, :]
                s.op("act", lambda e: e.activation(out=dst_sb, in_=psum[oi][0:nr, :], func=AF.Copy), reads=[oR], writes=[gR])
                s.dma(g[name].ap()[r0:r0 + nr, tok], dst_sb, reads=[gR], writes=[P.R(name, t)], q="act")
            for pc in range(WIN_TM):
                ensure(WIN_CM + pc + 2)
                bi, bR = loaded[WIN_CM + pc]
                name, c0, dt = tm_dst[pc]
                for sub in range(NTOK // 128):
                    oi, oR = PO.next()
                    mm_group(s, psum[oi][:], [(h_sb[:, kc, sub * 128:(sub + 1) * 128], wbuf[:, bi, kc * 512:(kc + 1) * 512])
                                               for kc in range(KC)], reads=[bR, hR], writes=[oR])
                    if dt == F32:
                        gi, gR = stgrot.next()
                        dst_sb = stg_sb[:, gi, :]
                    else:
                        gi, gR = stgbrot.next()
                        dst_sb = stgb_sb[:, gi, :]
                    eng = "act" if sub % 2 == 0 else "dve"
                    if eng == "act":
                        s.op("act", lambda e: e.activation(out=dst_sb, in_=psum[oi][:], func=AF.Copy), reads=[oR], writes=[gR])
                    else:
                        s.op("dve", lambda e: e.tensor_copy(out=dst_sb, in_=psum[oi][:]), reads=[oR], writes=[gR])
                    r0 = t * NTOK + sub * 128
                    s.dma(g[name].ap()[r0:r0 + 128, c0:c0 + 512], dst_sb, reads=[gR], writes=[P.R(name, t)], q="act")

        def load_x(src, t, srcname):
            tok = slice(t * NTOK, (t + 1) * NTOK)
            for kc in range(KC):
                s.dma(x_sb[:, kc, :], src.ap()[kc * 128:(kc + 1) * 128, tok], reads=[P.R(srcname, t, kc)], writes=[xR[kc]])

        def store_x(dst, t, dstname):
            tok = slice(t * NTOK, (t + 1) * NTOK)
            for kc in range(KC):
                s.dma(dst.ap()[kc * 128:(kc + 1) * 128, tok], x_sb[:, kc, :], reads=[xR[kc]], writes=[P.R(dstname, t, kc)], q="act")

        body(dict(load_x=load_x, store_x=store_x, ffn=ffn, proj=proj, wout=wout))


CM_COLS = ([list(range(128 * i, 128 * (i + 1))) for i in range(8)]
           + [list(range(2048 + 128 * i, 2048 + 128 * (i + 1))) for i in range(4)]
           + [list(range(3608 + 128 * i, 3608 + 128 * (i + 1))) for i in range(4)]
           + [list(range(5144, 5160)) + [-1] * 112])
TM_COLS = [list(range(1024, 1536)), list(range(1536, 2048)), list(range(2560, 3072)), list(range(3072, 3584)),
           list(range(3864, 4120)) + list(range(3584, 3608)) + [-1] * 232,
           list(range(4120, 4632)), list(range(4632, 5144))]


def _tile_cols(W, cols):
    cols = np.asarray(cols)
    Wz = np.concatenate([W, np.zeros((W.shape[0], 1), W.dtype)], axis=1)
    sel = Wz[:, cols]
    return sel.reshape(KC, 128, len(cols)).transpose(1, 0, 2)


def prep_weights(inp, L):
    out = {}
    g = np.stack([inp["ffn1_norm"][:L], inp["mix_norm"][:L], inp["ffn2_norm"][:L]], axis=1)
    out["gains"] = np.ascontiguousarray(g.reshape(L, 3, KC, 128).transpose(3, 0, 1, 2).reshape(128, L * 3 * KC))
    ffn_w = {1: (inp["ffn1_w_gate"], inp["ffn1_w_up"], inp["ffn1_w_down"]),
             2: (inp["ffn2_w_gate"], inp["ffn2_w_up"], inp["ffn2_w_down"])}
    for which in (1, 2):
        wg = ffn_w[which][0][:L].reshape(L, KC, 128, NFF, 128)
        wu = ffn_w[which][1][:L].reshape(L, KC, 128, NFF, 128)
        gu = np.stack([wg, wu], axis=1)
        out["wgu%d" % which] = np.ascontiguousarray(gu.transpose(0, 4, 3, 1, 2, 5)).reshape(L, NFF, 128, 2 * KC * 128)
        wd = ffn_w[which][2][:L].reshape(L, NFF, 128, KC, 128)
        out["wd%d" % which] = np.ascontiguousarray(wd.transpose(0, 3, 2, 1, 4)).reshape(L, KC, 128, NFF * 128)
    win = inp["w_in"][:L]
    out["wincm"] = np.stack([np.stack([_tile_cols(win[l], c) for c in CM_COLS]) for l in range(L)]).reshape(L, WIN_CM, 128, KC * 128)
    out["wintm"] = np.stack([np.stack([_tile_cols(win[l], c) for c in TM_COLS]) for l in range(L)]).reshape(L, WIN_TM, 128, KC * 512)
    wo = inp["w_out"][:L].reshape(L, KC, 128, KC, 128)
    out["wout"] = np.ascontiguousarray(wo.transpose(0, 3, 2, 1, 4)).reshape(L, KC, 128, KC * 128)
    return {k: np.ascontiguousarray(v, dtype=np.float32) for k, v in out.items()}


def make_consts(T):
    c = np.zeros((128, 6, 128), np.float32)
    i = np.arange(128)
    c[:, 0, :] = np.eye(128)
    c[:, 1, :] = np.where(i[:, None] <= i[None, :], -1.0 / 16.0, 0.0)
    c[:, 2, :] = np.where(i[:, None] > i[None, :], -1.0 / 16.0, 0.0)
    c[:, 3, :] = np.where(i[:, None] <= i[None, :], 1.0, 0.0)
    c[:, 4, :] = np.where(i[:, None] <= i[None, :], 0.0, -30000.0)
    c[:, 5, :] = np.where(i[:, None] > i[None, :], 0.0, -30000.0)
    out = {"c128": c}
    NCB = (T - 32) // 16 + 1
    NS = T // 64
    t = np.arange(T)
    n = np.arange(256)
    out["cmaskT"] = ((n[:, None] < NCB) & (16 * n[:, None] + 31 <= t[None, :])).astype(np.float32)
    sidx = np.arange(64)
    out["Efull"] = np.where((t[None, :] // 64) == sidx[:, None], 30000.0, 0.0).astype(np.float32)
    inv = (np.float32(1.0) / (np.float32(500000.0) ** (np.arange(0, 32, 2, dtype=np.float32) / np.float32(32)))).astype(np.float32)
    ang = (t.astype(np.float32)[:, None] * inv[None, :]).astype(np.float32)
    out["ropecs"] = np.concatenate([np.cos(ang), np.sin(ang)], axis=1).astype(np.float32)
    cur = t // 64
    forced = (sidx[None, :] == 0) | (sidx[None, :] == cur[:, None]) | (sidx[None, :] == cur[:, None] - 1)
    valid = (sidx[None, :] * 64 <= t[:, None]) & (sidx[None, :] < NS)
    forced = forced & valid
    fm = np.where(forced, 1e30, np.where(valid, 0.0, -1e30))
    vm = (valid & ~forced).astype(np.float32)
    out["fmvm"] = np.concatenate([fm, vm], axis=1).astype(np.float32)
    cstart = n * 16
    sstart = sidx * 64
    ov = (n[:, None] < NCB) & (sidx[None, :] < NS) & (cstart[:, None] < sstart[None, :] + 64) & (cstart[:, None] + 32 > sstart[None, :])
    out["ovl"] = ov.astype(np.float32)
    return out


def prep_small(inp, L):
    out = {}
    lv = np.zeros((L, 128, 4, 9), np.float32)

    def cp(v):
        return v.reshape(L, 4, 128).transpose(0, 2, 1)
    for k in range(4):
        lv[..., k] = cp(inp["lru_conv_w"][:L, k])
    lv[..., 4] = cp(inp["lru_conv_b"][:L])
    lv[..., 5] = cp(inp["lru_gate_a_b"][:L])
    lv[..., 6] = cp(inp["lru_gate_x_b"][:L])
    lv[..., 7] = cp(inp["lru_lambda"][:L])
    lv[..., 8] = cp(inp["lru_out_norm"][:L])
    out["lruv"] = lv
    gw = np.zeros((L, 2, 4, 128, 128), np.float32)
    for a, nm in enumerate(("lru_gate_a_w", "lru_gate_x_w")):
        w = inp[nm][:L]
        for c in range(4):
            gw[:, a, c, 0:64, 0:64] = w[:, 2 * c]
            gw[:, a, c, 64:128, 64:128] = w[:, 2 * c + 1]
    out["lrug"] = gw
    out["glaw"] = np.concatenate([inp["gla_a_w2"][:L], inp["gla_a_b"][:L, None, :]], axis=1)
    out["glan"] = inp["gla_out_norm"][:L]
    out["nsan"] = np.stack([inp["nsa_q_norm"][:L], inp["nsa_k_cmp_norm"][:L], inp["nsa_k_sel_norm"][:L], inp["nsa_k_win_norm"][:L]], axis=1)
    out["nsaon"] = inp["nsa_out_norm"][:L]
    out["nsagb"] = inp["nsa_gate_b"][:L]
    out["cmpw1"] = np.stack([inp[k][:L].reshape(L, 32, 128, 128).transpose(0, 2, 1, 3).reshape(L, 128, 4096)
                             for k in ("nsa_cmp_w1_k", "nsa_cmp_w1_v")], axis=1)
    out["cmpw2"] = np.stack([inp["nsa_cmp_w2_k"][:L], inp["nsa_cmp_w2_v"][:L]], axis=1)
    out["cmppe"] = np.stack([inp["nsa_cmp_pe_k"][:L].transpose(0, 2, 1), inp["nsa_cmp_pe_v"][:L].transpose(0, 2, 1)], axis=1)
    return {k: np.ascontiguousarray(v, dtype=np.float32) for k, v in out.items()}


_CACHE = {}


def kernel(**inputs):
    inp = {k: np.asarray(v) for k, v in inputs.items()}
    x = inp["x"]
    B, T, _ = x.shape
    L = inp["w_in"].shape[0]
    key = (T, L)
    if key not in _CACHE:
        _CACHE[key] = build(T, L)
    nc = _CACHE[key]
    shared = {}
    shared.update(prep_weights(inp, L))
    shared.update(prep_small(inp, L))
    shared.update(make_consts(T))
    in_maps = []
    for b in range(B):
        m = dict(shared)
        m["xT"] = np.ascontiguousarray(x[b].T)
        in_maps.append(m)
    res = run_bass_kernel_spmd(nc, in_maps, core_ids=list(range(B)))
    out = np.stack([np.asarray(r["outT"]).T for r in res.results], axis=0)
    return np.ascontiguousarray(out.astype(np.float32))
```

```python
from contextlib import ExitStack
import numpy as np
import concourse.bass as bass
import concourse.mybir as mybir
from concourse.bass_utils import run_bass_kernel_spmd

F32 = mybir.dt.float32
BF16 = mybir.dt.bfloat16
AF = mybir.ActivationFunctionType
ALU = mybir.AluOpType
AX = mybir.AxisListType

D = 2048
DFF = 5632
NFF = DFF // 128
KC = D // 128
EPS = 1e-6
NTOK = 512


class Res:
    __slots__ = ("w", "r")

    def __init__(self):
        self.w = None
        self.r = {}


class Sched:
    def __init__(self, nc, es, ndma=40):
        self.nc = nc
        self.eng = {"pe": nc.tensor, "act": nc.scalar, "dve": nc.vector, "pool": nc.gpsimd, "sp": nc.sync}
        self.sem = {}
        self.cnt = {}
        self.seen = {e: {} for e in self.eng}
        for e in ("pe", "act", "dve", "pool"):
            self.sem[("E", e)] = es.enter_context(nc.semaphore("sem_" + e))
            self.cnt[e] = 0
        self.ndma = {"sp": ndma, "pool": 8, "act": 8}
        for q, n in self.ndma.items():
            for i in range(n):
                self.sem[("D", q, i)] = es.enter_context(nc.semaphore("dsem_%s%d" % (q, i)))
        self.dma_i = {"sp": 0, "pool": 0, "act": 0}
        self.dlast = {}

    def _waits(self, eng, reads, writes):
        need = {}
        seen = self.seen[eng]
        own = ("E", eng)

        def add(k, v):
            if seen.get(k, 0) >= v:
                return
            if need.get(k, 0) < v:
                need[k] = v

        for r in reads:
            if r.w is not None:
                k, v = r.w
                if not (k == own and eng == "pe"):
                    add(k, v)
        pe_own = (eng == "pe")
        for w in writes:
            if w.w is not None and not (pe_own and w.w[0] == own):
                add(*w.w)
            for k, v in w.r.items():
                if not (pe_own and k == own):
                    add(k, v)
        return need

    def _emit_waits(self, eng, need):
        e = self.eng[eng]
        for k, v in need.items():
            e.wait_ge(self.sem[k], v)
            self.seen[eng][k] = v

    def op(self, eng, fn, reads=(), writes=()):
        need = self._waits(eng, reads, writes)
        self._emit_waits(eng, need)
        ins = fn(self.eng[eng])
        self.cnt[eng] += 1
        k = ("E", eng)
        v = self.cnt[eng]
        ins.then_inc(self.sem[k], 1)
        for r in reads:
            r.r[k] = v
        for w in writes:
            w.w = (k, v)
            w.r = {}

    def dma(self, out, in_, reads=(), writes=(), q="sp"):
        i = self.dma_i[q]
        self.dma_i[q] += 1
        slot = i % self.ndma[q]
        rnd = i // self.ndma[q]
        need = self._waits(q, reads, writes)
        k = ("D", q, slot)
        if rnd > 0 and self.seen[q].get(k, 0) < 16 * rnd:
            need[k] = max(need.get(k, 0), 16 * rnd)
        self._emit_waits(q, need)
        ins = self.eng[q].dma_start(out=out, in_=in_)
        v = 16 * (rnd + 1)
        ins.then_inc(self.sem[k], 16)
        self.dlast[k] = v
        for r in reads:
            r.r[k] = v
        for w in writes:
            w.w = (k, v)
            w.r = {}

    def barrier(self, include_pool=False):
        for eng in self.eng:
            need = {}
            for e2 in ("pe", "act", "dve", "pool"):
                k = ("E", e2)
                if e2 != eng and self.cnt[e2] > self.seen[eng].get(k, 0):
                    need[k] = self.cnt[e2]
            for k, v in self.dlast.items():
                if k[1] == "pool" and not include_pool:
                    continue
                if v > self.seen[eng].get(k, 0):
                    need[k] = v
            self._emit_waits(eng, need)


class Rot:
    def __init__(self, items):
        self.items = items
        self.i = 0

    def next(self):
        it = self.items[self.i % len(self.items)]
        self.i += 1
        return it


def mm_group(s, out, pairs, reads, writes, **kw):
    n = len(pairs)

    def fn(e):
        ins = None
        for i, (a, b) in enumerate(pairs):
            ins = e.matmul(out, a, b, start=(i == 0), stop=(i == n - 1), **kw)
        return ins

    s.op("pe", fn, reads=reads, writes=writes)


class Prog:
    def __init__(self, T, L, debug=()):
        self.T = T
        self.L = L
        self.debug = set(debug)
        self.nc = bass.Bass("TRN2", target_bir_lowering=False)
        self.dram = {}
        self.res = {}

    def din(self, name, shape, dt=F32):
        t = self.nc.dram_tensor(name, list(shape), dt, kind="ExternalInput")
        self.dram[name] = t
        return t

    def dout(self, name, shape, dt=F32):
        t = self.nc.dram_tensor(name, list(shape), dt, kind="ExternalOutput")
        self.dram[name] = t
        return t

    def dscr(self, name, shape, dt=F32):
        kind = "ExternalOutput" if name in self.debug else "Internal"
        t = self.nc.dram_tensor(name, list(shape), dt, kind=kind)
        self.dram[name] = t
        return t

    def R(self, *key):
        r = self.res.get(key)
        if r is None:
            r = Res()
            self.res[key] = r
        return r


WIN_CM = 17
WIN_TM = 7


def build(T, L, debug=(), stop_after=None, only_mix=None, skip_tt0=False, stage=99):
    P = Prog(T, L, debug)
    nc = P.nc
    NT = T // NTOK
    es = ExitStack()
    with es:
        s = Sched(nc, es)
        xT_in = P.din("xT", [D, T])
        outT = P.dout("outT", [D, T])
        gains = P.din("gains", [128, L * 3 * KC])
        w32 = {}
        w16 = {}
        wspec = {
            "wgu1": (NFF, 2 * KC * 128), "wd1": (KC, NFF * 128),
            "wgu2": (NFF, 2 * KC * 128), "wd2": (KC, NFF * 128),
            "wincm": (WIN_CM, KC * 128), "wintm": (WIN_TM, KC * 512), "wout": (KC, KC * 128),
            "cmpw1": (2, 4096), "cmpw2": (2, 128), "cmppe": (2, 32),
        }
        for nm, (npc, el) in wspec.items():
            w32[nm] = P.din(nm, [L, npc, 128, el])
            w16[nm] = P.dscr(nm + "_bf", [L, npc, 128, el], BF16)
        xT = P.dscr("xT_s", [D, T])
        mixT = P.dscr("mixT", [D, T], BF16)
        cm_rows = {"lru": 1024, "kvc": 512, "gqk": 512}
        lruT = P.dscr("lruT", [1024, T])
        kvcT = P.dscr("kvcT", [512, T], BF16)
        gqkT = P.dscr("gqkT", [512, T])
        gaT = P.dscr("gaT", [16, T], BF16)
        q_tm = P.dscr("q_tm", [T, 1024])
        ksw_tm = P.dscr("ksw_tm", [T, 1024])
        gkg_tm = P.dscr("gkg_tm", [T, 512])
        gv_tm = P.dscr("gv_tm", [T, 512], BF16)
        gg_tm = P.dscr("gg_tm", [T, 512])

        lruv = P.din("lruv", [L, 128, 4, 9])
        lrug = P.din("lrug", [L, 2, 4, 128, 128])
        glaw = P.din("glaw", [L, 17, 256])
        glan = P.din("glan", [L, 128])
        c128_d = P.din("c128", [128, 6, 128])
        NCBp = 256
        cmaskT = P.din("cmaskT", [256, T])
        Efull = P.din("Efull", [64, T])
        ropecs = P.din("ropecs", [T, 32])
        fmvm = P.din("fmvm", [T, 128])
        ovl = P.din("ovl", [256, 64])
        nsan = P.din("nsan", [L, 4, 128])
        nsaon = P.din("nsaon", [L, 1024])
        nsagb = P.din("nsagb", [L, 24])
        cast_order = ["wgu1", "wd1", "wincm", "wintm", "cmpw1", "cmpw2", "cmppe", "wout", "wgu2", "wd2"]

        def cast_jobs(l):
            jobs = []
            for nm in cast_order:
                npc, el = wspec[nm]
                grp = max(1, (1 << 20) // (128 * el))
                for p0 in range(0, npc, grp):
                    jobs.append((nm, l, p0, min(npc, p0 + grp)))
            return jobs

        def cast_job(job):
            nm, l, p0, p1 = job
            src = w32[nm].ap()[l, p0:p1].rearrange("c p e -> (c p) e")
            dst = w16[nm].ap()[l, p0:p1].rearrange("c p e -> (c p) e")
            s.dma(dst, src, writes=[P.R("w", nm, l, pc) for pc in range(p0, p1)], q="pool")

        def cast_layer(l):
            for job in cast_jobs(l):
                cast_job(job)

        def cast_layer_old(l):
            for nm, (npc, el) in wspec.items():
                grp = max(1, (1 << 20) // (128 * el))
                for p0 in range(0, npc, grp):
                    p1 = min(npc, p0 + grp)
                    src = w32[nm].ap()[l, p0:p1].rearrange("c p e -> (c p) e")
                    dst = w16[nm].ap()[l, p0:p1].rearrange("c p e -> (c p) e")
                    ws = [P.R("w", nm, l, pc) for pc in range(p0, p1)]
                    s.dma(dst, src, writes=ws, q="pool")

        cmask_bf = P.dscr("cmask_bf", [256, T], BF16)
        Efull_bf = P.dscr("Efull_bf", [64, T], BF16)
        ovl_bf = P.dscr("ovl_bf", [256, 64], BF16)
        s.dma(cmask_bf.ap(), cmaskT.ap(), writes=[P.R("cmask_bf")], q="pool")
        s.dma(Efull_bf.ap(), Efull.ap(), writes=[P.R("Efull_bf")], q="pool")
        s.dma(ovl_bf.ap(), ovl.ap(), writes=[P.R("ovl_bf")], q="pool")
        cast_layer(0)

        def sb(name, shape, dt):
            return es.enter_context(nc.sbuf_tensor(name, list(shape), dt))

        psall = es.enter_context(nc.psum_tensor("psall", [128, 4096], F32))
        psb = psall.bitcast(BF16)
        psum = [psall[:, i * 512:(i + 1) * 512] for i in range(8)]
        psR = [P.R("ps", i) for i in range(8)]
        ones32 = sb("ones32", [128, 128], F32)
        gains_sb = sb("gains_sb", [128, L * 3 * KC], F32)
        s.op("dve", lambda e: e.memset(ones32[:], 1.0), writes=[P.R("ones32")])
        s.dma(gains_sb[:], gains.ap(), writes=[P.R("gains")])
        cst = sb("cst", [128, 4], F32)
        s.op("dve", lambda e: e.memset(cst[:, 0:1], 1.0), writes=[P.R("cst")])
        s.op("dve", lambda e: e.memset(cst[:, 1:2], EPS), writes=[P.R("cst")])
        c128 = sb("c128_sb", [128, 6, 128], F32)
        s.dma(c128[:], c128_d.ap(), writes=[P.R("c128")])
        identb = sb("identb", [128, 128], BF16)
        s.op("dve", lambda e: e.tensor_copy(out=identb[:], in_=c128[:, 0, :]), reads=[P.R("c128")], writes=[P.R("identb")])

        g = dict(locals())
        orchestrate(P, s, g)
        s.barrier(include_pool=True)
    return nc


def orchestrate(P, s, g):
    T, L = P.T, P.L
    NT = T // NTOK
    stop_after = g.get("stop_after")

    def body0(f):
        for t in range(NT):
            f["load_x"](g["xT_in"], t, "xT_in")
            f["ffn"](0, 1, False, True)
            f["proj"](0, t, True)
            f["store_x"](g["xT"] if stop_after != "tt0" else g["outT"], t, "xT")
    if not g.get("skip_tt0"):
        tt_phase(P, s, g, body0, "a")
        s.barrier()
    if stop_after == "tt0":
        return
    for l in range(L):
        mix_phase(P, s, g, l)
        s.barrier()
        if stop_after == "mix%d" % l:
            return

        def body(f, l=l):
            for t in range(NT):
                f["load_x"](g["xT"], t, "xT")
                f["wout"](l, t, True)
                f["ffn"](l, 2, True, l + 1 < L)
                if l + 1 < L:
                    f["ffn"](l + 1, 1, True, True)
                    f["proj"](l + 1, t, True)
                    f["store_x"](g["xT"], t, "xT")
                else:
                    f["store_x"](g["outT"], t, "outT")
        tt_phase(P, s, g, body, "b%d" % l)
        s.barrier()


def mix_phase(P, s, g, l):
    only = g.get("only_mix")
    if only is None or "lru" in only:
        lru_mix(P, s, g, l)
        s.barrier()
    if only is None or "gla" in only:
        gla_mix(P, s, g, l)
        s.barrier()
    if only is None or "nsa" in only:
        nsa_mix(P, s, g, l)
        s.barrier()


def gelu2(s, dst, src, tmp, rsrc, rtmp, rdst):
    s.op("dve", lambda e: e.tensor_tensor(out=tmp, in0=src, in1=src, op=ALU.mult), reads=[rsrc], writes=[rtmp])
    s.op("dve", lambda e: e.tensor_scalar(out=tmp, in0=tmp, scalar1=0.044715, scalar2=1.0, op0=ALU.mult, op1=ALU.add),
         reads=[rtmp], writes=[rtmp])
    s.op("dve", lambda e: e.tensor_tensor(out=tmp, in0=tmp, in1=src, op=ALU.mult), reads=[rtmp, rsrc], writes=[rtmp])
    s.op("act", lambda e: e.activation(out=tmp, in_=tmp, func=AF.Tanh, scale=0.7978845608028654), reads=[rtmp], writes=[rtmp])
    s.op("dve", lambda e: e.scalar_tensor_tensor(out=dst, in0=tmp, scalar=1.0, in1=src, op0=ALU.add, op1=ALU.mult),
         reads=[rtmp, rsrc], writes=[rdst])


def lru_mix(P, s, g, l):
    nc = P.nc
    T = P.T
    NTT = T // 512
    psum, ones32, cst = g["psum"], g["ones32"], g["cst"]
    lruT, mixT = g["lruT"], g["mixT"]
    R = P.R
    with ExitStack() as ms:
        def sb(name, shape, dt):
            return ms.enter_context(nc.sbuf_tensor("lru_%s_%d" % (name, l), list(shape), dt))
        lv = sb("lv", [128, 4, 9], F32)
        gw = sb("gw", [128, 2, 4, 128], F32)
        c12 = sb("c12", [128, 2, 4], F32)
        u_sb = sb("u", [128, 4, 515], F32)
        y_sb = sb("y", [128, 4, 512], F32)
        xc = sb("xc", [128, 4, 512], F32)
        r_sb = sb("r", [128, 4, 512], F32)
        i_sb = sb("i", [128, 4, 512], F32)
        a_sb = sb("a", [128, 4, 512], F32)
        m_sb = sb("m", [128, 4, 512], F32)
        h_sb = sb("h", [128, 4, 512], F32)
        gl = sb("gl", [128, 4, 512], F32)
        tmp = sb("tmp", [128, 4, 512], F32)
        ya = sb("ya", [128, 4, 512], F32)
        sq = sb("sq", [128, 2, 512], F32)
        rstd = sb("rstd", [128, 512], F32)
        outb = sb("outb", [128, 4, 512], BF16)
        hprev = sb("hprev", [128, 4], F32)
        s.dma(lv[:], g["lruv"].ap()[l], writes=[R("lv")])
        s.dma(gw[:], g["lrug"].ap()[l].rearrange("a c p o -> p a c o"), writes=[R("gw")])
        s.op("act", lambda e: e.activation(out=c12[:, 0, :], in_=lv[:, :, 7], func=AF.Exp, scale=-1.0), reads=[R("lv")], writes=[R("c12")])
        s.op("act", lambda e: e.activation(out=c12[:, 0, :], in_=c12[:, 0, :], func=AF.Ln, bias=cst[:, 0:1]), reads=[R("c12"), R("cst")], writes=[R("c12")])
        s.op("dve", lambda e: e.tensor_scalar(out=c12[:, 1, :], in0=c12[:, 0, :], scalar1=-16.0, scalar2=None, op0=ALU.mult), reads=[R("c12")], writes=[R("c12")])
        s.op("dve", lambda e: e.tensor_scalar(out=c12[:, 0, :], in0=c12[:, 0, :], scalar1=-8.0, scalar2=None, op0=ALU.mult), reads=[R("c12")], writes=[R("c12")])
        sqrot = Rot([(0, R("lsq", 0)), (1, R("lsq", 1))])
        C4 = range(4)
        for tt in range(NTT):
            t0 = tt * 512
            for c in C4:
                rows = slice(c * 128, (c + 1) * 128)
                uR, yR = R("lu", c), R("ly", c)
                if tt == 0:
                    s.op("dve", lambda e: e.memset(u_sb[:, c, 0:3], 0.0), writes=[uR])
                    s.dma(u_sb[:, c, 3:515], lruT.ap()[rows, 0:512], reads=[R("lruT", 0)], writes=[uR])
                else:
                    s.dma(u_sb[:, c, :], lruT.ap()[rows, t0 - 3:t0 + 512], reads=[R("lruT", tt - 1), R("lruT", tt)], writes=[uR])
                s.dma(y_sb[:, c, :], lruT.ap()[512 + c * 128:512 + (c + 1) * 128, t0:t0 + 512], reads=[R("lruT", tt)], writes=[yR])
            for c in C4:
                s.op("dve", lambda e: e.tensor_scalar(out=xc[:, c, :], in0=u_sb[:, c, 3:515], scalar1=lv[:, c, 3:4], scalar2=lv[:, c, 4:5],
                                                      op0=ALU.mult, op1=ALU.add), reads=[R("lu", c), R("lv")], writes=[R("lxc", c)])
            for k in (2, 1, 0):
                for c in C4:
                    s.op("dve", lambda e: e.scalar_tensor_tensor(out=xc[:, c, :], in0=u_sb[:, c, k:k + 512], scalar=lv[:, c, k:k + 1],
                                                                  in1=xc[:, c, :], op0=ALU.mult, op1=ALU.add), reads=[R("lu", c), R("lxc", c)], writes=[R("lxc", c)])
            for c in C4:
                for a, (dst, nm, bcol) in enumerate(((r_sb, "lr", 5), (i_sb, "li", 6))):
                    bk = a + 2 * (c % 2)
                    pr = g["psR"][bk]
                    s.op("pe", lambda e: e.matmul(psum[bk][:], gw[:, a, c, :], xc[:, c, :], start=True, stop=True), reads=[R("gw"), R("lxc", c)], writes=[pr])
                    s.op("act", lambda e: e.activation(out=dst[:, c, :], in_=psum[bk][:], func=AF.Sigmoid, bias=lv[:, c, bcol:bcol + 1]),
                         reads=[pr, R("lv")], writes=[R(nm, c)])
            for c in C4:
                s.op("act", lambda e: e.activation(out=a_sb[:, c, :], in_=r_sb[:, c, :], func=AF.Exp, scale=c12[:, 0, c:c + 1]), reads=[R("lr", c), R("c12")], writes=[R("la", c)])
                s.op("act", lambda e: e.activation(out=m_sb[:, c, :], in_=r_sb[:, c, :], func=AF.Exp, scale=c12[:, 1, c:c + 1]), reads=[R("lr", c), R("c12")], writes=[R("lm", c)])
            for c in C4:
                s.op("act", lambda e: e.activation(out=m_sb[:, c, :], in_=m_sb[:, c, :], func=AF.Sqrt, scale=-1.0, bias=cst[:, 0:1]), reads=[R("lm", c), R("cst")], writes=[R("lm", c)])
            for c in C4:
                s.op("dve", lambda e: e.tensor_tensor(out=m_sb[:, c, :], in0=m_sb[:, c, :], in1=i_sb[:, c, :], op=ALU.mult), reads=[R("lm", c), R("li", c)], writes=[R("lm", c)])
            for c in C4:
                s.op("dve", lambda e: e.tensor_tensor(out=m_sb[:, c, :], in0=m_sb[:, c, :], in1=xc[:, c, :], op=ALU.mult), reads=[R("lm", c), R("lxc", c)], writes=[R("lm", c)])
            for c in C4:
                init = 0.0 if tt == 0 else hprev[:, c:c + 1]
                s.op("dve", lambda e: e.tensor_tensor_scan(out=h_sb[:, c, :], data0=a_sb[:, c, :], data1=m_sb[:, c, :], initial=init,
                                                           op0=ALU.mult, op1=ALU.add), reads=[R("la", c), R("lm", c), R("lhp", c)], writes=[R("lh", c)])
            for c in C4:
                s.op("dve", lambda e: e.tensor_copy(out=hprev[:, c:c + 1], in_=h_sb[:, c, 511:512]), reads=[R("lh", c)], writes=[R("lhp", c)])
            for c in C4:
                s.op("dve", lambda e: e.tensor_tensor(out=tmp[:, c, :], in0=y_sb[:, c, :], in1=y_sb[:, c, :], op=ALU.mult), reads=[R("ly", c)], writes=[R("ltmp", c)])
            for c in C4:
                s.op("dve", lambda e: e.tensor_scalar(out=tmp[:, c, :], in0=tmp[:, c, :], scalar1=0.044715, scalar2=1.0, op0=ALU.mult, op1=ALU.add),
                     reads=[R("ltmp", c)], writes=[R("ltmp", c)])
            for c in C4:
                s.op("dve", lambda e: e.tensor_tensor(out=tmp[:, c, :], in0=tmp[:, c, :], in1=y_sb[:, c, :], op=ALU.mult), reads=[R("ltmp", c), R("ly", c)], writes=[R("ltmp", c)])
            for c in C4:
                s.op("act", lambda e: e.activation(out=tmp[:, c, :], in_=tmp[:, c, :], func=AF.Tanh, scale=0.7978845608028654), reads=[R("ltmp", c)], writes=[R("ltmp", c)])
            for c in C4:
                s.op("dve", lambda e: e.scalar_tensor_tensor(out=gl[:, c, :], in0=tmp[:, c, :], scalar=1.0, in1=y_sb[:, c, :], op0=ALU.add, op1=ALU.mult),
                     reads=[R("ltmp", c), R("ly", c)], writes=[R("lgl", c)])
            for c in C4:
                s.op("dve", lambda e: e.scalar_tensor_tensor(out=ya[:, c, :], in0=gl[:, c, :], scalar=0.5, in1=h_sb[:, c, :], op0=ALU.mult, op1=ALU.mult),
                     reads=[R("lgl", c), R("lh", c)], writes=[R("lya", c)])
            for c in C4:
                qi, qR = sqrot.next()
                s.op("act", lambda e: e.activation(out=sq[:, qi, :], in_=ya[:, c, :], func=AF.Square), reads=[R("lya", c)], writes=[qR])
                s.op("pe", lambda e: e.matmul(psum[6][:], ones32[:], sq[:, qi, :], start=(c == 0), stop=(c == 3)), reads=[qR, R("ones32")], writes=[g["psR"][6]])
            s.op("act", lambda e: e.activation(out=rstd[:], in_=psum[6][:], func=AF.Sqrt, scale=1.0 / 512, bias=cst[:, 1:2]),
                 reads=[g["psR"][6], R("cst")], writes=[R("lrstd")])
            s.op("dve", lambda e: e.reciprocal(out=rstd[:], in_=rstd[:]), reads=[R("lrstd")], writes=[R("lrstd")])
            for c in C4:
                s.op("dve", lambda e: e.scalar_tensor_tensor(out=outb[:, c, :], in0=ya[:, c, :], scalar=lv[:, c, 8:9], in1=rstd[:], op0=ALU.mult, op1=ALU.mult),
                     reads=[R("lya", c), R("lrstd"), R("lv")], writes=[R("loutb", c)])
                s.dma(mixT.ap()[c * 128:(c + 1) * 128, t0:t0 + 512], outb[:, c, :], reads=[R("loutb", c)], writes=[R("mixT", tt)], q="act")


def gla_mix(P, s, g, l):
    nc = P.nc
    T = P.T
    NG = T // 512
    psum, psb, psR, ones32, cst, c128 = g["psum"], g["psb"], g["psR"], g["ones32"], g["cst"], g["c128"]
    identb = g["identb"]
    R = P.R
    with ExitStack() as ms:
        def sb(name, shape, dt):
            return ms.enter_context(nc.sbuf_tensor("gla_%s_%d" % (name, l), list(shape), dt))
        aw32 = sb("aw32", [17, 256], F32)
        aw = sb("aw", [17, 256], BF16)
        gout = sb("gout", [128, 128], F32)
        qT = sb("qT", [64, 4, 512], F32)
        kT = sb("kT", [64, 4, 512], F32)
        gaT = sb("gaT", [17, 512], BF16)
        ktm = sb("ktm", [128, 4, 256], F32)
        vtm = sb("vtm", [128, 4, 512], BF16)
        ggt = sb("ggt", [128, 4, 512], F32)
        lsp = sb("lsp", [128, 256], F32)
        ekb = sb("ekb", [128, 256], F32)
        kp = sb("kp", [128, 2, 256], BF16)
        eb = sb("eb", [64, 2, 4, 128], F32)
        enb = sb("enb", [64, 4, 128], F32)
        qt_b = sb("qtb", [64, 2, 4, 128], BF16)
        kt_b = sb("ktb", [64, 2, 4, 128], BF16)
        atm = sb("atm", [128, 2, 4, 128], BF16)
        S32 = sb("S32", [64, 4, 128], F32)
        Sb = sb("Sb", [64, 4, 128], BF16)
        osq = sb("osq", [128, 4, 128], F32)
        on = sb("on", [128, 4, 128], F32)
        ssum = sb("ssum", [128, 4], F32)
        sgg = sb("sgg", [128, 512], F32)
        yc = sb("yc", [128, 512], BF16)
        stg = sb("stg", [128, 4, 512], BF16)
        s.dma(aw32[:], g["glaw"].ap()[l], writes=[R("aw32")])
        s.op("dve", lambda e: e.tensor_copy(out=aw[:], in_=aw32[:]), reads=[R("aw32")], writes=[R("aw")])
        s.dma(gout[:], g["glan"].ap()[l:l + 1, :].to_broadcast([128, 128]), writes=[R("gout")])
        s.op("dve", lambda e: e.memset(gaT[:], 1.0), writes=[R("gaT")])
        s.op("dve", lambda e: e.memset(S32[:], 0.0), writes=[R("S32")])
        s.op("dve", lambda e: e.memset(Sb[:], 0.0), writes=[R("Sb")])
        for gi in range(NG):
            tok = slice(gi * 512, (gi + 1) * 512)
            s.dma(qT[:], g["gqkT"].ap()[0:256, tok].rearrange("(a p) n -> p a n", p=64), reads=[R("gqkT", gi)], writes=[R("gqT")])
            s.dma(kT[:], g["gqkT"].ap()[256:512, tok].rearrange("(a p) n -> p a n", p=64), reads=[R("gqkT", gi)], writes=[R("gkT")])
            s.dma(gaT[0:16, :], g["gaT"].ap()[:, tok], reads=[R("gaT", gi)], writes=[R("gaT")])
            s.dma(ktm[:], g["gkg_tm"].ap()[tok, 0:256].rearrange("(c p) n -> p c n", p=128), reads=[R("gkg_tm", gi)], writes=[R("gktm")])
            s.dma(vtm[:], g["gv_tm"].ap()[tok, :].rearrange("(c p) n -> p c n", p=128), reads=[R("gv_tm", gi)], writes=[R("gvtm")])
            s.dma(ggt[:], g["gg_tm"].ap()[tok, :].rearrange("(c p) n -> p c n", p=128), reads=[R("gg_tm", gi)], writes=[R("gggt")])
            def s1(cc):
                pb = cc % 2
                ct = slice(cc * 128, (cc + 1) * 128)
                s.op("pe", lambda e: e.matmul(psum[0][:, 0:256], gaT[:, ct], aw[:], start=True, stop=True), reads=[R("gaT"), R("aw")], writes=[psR[0]])
                s.op("act", lambda e: e.activation(out=lsp[:], in_=psum[0][:, 0:256], func=AF.Exp, scale=-1.0), reads=[psR[0]], writes=[R("lsp")])
                s.op("act", lambda e: e.activation(out=lsp[:], in_=lsp[:], func=AF.Ln, bias=cst[:, 0:1]), reads=[R("lsp"), R("cst")], writes=[R("lsp")])
                s.op("pe", lambda e: e.matmul(psum[1][:, 0:256], c128[:, 2, :], lsp[:], start=True, stop=True), reads=[R("lsp"), R("c128")], writes=[psR[1]])
                for a in range(4):
                    s.op("pe", lambda e: e.matmul(psum[2][0:64, a * 128:(a + 1) * 128], lsp[:, a * 64:(a + 1) * 64], c128[:, 1, :], start=True, stop=True),
                         reads=[R("lsp"), R("c128")], writes=[psR[2]])
                s.op("act", lambda e: e.activation(out=ekb[:], in_=psum[1][:, 0:256], func=AF.Exp), reads=[psR[1]], writes=[R("ekb")])
                s.op("dve", lambda e: e.tensor_tensor(out=kp[:, pb, :], in0=ktm[:, cc, :], in1=ekb[:], op=ALU.mult), reads=[R("gktm"), R("ekb")], writes=[R("kp", pb)])
                s.op("act", lambda e: e.activation(out=eb[:, pb].rearrange("p a n -> p (a n)"), in_=psum[2][0:64, :], func=AF.Exp), reads=[psR[2]], writes=[R("eb", pb)])
                s.op("act", lambda e: e.activation(out=enb[:].rearrange("p a n -> p (a n)"), in_=psum[2][0:64, :], func=AF.Exp, scale=-1.0), reads=[psR[2]], writes=[R("enb")])
                s.op("dve", lambda e: e.scalar_tensor_tensor(out=qt_b[:, pb], in0=qT[:, :, ct], scalar=0.125, in1=eb[:, pb], op0=ALU.mult, op1=ALU.mult),
                     reads=[R("gqT"), R("eb", pb)], writes=[R("qtb", pb)])
                s.op("dve", lambda e: e.tensor_tensor(out=kt_b[:, pb], in0=kT[:, :, ct], in1=enb[:], op=ALU.mult), reads=[R("gkT"), R("enb")], writes=[R("ktb", pb)])
                def at_fn(e):
                    ins = None
                    for h in range(4):
                        ins = e.matmul(psum[3][:, h * 128:(h + 1) * 128], kt_b[:, pb, h, :], qt_b[:, pb, h, :], start=True, stop=True)
                    return ins
                s.op("pe", at_fn, reads=[R("ktb", pb), R("qtb", pb)], writes=[psR[3]])
                s.op("dve", lambda e: e.tensor_tensor(out=atm[:, pb], in0=psum[3][:].rearrange("p (h n) -> p h n", h=4),
                                                      in1=c128[:, 3, :].unsqueeze(1).to_broadcast([128, 4, 128]), op=ALU.mult),
                     reads=[psR[3], R("c128")], writes=[R("atm", pb)])
            def s2(cc):
                pb = cc % 2
                ct = slice(cc * 128, (cc + 1) * 128)
                def o_fn(e):
                    ins = None
                    for h in range(4):
                        e.matmul(psum[4][:, h * 128:(h + 1) * 128], qt_b[:, pb, h, :], Sb[:, h, :], start=True, stop=False)
                        ins = e.matmul(psum[4][:, h * 128:(h + 1) * 128], atm[:, pb, h, :], vtm[:, cc, h * 128:(h + 1) * 128], start=False, stop=True)
                    return ins
                s.op("pe", o_fn, reads=[R("qtb", pb), R("Sb"), R("atm", pb), R("gvtm")], writes=[psR[4]])
                def kv_fn(e):
                    ins = None
                    for h in range(4):
                        ins = e.matmul(psum[5][0:64, h * 128:(h + 1) * 128], kp[:, pb, h * 64:(h + 1) * 64], vtm[:, cc, h * 128:(h + 1) * 128], start=True, stop=True)
                    return ins
                s.op("pe", kv_fn, reads=[R("kp", pb), R("gvtm")], writes=[psR[5]])
                s.op("dve", lambda e: e.tensor_tensor(out=S32[:], in0=S32[:], in1=eb[:, pb, :, 127:128].to_broadcast([64, 4, 128]), op=ALU.mult),
                     reads=[R("S32"), R("eb", pb)], writes=[R("S32")])
                s.op("dve", lambda e: e.tensor_tensor(out=S32[:], in0=S32[:], in1=psum[5][0:64, :].rearrange("p (h n) -> p h n", h=4), op=ALU.add),
                     reads=[R("S32"), psR[5]], writes=[R("S32")])
                s.op("act", lambda e: e.activation(out=Sb[:], in_=S32[:], func=AF.Copy), reads=[R("S32")], writes=[R("Sb")])
                o3 = psum[4][:].rearrange("p (h n) -> p h n", h=4)
                s.op("act", lambda e: e.activation(out=osq[:], in_=o3, func=AF.Square), reads=[psR[4]], writes=[R("osq")])
                s.op("dve", lambda e: e.tensor_reduce(out=ssum[:], in_=osq[:], axis=AX.X, op=ALU.add), reads=[R("osq")], writes=[R("ssum")])
                s.op("act", lambda e: e.activation(out=ssum[:], in_=ssum[:], func=AF.Sqrt, scale=1.0 / 128, bias=cst[:, 1:2]), reads=[R("ssum"), R("cst")], writes=[R("ssum")])
                s.op("dve", lambda e: e.reciprocal(out=ssum[:], in_=ssum[:]), reads=[R("ssum")], writes=[R("ssum")])
                s.op("dve", lambda e: e.tensor_tensor(out=on[:], in0=o3, in1=ssum[:].unsqueeze(2).to_broadcast([128, 4, 128]), op=ALU.mult),
                     reads=[psR[4], R("ssum")], writes=[R("on")])
                s.op("dve", lambda e: e.tensor_tensor(out=on[:], in0=on[:], in1=gout[:].unsqueeze(1).to_broadcast([128, 4, 128]), op=ALU.mult),
                     reads=[R("on"), R("gout")], writes=[R("on")])
                s.op("act", lambda e: e.activation(out=sgg[:], in_=ggt[:, cc, :], func=AF.Silu), reads=[R("gggt")], writes=[R("sgg")])
                s.op("dve", lambda e: e.tensor_tensor(out=yc[:], in0=on[:].rearrange("p h n -> p (h n)"), in1=sgg[:], op=ALU.mult),
                     reads=[R("on"), R("sgg")], writes=[R("yc")])
                def tr_fn(e):
                    ins = None
                    for h in range(4):
                        ins = e.transpose(psb[:, 6 * 1024 + h * 128:6 * 1024 + (h + 1) * 128], yc[:, h * 128:(h + 1) * 128], identb[:])
                    return ins
                s.op("pe", tr_fn, reads=[R("yc"), R("identb")], writes=[psR[6]])
                s.op("act", lambda e: e.activation(out=stg[:, :, ct], in_=psb[:, 6 * 1024:6 * 1024 + 512].rearrange("p (h n) -> p h n", h=4), func=AF.Copy),
                     reads=[psR[6]], writes=[R("gstg")])
            s1(0)
            for cc in range(4):
                if cc + 1 < 4:
                    s1(cc + 1)
                s2(cc)
            s.dma(g["mixT"].ap()[1536:2048, tok].rearrange("(h p) n -> p h n", p=128), stg[:], reads=[R("gstg")], writes=[R("mixT", gi)], q="act")


def nsa_mix(P, s, g, l):
    nc = P.nc
    T = P.T
    NQ = T // 128
    NCB = (T - 32) // 16 + 1
    SCALE = 128 ** -0.5
    psum, psb, psall, psR, ones32, cst, c128, identb = (g["psum"], g["psb"], g["psall"], g["psR"], g["ones32"], g["cst"],
                                                        g["c128"], g["identb"])
    w16 = g["w16"]
    R = P.R
    with ExitStack() as ms:
        def sbuf(name, shape, dt, st=ms):
            return st.enter_context(nc.sbuf_tensor("nsa_%s_%d" % (name, l), list(shape), dt))
        ksT = sbuf("ksT", [128, 2, T], BF16)
        kwT = sbuf("kwT", [128, 2, T], BF16)
        vsa = sbuf("vsa", [128, NQ, 2, 129], BF16)
        vwa = sbuf("vwa", [128, NQ, 2, 129], BF16)
        cmask = sbuf("cmask", [128, 2, T], BF16)
        Efull = sbuf("Efull", [64, T], BF16)
        cs_sb = sbuf("cs", [128, NQ, 32], F32)
        fmvm = sbuf("fmvm", [128, NQ, 128], F32)
        gates = sbuf("gates", [128, NQ, 24], F32)
        outn = sbuf("outn", [128, 1024], F32)
        nrm_bc = sbuf("nrm_bc", [128, 4, 128], F32)
        kcg = sbuf("kcg", [128, 1], F32)
        gb_bc = sbuf("gb_bc", [128, 24], F32)
        kcn = sbuf("kcn", [128, 2, 256], BF16)
        vca = sbuf("vca", [128, 2, 2, 193], BF16)
        negc4 = sbuf("negc4", [128, 4, 128], BF16)
        negu4 = sbuf("negu4", [128, 4, 128], BF16)
        w2 = sbuf("w2", [128, 2, 128], BF16)
        pe_bf = sbuf("pe_bf", [128, 2, 32], BF16)

        s.dma(cmask[:], g["cmask_bf"].ap().rearrange("(c p) t -> p c t", p=128), reads=[R("cmask_bf")], writes=[R("cmask")])
        s.dma(Efull[:], g["Efull_bf"].ap(), reads=[R("Efull_bf")], writes=[R("Efull")])
        s.dma(cs_sb[:], g["ropecs"].ap().rearrange("(i p) c -> p i c", p=128), writes=[R("cs")])
        s.dma(fmvm[:], g["fmvm"].ap().rearrange("(i p) c -> p i c", p=128), writes=[R("fmvm")])
        s.dma(outn[:], g["nsaon"].ap()[l:l + 1, :].to_broadcast([128, 1024]), writes=[R("outn")])
        for k in range(4):
            s.dma(nrm_bc[:, k, :], g["nsan"].ap()[l, k:k + 1, :].to_broadcast([128, 128]), writes=[R("nrm_bc")])
        s.dma(kcg[:], g["nsan"].ap()[l, 1, :].rearrange("(p o) -> p o", o=1), writes=[R("kcg")])
        s.dma(gb_bc[:], g["nsagb"].ap()[l:l + 1, :].to_broadcast([128, 24]), writes=[R("gb_bc")])
        s.op("dve", lambda e: e.memset(kcn[:], 0.0), writes=[R("kcn")])
        s.op("dve", lambda e: e.memset(vca[:], 0.0), writes=[R("vca")])
        s.op("dve", lambda e: e.memset(vca[:, :, :, 128:129], 1.0), writes=[R("vca")])
        for kv in range(2):
            s.dma(vca[:, :, kv, 129:193], g["ovl_bf"].ap().rearrange("(c p) s -> p c s", p=128), reads=[R("vca"), R("ovl_bf")], writes=[R("vca")])
        s.op("dve", lambda e: e.memset(vsa[:, :, :, 128:129], 1.0), writes=[R("vsa1")])
        s.op("dve", lambda e: e.memset(vwa[:, :, :, 128:129], 1.0), writes=[R("vwa1")])
        s.op("dve", lambda e: e.tensor_copy(out=negc4[:], in_=c128[:, 4, :].unsqueeze(1).to_broadcast([128, 4, 128])), reads=[R("c128")], writes=[R("negc4")])
        s.op("dve", lambda e: e.tensor_copy(out=negu4[:], in_=c128[:, 5, :].unsqueeze(1).to_broadcast([128, 4, 128])), reads=[R("c128")], writes=[R("negu4")])
        s.dma(w2[:], w16["cmpw2"].ap()[l].rearrange("k p o -> p k o"), reads=[R("w", "cmpw2", l, 0), R("w", "cmpw2", l, 1)], writes=[R("w2")])
        s.dma(pe_bf[:], w16["cmppe"].ap()[l].rearrange("k p o -> p k o"), reads=[R("w", "cmppe", l, 0), R("w", "cmppe", l, 1)], writes=[R("pe_bf")])

        with ExitStack() as cs_:
            xT_sb = sbuf("xT", [128, 4, T], BF16, cs_)
            w1 = sbuf("w1", [128, 2, 4096], BF16, cs_)
            hid = sbuf("hid", [128, 4, 256], BF16, cs_)
            hx = sbuf("hx", [128, 256], F32, cs_)
            htmp = sbuf("htmp", [128, 256], F32, cs_)
            hg = sbuf("hg", [128, 256], F32, cs_)
            cvec = sbuf("cvec", [128, 2], F32, cs_)
            ksq = sbuf("ksq", [128, 256], F32, cs_)
            krs = sbuf("krs", [128, 256], F32, cs_)
            s.dma(xT_sb[:], g["kvcT"].ap().rearrange("(a p) t -> p a t", p=128), reads=[R("kvcT", t) for t in range(T // NTOK)], writes=[R("nxT")])
            s.dma(w1[:], w16["cmpw1"].ap()[l].rearrange("k p o -> p k o"), reads=[R("w", "cmpw1", l, 0), R("w", "cmpw1", l, 1)], writes=[R("nw1")])
            s.op("dve", lambda e: e.memset(hid[:], 0.0), writes=[R("nhid")])
            for kvi in range(2):
                mm_group(s, psum[7][:, 0:1], [(w1[:, kvi, ll * 128:(ll + 1) * 128], pe_bf[:, kvi, ll:ll + 1]) for ll in range(32)],
                         reads=[R("nw1"), R("pe_bf")], writes=[psR[7]])
                s.op("dve", lambda e: e.tensor_copy(out=cvec[:, kvi:kvi + 1], in_=psum[7][:, 0:1]), reads=[psR[7]], writes=[R("ncvec")])
                for hd in range(2):
                    a = kvi * 2 + hd
                    mm_group(s, psum[0][:, 0:NCB], [(w1[:, kvi, ll * 128:(ll + 1) * 128], xT_sb[:, a, ll:ll + 16 * (NCB - 1) + 1:16]) for ll in range(32)],
                             reads=[R("nw1"), R("nxT")], writes=[psR[0]])
                    s.op("dve", lambda e: e.tensor_scalar(out=hx[:, 0:NCB], in0=psum[0][:, 0:NCB], scalar1=cvec[:, kvi:kvi + 1], scalar2=None, op0=ALU.add),
                         reads=[psR[0], R("ncvec")], writes=[R("nhx")])
                    gelu2(s, hg[:, 0:NCB], hx[:, 0:NCB], htmp[:, 0:NCB], R("nhx"), R("nhtmp"), R("nhg"))
                    s.op("act", lambda e: e.activation(out=hid[:, a, 0:NCB], in_=hg[:, 0:NCB], func=AF.Copy, scale=0.5), reads=[R("nhg")], writes=[R("nhid")])
                    if kvi == 0:
                        s.op("pe", lambda e: e.matmul(psum[1][:, 0:NCB], w2[:, 0, :], hid[:, a, 0:NCB], start=True, stop=True), reads=[R("w2"), R("nhid")], writes=[psR[1]])
                        s.op("act", lambda e: e.activation(out=ksq[:, 0:NCB], in_=psum[1][:, 0:NCB], func=AF.Square), reads=[psR[1]], writes=[R("nksq")])
                        s.op("pe", lambda e: e.matmul(psum[2][:, 0:NCB], ones32[:], ksq[:, 0:NCB], start=True, stop=True), reads=[R("nksq"), R("ones32")], writes=[psR[2]])
                        s.op("act", lambda e: e.activation(out=krs[:, 0:NCB], in_=psum[2][:, 0:NCB], func=AF.Sqrt, scale=1.0 / 128, bias=cst[:, 1:2]),
                             reads=[psR[2], R("cst")], writes=[R("nkrs")])
                        s.op("dve", lambda e: e.reciprocal(out=krs[:, 0:NCB], in_=krs[:, 0:NCB]), reads=[R("nkrs")], writes=[R("nkrs")])
                        s.op("dve", lambda e: e.scalar_tensor_tensor(out=kcn[:, hd, 0:NCB], in0=psum[1][:, 0:NCB], scalar=kcg[:, 0:1], in1=krs[:, 0:NCB],
                                                                      op0=ALU.mult, op1=ALU.mult), reads=[psR[1], R("kcg"), R("nkrs")], writes=[R("kcn")])
                    else:
                        for c in range((NCB + 127) // 128):
                            s.op("pe", lambda e: e.matmul(psum[3][:, 0:128], hid[:, a, c * 128:(c + 1) * 128], w2[:, 1, :], start=True, stop=True),
                                 reads=[R("w2"), R("nhid")], writes=[psR[3]])
                            s.op("act", lambda e: e.activation(out=vca[:, c, hd, 0:128], in_=psum[3][:, 0:128], func=AF.Copy), reads=[psR[3]], writes=[R("vca")])
            s.barrier()

        q_in = sbuf("q_in", [128, 8, 128], F32)
        ksw_in = sbuf("ksw_in", [128, 8, 128], F32)
        gl_in = sbuf("gl_in", [128, 24], F32)
        sqb = sbuf("sqb", [128, 8, 128], F32)
        xn = sbuf("xn", [128, 8, 128], F32)
        ss = sbuf("ss", [128, 8], F32)
        rt = sbuf("rt", [128, 4, 8, 16], F32)
        qc_bf = sbuf("qc_bf", [128, 8, 128], BF16)
        qr_bf = sbuf("qr_bf", [128, 8, 128], BF16)
        kr_bf = sbuf("kr_bf", [128, 4, 128], BF16)
        qcT = sbuf("qcT", [128, 8, 128], BF16)
        qrT = sbuf("qrT", [128, 8, 128], BF16)
        pT = sbuf("pT", [128, 3, 512], BF16)
        yb = sbuf("yb", [128, 8, 128], F32)
        tmpo = sbuf("tmpo", [128, 4, 128], F32)
        tmpu = sbuf("tmpu", [128, 4, 64], F32)
        imp = sbuf("imp", [128, 64], F32)
        imp2 = sbuf("imp2", [128, 64], F32)
        m8 = sbuf("m8", [128, 2, 8], F32)
        negsel = sbuf("negsel", [128, 2, 64], F32)
        sqy = sbuf("sqy", [128, 8, 128], F32)
        nsT4 = sbuf("nsT4", [64, 2, 4, 128], BF16)
        rden = sbuf("rden", [128, 3, 4], F32)
        coef = sbuf("coef", [128, 3, 4], F32)
        ss1 = sbuf("ss1", [128, 2], F32)
        yn = sbuf("yn", [128, 8, 128], BF16)
        stg = sbuf("stg", [128, 8, 512], BF16)
        pTrot = Rot([(k, R("npT", k)) for k in range(3)])
        SC = Rot([(0, psR[0]), (1, psR[1])])
        OA = Rot([(2, [psR[2], psR[3]]), (4, [psR[4], psR[5]])])

        def oview(b):
            return psall[:, b * 512:(b + 2) * 512].rearrange("p (g c) -> p g c", g=4)

        def norm_rope(i, src, H, gain, out_c, out_r, rsrc, rout):
            s.op("dve", lambda e: e.tensor_tensor(out=sqb[:, 0:H, :], in0=src, in1=src, op=ALU.mult), reads=[rsrc], writes=[R("nsqb")])
            s.op("dve", lambda e: e.tensor_reduce(out=ss[:, 0:H], in_=sqb[:, 0:H, :], axis=AX.X, op=ALU.add), reads=[R("nsqb")], writes=[R("nss")])
            s.op("act", lambda e: e.activation(out=ss[:, 0:H], in_=ss[:, 0:H], func=AF.Sqrt, scale=1.0 / 128, bias=cst[:, 1:2]), reads=[R("nss"), R("cst")], writes=[R("nss")])
            s.op("dve", lambda e: e.reciprocal(out=ss[:, 0:H], in_=ss[:, 0:H]), reads=[R("nss")], writes=[R("nss")])
            s.op("dve", lambda e: e.tensor_tensor(out=xn[:, 0:H, :], in0=src, in1=ss[:, 0:H].unsqueeze(2).to_broadcast([128, H, 128]), op=ALU.mult),
                 reads=[rsrc, R("nss")], writes=[R("nxn")])
            s.op("dve", lambda e: e.tensor_tensor(out=xn[:, 0:H, :], in0=xn[:, 0:H, :], in1=gain.unsqueeze(1).to_broadcast([128, H, 128]), op=ALU.mult),
                 reads=[R("nxn"), R("nrm_bc")], writes=[R("nxn")])
            if out_c is not None:
                s.op("act", lambda e: e.activation(out=out_c, in_=xn[:, 0:H, :], func=AF.Copy), reads=[R("nxn")], writes=[rout])
            cosb = cs_sb[:, i, 0:16].unsqueeze(1).to_broadcast([128, H, 16])
            sinb = cs_sb[:, i, 16:32].unsqueeze(1).to_broadcast([128, H, 16])
            x1 = xn[:, 0:H, 0:16]
            x2 = xn[:, 0:H, 16:32]
            for k, (a, b) in enumerate(((x1, cosb), (x2, sinb), (x2, cosb), (x1, sinb))):
                s.op("dve", lambda e: e.tensor_tensor(out=rt[:, k, 0:H, :], in0=a, in1=b, op=ALU.mult), reads=[R("nxn"), R("cs")], writes=[R("nrt")])
            s.op("dve", lambda e: e.tensor_tensor(out=out_r[:, :, 0:16], in0=rt[:, 0, 0:H, :], in1=rt[:, 1, 0:H, :], op=ALU.subtract), reads=[R("nrt")], writes=[rout])
            s.op("dve", lambda e: e.tensor_tensor(out=out_r[:, :, 16:32], in0=rt[:, 2, 0:H, :], in1=rt[:, 3, 0:H, :], op=ALU.add), reads=[R("nrt")], writes=[rout])
            s.op("act", lambda e: e.activation(out=out_r[:, :, 32:128], in_=xn[:, 0:H, 32:128], func=AF.Copy), reads=[R("nxn")], writes=[rout])

        def transposes(bank, srcs, rsrc):
            def fn(e):
                ins = None
                for k, a in enumerate(srcs):
                    ins = e.transpose(psb[:, bank * 1024 + k * 128:bank * 1024 + (k + 1) * 128], a, identb[:])
                return ins
            s.op("pe", fn, reads=rsrc + [R("identb")], writes=[psR[bank]])

        def branch(i, kv, kind):
            if kind == "sel":
                js = list(range(0, i + 1))
                kT_, va, bi = ksT, vsa, 1
            else:
                js = list(range(max(0, i - 4), i + 1))
                kT_, va, bi = kwT, vwa, 2
            ob, oR = OA.next()
            ov = oview(ob)
            q4 = qrT[:, kv * 4:(kv + 1) * 4, :].rearrange("p g n -> p (g n)")
            def score(j):
                si, sR = SC.next()
                pairs = [(kT_[:, kv, j * 128:(j + 1) * 128], q4)]
                rd = [R("nkT", kind, j), R("nqrT")]
                if kind == "sel":
                    pairs.append((Efull[:, j * 128:(j + 1) * 128], nsT4[:, kv, :, :].rearrange("p g n -> p (g n)")))
                    rd += [R("Efull"), R("nnsT4", kv)]
                if j == i:
                    pairs.append((identb[:], negc4[:].rearrange("p g n -> p (g n)")))
                    rd += [R("identb"), R("negc4")]
                if kind == "win" and j == i - 4:
                    pairs.append((identb[:], negu4[:].rearrange("p g n -> p (g n)")))
                    rd += [R("identb"), R("negu4")]
                mm_group(s, psum[si][:], pairs, reads=rd, writes=[sR])
                return si, sR

            def finish(j, si, sR):
                pi, pR = pTrot.next()
                s.op("act", lambda e: e.activation(out=pT[:, pi, :], in_=psum[si][:], func=AF.Exp, scale=SCALE), reads=[sR], writes=[pR])

                def pv(e):
                    ins = None
                    for gq in range(4):
                        ins = e.matmul(ov[:, gq, 0:129], pT[:, pi, gq * 128:(gq + 1) * 128], va[:, j, kv, :], start=(j == js[0] and gq % 2 == 0), stop=(j == js[-1] and gq % 2 == 1))
                    return ins
                s.op("pe", pv, reads=[pR, R("nva", kind, j), R("vsa1" if kind == "sel" else "vwa1")], writes=oR)

            nxt = score(js[0])
            for idx, j in enumerate(js):
                cur = nxt
                if idx + 1 < len(js):
                    nxt = score(js[idx + 1])
                finish(j, *cur)
            s.op("dve", lambda e: e.reciprocal(out=rden[:, bi, :], in_=ov[:, :, 128]), reads=oR, writes=[R("nrden", bi)])
            s.op("dve", lambda e: e.tensor_tensor(out=coef[:, bi, :], in0=gates[:, i, kv * 12 + bi:kv * 12 + 12:3], in1=rden[:, bi, :], op=ALU.mult),
                 reads=[R("ngates", i), R("nrden", bi)], writes=[R("ncoef", bi)])
            s.op("dve", lambda e: e.tensor_tensor(out=tmpo[:], in0=ov[:, :, 0:128], in1=coef[:, bi, :].unsqueeze(2).to_broadcast([128, 4, 128]), op=ALU.mult),
                 reads=oR + [R("ncoef", bi)], writes=[R("ntmpo")])
            s.op("dve", lambda e: e.tensor_tensor(out=yb[:, kv * 4:(kv + 1) * 4, :], in0=yb[:, kv * 4:(kv + 1) * 4, :], in1=tmpo[:], op=ALU.add),
                 reads=[R("ntmpo"), R("nyb")], writes=[R("nyb")])

        def prep_dve(i):
            rows = slice(i * 128, (i + 1) * 128)
            tt_i = i // 4
            s.dma(q_in[:].rearrange("p h d -> p (h d)"), g["q_tm"].ap()[rows, :], reads=[R("q_tm", tt_i)], writes=[R("nq_in")])
            s.dma(ksw_in[:].rearrange("p h d -> p (h d)"), g["ksw_tm"].ap()[rows, :], reads=[R("ksw_tm", tt_i)], writes=[R("nksw_in")])
            s.dma(gl_in[:], g["gkg_tm"].ap()[rows, 256:280], reads=[R("gkg_tm", tt_i)], writes=[R("ngl_in")])
            s.op("dve", lambda e: e.tensor_tensor(out=gl_in[:], in0=gl_in[:], in1=gb_bc[:], op=ALU.add), reads=[R("ngl_in"), R("gb_bc")], writes=[R("ngl_in")])
            s.op("act", lambda e: e.activation(out=gates[:, i, :], in_=gl_in[:], func=AF.Sigmoid), reads=[R("ngl_in")], writes=[R("ngates", i)])
            norm_rope(i, q_in[:], 8, nrm_bc[:, 0, :], qc_bf[:], qr_bf[:], R("nq_in"), R("nqbf"))
            norm_rope(i, ksw_in[:, 0:2, :], 2, nrm_bc[:, 2, :], None, kr_bf[:, 0:2, :], R("nksw_in"), R("nkrbf"))
            norm_rope(i, ksw_in[:, 4:6, :], 2, nrm_bc[:, 3, :], None, kr_bf[:, 2:4, :], R("nksw_in"), R("nkrbf"))
            s.op("dve", lambda e: e.tensor_copy(out=vsa[:, i, :, 0:128], in_=ksw_in[:, 2:4, :]), reads=[R("nksw_in")], writes=[R("nva", "sel", i)])
            s.op("dve", lambda e: e.tensor_copy(out=vwa[:, i, :, 0:128], in_=ksw_in[:, 6:8, :]), reads=[R("nksw_in")], writes=[R("nva", "win", i)])

        def prep_pe(i):
            rows = slice(i * 128, (i + 1) * 128)
            transposes(6, [qc_bf[:, h, :] for h in range(8)], [R("nqbf")])
            s.op("act", lambda e: e.activation(out=qcT[:].rearrange("p h n -> p (h n)"), in_=psb[:, 6 * 1024:7 * 1024], func=AF.Copy), reads=[psR[6]], writes=[R("nqcT")])
            transposes(7, [qr_bf[:, h, :] for h in range(8)], [R("nqbf")])
            s.op("dve", lambda e: e.tensor_copy(out=qrT[:].rearrange("p h n -> p (h n)"), in_=psb[:, 7 * 1024:8 * 1024]), reads=[psR[7]], writes=[R("nqrT")])
            transposes(6, [kr_bf[:, h, :] for h in range(4)], [R("nkrbf")])
            s.op("act", lambda e: e.activation(out=ksT[:, :, rows], in_=psb[:, 6 * 1024:6 * 1024 + 256].rearrange("p (h n) -> p h n", h=2), func=AF.Copy),
                 reads=[psR[6]], writes=[R("nkT", "sel", i)])
            s.op("act", lambda e: e.activation(out=kwT[:, :, rows], in_=psb[:, 6 * 1024 + 256:6 * 1024 + 512].rearrange("p (h n) -> p h n", h=2), func=AF.Copy),
                 reads=[psR[6]], writes=[R("nkT", "win", i)])

        def cmp_topk(i, kv):
            rows = slice(i * 128, (i + 1) * 128)
            n_max = min(NCB - 1, 8 * i + 6)
            ncc = n_max // 128 + 1
            ob, oR = OA.next()
            ov = oview(ob)
            q4c = qcT[:, kv * 4:(kv + 1) * 4, :].rearrange("p g n -> p (g n)")
            for c in range(ncc):
                si, sR = SC.next()
                s.op("pe", lambda e: e.matmul(psum[si][:], kcn[:, kv, c * 128:(c + 1) * 128], q4c, start=True, stop=True), reads=[R("kcn"), R("nqcT")], writes=[sR])
                pi, pR = pTrot.next()
                s.op("act", lambda e: e.activation(out=pT[:, pi, :], in_=psum[si][:], func=AF.Exp, scale=SCALE), reads=[sR], writes=[pR])
                s.op("dve", lambda e: e.tensor_tensor(out=pT[:, pi, :].rearrange("p (g n) -> p g n", g=4), in0=pT[:, pi, :].rearrange("p (g n) -> p g n", g=4),
                                                      in1=cmask[:, c, rows].unsqueeze(1).to_broadcast([128, 4, 128]), op=ALU.mult),
                     reads=[pR, R("cmask")], writes=[pR])

                def pvc(e):
                    ins = None
                    for gq in range(4):
                        ins = e.matmul(ov[:, gq, 0:193], pT[:, pi, gq * 128:(gq + 1) * 128], vca[:, c, kv, :], start=(c == 0 and gq % 2 == 0), stop=(c == ncc - 1 and gq % 2 == 1))
                    return ins
                s.op("pe", pvc, reads=[pR, R("vca")], writes=oR)
            s.op("dve", lambda e: e.tensor_scalar(out=rden[:, 0, :], in0=ov[:, :, 128], scalar1=1e-30, scalar2=None, op0=ALU.max), reads=oR, writes=[R("nrden", 0)])
            s.op("dve", lambda e: e.reciprocal(out=rden[:, 0, :], in_=rden[:, 0, :]), reads=[R("nrden", 0)], writes=[R("nrden", 0)])
            s.op("dve", lambda e: e.tensor_tensor(out=tmpu[:], in0=ov[:, :, 129:193], in1=rden[:, 0, :].unsqueeze(2).to_broadcast([128, 4, 64]), op=ALU.mult),
                 reads=oR + [R("nrden", 0)], writes=[R("ntmpu")])
            s.op("dve", lambda e: e.tensor_reduce(out=imp[:], in_=tmpu[:].rearrange("p g s -> p s g"), axis=AX.X, op=ALU.add), reads=[R("ntmpu")], writes=[R("nimp")])
            s.op("dve", lambda e: e.tensor_tensor(out=coef[:, 0, :], in0=gates[:, i, kv * 12:kv * 12 + 12:3], in1=rden[:, 0, :], op=ALU.mult),
                 reads=[R("ngates", i), R("nrden", 0)], writes=[R("ncoef", 0)])
            s.op("dve", lambda e: e.tensor_tensor(out=yb[:, kv * 4:(kv + 1) * 4, :], in0=ov[:, :, 0:128], in1=coef[:, 0, :].unsqueeze(2).to_broadcast([128, 4, 128]), op=ALU.mult),
                 reads=oR + [R("ncoef", 0)], writes=[R("nyb")])
            s.op("dve", lambda e: e.tensor_tensor(out=imp[:], in0=imp[:], in1=fmvm[:, i, 64:128], op=ALU.mult), reads=[R("nimp"), R("fmvm")], writes=[R("nimp")])
            s.op("dve", lambda e: e.tensor_tensor(out=imp[:], in0=imp[:], in1=fmvm[:, i, 0:64], op=ALU.add), reads=[R("nimp"), R("fmvm")], writes=[R("nimp")])
            s.op("dve", lambda e: e.max(out=m8[:, 0, :], in_=imp[:]), reads=[R("nimp")], writes=[R("nm8")])
            s.op("dve", lambda e: e.match_replace(out=imp2[:], in_to_replace=m8[:, 0, :], in_values=imp[:], imm_value=-3.0e38), reads=[R("nimp"), R("nm8")], writes=[R("nimp2")])
            s.op("dve", lambda e: e.max(out=m8[:, 1, :], in_=imp2[:]), reads=[R("nimp2")], writes=[R("nm8")])
            s.op("dve", lambda e: e.tensor_scalar(out=negsel[:, kv, :], in0=imp[:], scalar1=m8[:, 1, 7:8], scalar2=1.0, op0=ALU.is_ge, op1=ALU.subtract),
                 reads=[R("nimp"), R("nm8")], writes=[R("nnegsel", kv)])

        def sel_mask_T(kv):
            s.op("pe", lambda e: e.transpose(psum[7][0:64, 0:128], negsel[:, kv, :], c128[:, 0, :]), reads=[R("nnegsel", kv), R("c128")], writes=[psR[7]])
            s.op("act", lambda e: e.activation(out=nsT4[:, kv, :, :], in_=psum[7][0:64, 0:128].unsqueeze(1).to_broadcast([64, 4, 128]), func=AF.Copy),
                 reads=[psR[7]], writes=[R("nnsT4", kv)])

        def out_tile(i):
            tt_i = i // 4
            s.op("dve", lambda e: e.tensor_tensor(out=sqy[:], in0=yb[:], in1=yb[:], op=ALU.mult), reads=[R("nyb")], writes=[R("nsqy")])
            s.op("dve", lambda e: e.tensor_reduce(out=ss1[:, 0:1], in_=sqy[:].rearrange("p h d -> p (h d)"), axis=AX.X, op=ALU.add), reads=[R("nsqy")], writes=[R("nss1")])
            s.op("act", lambda e: e.activation(out=ss1[:, 0:1], in_=ss1[:, 0:1], func=AF.Sqrt, scale=1.0 / 1024, bias=cst[:, 1:2]), reads=[R("nss1"), R("cst")], writes=[R("nss1")])
            s.op("dve", lambda e: e.reciprocal(out=ss1[:, 0:1], in_=ss1[:, 0:1]), reads=[R("nss1")], writes=[R("nss1")])
            s.op("dve", lambda e: e.scalar_tensor_tensor(out=yn[:].rearrange("p h d -> p (h d)"), in0=yb[:].rearrange("p h d -> p (h d)"), scalar=ss1[:, 0:1], in1=outn[:],
                                                          op0=ALU.mult, op1=ALU.mult), reads=[R("nyb"), R("nss1"), R("outn")], writes=[R("nyn")])
            transposes(6, [yn[:, h, :] for h in range(8)], [R("nyn")])
            ci = i % 4
            s.op("act", lambda e: e.activation(out=stg[:, :, ci * 128:(ci + 1) * 128], in_=psb[:, 6 * 1024:7 * 1024].rearrange("p (h n) -> p h n", h=8), func=AF.Copy),
                 reads=[psR[6]], writes=[R("nstg")])
            if ci == 3:
                tok = slice(tt_i * 512, (tt_i + 1) * 512)
                s.dma(g["mixT"].ap()[512:1536, tok].rearrange("(h p) n -> p h n", p=128), stg[:], reads=[R("nstg")], writes=[R("mixT", tt_i)], q="act")

        jobs = g["cast_jobs"](l + 1) if l + 1 < P.L else []
        per_tile = (len(jobs) + NQ - 1) // NQ if jobs else 0
        prep_dve(0)
        prep_pe(0)
        for i in range(NQ):
            for job in jobs[i * per_tile:(i + 1) * per_tile]:
                g["cast_job"](job)
            for kv in range(2):
                cmp_topk(i, kv)
            if i + 1 < NQ:
                prep_dve(i + 1)
            for kv in range(2):
                branch(i, kv, "win")
                sel_mask_T(kv)
                branch(i, kv, "sel")
            out_tile(i)
            if i + 1 < NQ:
                prep_pe(i + 1)


def tt_phase(P, s, g, body, sfx):
    nc = P.nc
    T, L = P.T, P.L
    NT = T // NTOK
    psum, psR = g["psum"], g["psR"]
    w16 = g["w16"]
    ones32, gains_sb = g["ones32"], g["gains_sb"]

    with ExitStack() as ts:
        def sb(name, shape, dt):
            return ts.enter_context(nc.sbuf_tensor(name + sfx, list(shape), dt))

        x_sb = sb("x_sb", [128, KC, NTOK], F32)
        h_sb = sb("h_sb", [128, KC, NTOK], BF16)
        act_sb = sb("act_sb", [128, NFF, NTOK], BF16)
        wbuf = sb("wbuf", [128, 4, 8192], BF16)
        sq_sb = sb("sq_sb", [128, 2, NTOK], F32)
        rstd_sb = sb("rstd_sb", [128, NTOK], F32)
        sg_sb = sb("sg_sb", [128, 2, NTOK], F32)
        stg_sb = sb("stg_sb", [128, 3, NTOK], F32)
        stgb_sb = sb("stgb_sb", [128, 3, NTOK], BF16)
        xR = [P.R("x_sb", i) for i in range(KC)]
        hRs = [P.R("h_sb", i) for i in range(KC)]
        actR = [P.R("act_sb", i) for i in range(NFF)]
        wrot = Rot([(i, P.R("wbuf", i)) for i in range(4)])
        sqrot = Rot([(i, P.R("sq", i)) for i in range(2)])
        sgrot = Rot([(i, P.R("sg", i)) for i in range(2)])
        stgrot = Rot([(i, P.R("stg", i)) for i in range(3)])
        stgbrot = Rot([(i, P.R("stgb", i)) for i in range(3)])
        rstdR = P.R("rstd")
        PG = Rot([(0, psR[0]), (1, psR[1])])
        PU = Rot([(2, psR[2]), (3, psR[3])])
        PO = Rot([(4, psR[4]), (5, psR[5])])
        PSTAT = (6, psR[6])

        def load_w(nm, l, pc, nel):
            bi, bR = wrot.next()
            s.dma(wbuf[:, bi, 0:nel], w16[nm].ap()[l, pc], reads=[P.R("w", nm, l, pc)], writes=[bR])
            return bi, bR

        def stats_act(kc):
            qi, qR = sqrot.next()
            s.op("act", lambda e: e.activation(out=sq_sb[:, qi, :], in_=x_sb[:, kc, :], func=AF.Square),
                 reads=[xR[kc]], writes=[qR])
            return kc, qi, qR

        def stats_pe(job):
            kc, qi, qR = job
            pi, pR = PSTAT
            s.op("pe", lambda e: e.matmul(psum[pi][:], ones32[:], sq_sb[:, qi, :], start=(kc == 0), stop=(kc == KC - 1)),
                 reads=[qR, P.R("ones32")], writes=[pR])

        def norm(l, which, pre=False):
            pi, pR = PSTAT
            if not pre:
                for kc in range(KC):
                    stats_pe(stats_act(kc))
            s.op("act", lambda e: e.activation(out=rstd_sb[:], in_=psum[pi][:], func=AF.Sqrt, scale=1.0 / D, bias=eps_sb[:, 0:1]),
                 reads=[pR, P.R("eps")], writes=[rstdR])
            s.op("dve", lambda e: e.reciprocal(out=rstd_sb[:], in_=rstd_sb[:]), reads=[rstdR], writes=[rstdR])
            gbase = (l * 3 + which) * KC
            for kc in range(KC):
                s.op("dve", lambda e: e.scalar_tensor_tensor(out=h_sb[:, kc, :], in0=x_sb[:, kc, :],
                                                              scalar=gains_sb[:, gbase + kc:gbase + kc + 1], in1=rstd_sb[:],
                                                              op0=ALU.mult, op1=ALU.mult),
                     reads=[xR[kc], rstdR, P.R("gains")], writes=[hRs[kc]])

        eps_sb = sb("eps_sb", [128, 1], F32)
        s.op("dve", lambda e: e.memset(eps_sb[:], EPS), writes=[P.R("eps")])

        def ffn(l, which, pre=False, nxt=False):
            nm_gu = "wgu%d" % which
            nm_d = "wd%d" % which
            norm(l, 0 if which == 1 else 2, pre)
            for f in range(NFF):
                bi, bR = load_w(nm_gu, l, f, 2 * KC * 128)
                gi, gR = PG.next()
                ui, uR = PU.next()
                mm_group(s, psum[gi][:], [(wbuf[:, bi, kc * 128:(kc + 1) * 128], h_sb[:, kc, :]) for kc in range(KC)],
                         reads=[bR] + hRs, writes=[gR])
                mm_group(s, psum[ui][:], [(wbuf[:, bi, (KC + kc) * 128:(KC + kc + 1) * 128], h_sb[:, kc, :]) for kc in range(KC)],
                         reads=[bR] + hRs, writes=[uR])
                si, sR = sgrot.next()
                s.op("act", lambda e: e.activation(out=sg_sb[:, si, :], in_=psum[gi][:], func=AF.Silu), reads=[gR], writes=[sR])
                s.op("dve", lambda e: e.tensor_tensor(out=act_sb[:, f, :], in0=sg_sb[:, si, :], in1=psum[ui][:], op=ALU.mult),
                     reads=[sR, uR], writes=[actR[f]])
            pend = None
            for m in range(KC):
                bi, bR = load_w(nm_d, l, m, NFF * 128)
                oi, oR = PO.next()
                mm_group(s, psum[oi][:], [(wbuf[:, bi, fc * 128:(fc + 1) * 128], act_sb[:, fc, :]) for fc in range(NFF)],
                         reads=[bR] + actR, writes=[oR])
                if pend is not None:
                    stats_pe(pend)
                    pend = None
                s.op("dve", lambda e: e.scalar_tensor_tensor(out=x_sb[:, m, :], in0=psum[oi][:], scalar=0.5, in1=x_sb[:, m, :],
                                                              op0=ALU.mult, op1=ALU.add),
                     reads=[oR, xR[m]], writes=[xR[m]])
                if nxt:
                    pend = stats_act(m)
            if pend is not None:
                stats_pe(pend)

        def wout(l, t, nxt=False):
            tok = slice(t * NTOK, (t + 1) * NTOK)
            s.dma(h_sb[:], g["mixT"].ap()[:, tok].rearrange("(kc p) n -> p kc n", p=128),
                  reads=[P.R("mixT", t)], writes=hRs)
            pend = None
            for m in range(KC):
                bi, bR = load_w("wout", l, m, KC * 128)
                oi, oR = PO.next()
                mm_group(s, psum[oi][:], [(wbuf[:, bi, kc * 128:(kc + 1) * 128], h_sb[:, kc, :]) for kc in range(KC)],
                         reads=[bR] + hRs, writes=[oR])
                if pend is not None:
                    stats_pe(pend)
                    pend = None
                s.op("dve", lambda e: e.tensor_tensor(out=x_sb[:, m, :], in0=psum[oi][:], in1=x_sb[:, m, :], op=ALU.add),
                     reads=[oR, xR[m]], writes=[xR[m]])
                if nxt:
                    pend = stats_act(m)
            if pend is not None:
                stats_pe(pend)

        cm_dst = ([("lruT", 128 * i, 128, F32) for i in range(8)] + [("kvcT", 128 * i, 128, BF16) for i in range(4)]
                  + [("gqkT", 128 * i, 128, F32) for i in range(4)] + [("gaT", 0, 16, BF16)])
        tm_dst = [("q_tm", 0, F32), ("q_tm", 512, F32), ("ksw_tm", 0, F32), ("ksw_tm", 512, F32), ("gkg_tm", 0, F32),
                  ("gv_tm", 0, BF16), ("gg_tm", 0, F32)]

        def proj(l, t, pre=False):
            tok = slice(t * NTOK, (t + 1) * NTOK)
            norm(l, 1, pre)
            pieces = [("wincm", c, KC * 128) for c in range(WIN_CM)] + [("wintm", pc, KC * 512) for pc in range(WIN_TM)]
            loaded = {}

            def ensure(k):
                if k < len(pieces) and k not in loaded:
                    loaded[k] = load_w(pieces[k][0], l, pieces[k][1], pieces[k][2])
            ensure(0)
            ensure(1)
            for c in range(WIN_CM):
                ensure(c + 2)
                bi, bR = loaded[c]
                name, r0, nr, dt = cm_dst[c]
                oi, oR = PO.next()
                mm_group(s, psum[oi][0:nr, :], [(wbuf[:, bi, kc * 128:kc * 128 + nr], h_sb[:, kc, :]) for kc in range(KC)],
                         reads=[bR] + hRs, writes=[oR])
                if dt == F32:
                    gi, gR = stgrot.next()
                    dst_sb = stg_sb[0:nr, gi, :]
                else:
                    gi, gR = stgbrot.next()
                    dst_sb = stgb_sb[0:nr, gi, :]
                s.op("act", lambda e: e.activation(out=dst_sb, in_=psum[oi][0:nr, :], func=AF.Copy), reads=[oR], writes=[gR])
                s.dma(g[name].ap()[r0:r0 + nr, tok], dst_sb, reads=[gR], writes=[P.R(name, t)], q="act")
            for pc in range(WIN_TM):
                ensure(WIN_CM + pc + 2)
                bi, bR = loaded[WIN_CM + pc]
                name, c0, dt = tm_dst[pc]
                for sub in range(NTOK // 128):
                    oi, oR = PO.next()
                    mm_group(s, psum[oi][:], [(h_sb[:, kc, sub * 128:(sub + 1) * 128], wbuf[:, bi, kc * 512:(kc + 1) * 512])
                                               for kc in range(KC)], reads=[bR] + hRs, writes=[oR])
                    if dt == F32:
                        gi, gR = stgrot.next()
                        dst_sb = stg_sb[:, gi, :]
                    else:
                        gi, gR = stgbrot.next()
                        dst_sb = stgb_sb[:, gi, :]
                    eng = "act" if sub % 2 == 0 else "dve"
                    if eng == "act":
                        s.op("act", lambda e: e.activation(out=dst_sb, in_=psum[oi][:], func=AF.Copy), reads=[oR], writes=[gR])
                    else:
                        s.op("dve", lambda e: e.tensor_copy(out=dst_sb, in_=psum[oi][:]), reads=[oR], writes=[gR])
                    r0 = t * NTOK + sub * 128
                    s.dma(g[name].ap()[r0:r0 + 128, c0:c0 + 512], dst_sb, reads=[gR], writes=[P.R(name, t)], q="act")

        def load_x(src, t, srcname):
            tok = slice(t * NTOK, (t + 1) * NTOK)
            for kc in range(KC):
                s.dma(x_sb[:, kc, :], src.ap()[kc * 128:(kc + 1) * 128, tok], reads=[P.R(srcname, t, kc)], writes=[xR[kc]])

        def store_x(dst, t, dstname):
            tok = slice(t * NTOK, (t + 1) * NTOK)
            for kc in range(KC):
                s.dma(dst.ap()[kc * 128:(kc + 1) * 128, tok], x_sb[:, kc, :], reads=[xR[kc]], writes=[P.R(dstname, t, kc)], q="act")

        body(dict(load_x=load_x, store_x=store_x, ffn=ffn, proj=proj, wout=wout))


CM_COLS = ([list(range(128 * i, 128 * (i + 1))) for i in range(8)]
           + [list(range(2048 + 128 * i, 2048 + 128 * (i + 1))) for i in range(4)]
           + [list(range(3608 + 128 * i, 3608 + 128 * (i + 1))) for i in range(4)]
           + [list(range(5144, 5160)) + [-1] * 112])
TM_COLS = [list(range(1024, 1536)), list(range(1536, 2048)), list(range(2560, 3072)), list(range(3072, 3584)),
           list(range(3864, 4120)) + list(range(3584, 3608)) + [-1] * 232,
           list(range(4120, 4632)), list(range(4632, 5144))]


def _tile_cols(W, cols):
    cols = np.asarray(cols)
    Wz = np.concatenate([W, np.zeros((W.shape[0], 1), W.dtype)], axis=1)
    sel = Wz[:, cols]
    return sel.reshape(KC, 128, len(cols)).transpose(1, 0, 2)


def prep_weights(inp, L):
    out = {}
    g = np.stack([inp["ffn1_norm"][:L], inp["mix_norm"][:L], inp["ffn2_norm"][:L]], axis=1)
    out["gains"] = np.ascontiguousarray(g.reshape(L, 3, KC, 128).transpose(3, 0, 1, 2).reshape(128, L * 3 * KC))
    ffn_w = {1: (inp["ffn1_w_gate"], inp["ffn1_w_up"], inp["ffn1_w_down"]),
             2: (inp["ffn2_w_gate"], inp["ffn2_w_up"], inp["ffn2_w_down"])}
    for which in (1, 2):
        wg = ffn_w[which][0][:L].reshape(L, KC, 128, NFF, 128)
        wu = ffn_w[which][1][:L].reshape(L, KC, 128, NFF, 128)
        gu = np.stack([wg, wu], axis=1)
        out["wgu%d" % which] = np.ascontiguousarray(gu.transpose(0, 4, 3, 1, 2, 5)).reshape(L, NFF, 128, 2 * KC * 128)
        wd = ffn_w[which][2][:L].reshape(L, NFF, 128, KC, 128)
        out["wd%d" % which] = np.ascontiguousarray(wd.transpose(0, 3, 2, 1, 4)).reshape(L, KC, 128, NFF * 128)
    win = inp["w_in"][:L]
    out["wincm"] = np.stack([np.stack([_tile_cols(win[l], c) for c in CM_COLS]) for l in range(L)]).reshape(L, WIN_CM, 128, KC * 128)
    out["wintm"] = np.stack([np.stack([_tile_cols(win[l], c) for c in TM_COLS]) for l in range(L)]).reshape(L, WIN_TM, 128, KC * 512)
    wo = inp["w_out"][:L].reshape(L, KC, 128, KC, 128)
    out["wout"] = np.ascontiguousarray(wo.transpose(0, 3, 2, 1, 4)).reshape(L, KC, 128, KC * 128)
    return {k: np.ascontiguousarray(v, dtype=np.float32) for k, v in out.items()}


def make_consts(T):
    c = np.zeros((128, 6, 128), np.float32)
    i = np.arange(128)
    c[:, 0, :] = np.eye(128)
    c[:, 1, :] = np.where(i[:, None] <= i[None, :], -1.0 / 16.0, 0.0)
    c[:, 2, :] = np.where(i[:, None] > i[None, :], -1.0 / 16.0, 0.0)
    c[:, 3, :] = np.where(i[:, None] <= i[None, :], 1.0, 0.0)
    c[:, 4, :] = np.where(i[:, None] <= i[None, :], 0.0, -30000.0)
    c[:, 5, :] = np.where(i[:, None] > i[None, :], 0.0, -30000.0)
    out = {"c128": c}
    NCB = (T - 32) // 16 + 1
    NS = T // 64
    t = np.arange(T)
    n = np.arange(256)
    out["cmaskT"] = ((n[:, None] < NCB) & (16 * n[:, None] + 31 <= t[None, :])).astype(np.float32)
    sidx = np.arange(64)
    out["Efull"] = np.where((t[None, :] // 64) == sidx[:, None], 30000.0, 0.0).astype(np.float32)
    inv = (np.float32(1.0) / (np.float32(500000.0) ** (np.arange(0, 32, 2, dtype=np.float32) / np.float32(32)))).astype(np.float32)
    ang = (t.astype(np.float32)[:, None] * inv[None, :]).astype(np.float32)
    out["ropecs"] = np.concatenate([np.cos(ang), np.sin(ang)], axis=1).astype(np.float32)
    cur = t // 64
    forced = (sidx[None, :] == 0) | (sidx[None, :] == cur[:, None]) | (sidx[None, :] == cur[:, None] - 1)
    valid = (sidx[None, :] * 64 <= t[:, None]) & (sidx[None, :] < NS)
    forced = forced & valid
    fm = np.where(forced, 1e30, np.where(valid, 0.0, -1e30))
    vm = (valid & ~forced).astype(np.float32)
    out["fmvm"] = np.concatenate([fm, vm], axis=1).astype(np.float32)
    cstart = n * 16
    sstart = sidx * 64
    ov = (n[:, None] < NCB) & (sidx[None, :] < NS) & (cstart[:, None] < sstart[None, :] + 64) & (cstart[:, None] + 32 > sstart[None, :])
    out["ovl"] = ov.astype(np.float32)
    return out


def prep_small(inp, L):
    out = {}
    lv = np.zeros((L, 128, 4, 9), np.float32)

    def cp(v):
        return v.reshape(L, 4, 128).transpose(0, 2, 1)
    for k in range(4):
        lv[..., k] = cp(inp["lru_conv_w"][:L, k])
    lv[..., 4] = cp(inp["lru_conv_b"][:L])
    lv[..., 5] = cp(inp["lru_gate_a_b"][:L])
    lv[..., 6] = cp(inp["lru_gate_x_b"][:L])
    lv[..., 7] = cp(inp["lru_lambda"][:L])
    lv[..., 8] = cp(inp["lru_out_norm"][:L])
    out["lruv"] = lv
    gw = np.zeros((L, 2, 4, 128, 128), np.float32)
    for a, nm in enumerate(("lru_gate_a_w", "lru_gate_x_w")):
        w = inp[nm][:L]
        for c in range(4):
            gw[:, a, c, 0:64, 0:64] = w[:, 2 * c]
            gw[:, a, c, 64:128, 64:128] = w[:, 2 * c + 1]
    out["lrug"] = gw
    out["glaw"] = np.concatenate([inp["gla_a_w2"][:L], inp["gla_a_b"][:L, None, :]], axis=1)
    out["glan"] = inp["gla_out_norm"][:L]
    out["nsan"] = np.stack([inp["nsa_q_norm"][:L], inp["nsa_k_cmp_norm"][:L], inp["nsa_k_sel_norm"][:L], inp["nsa_k_win_norm"][:L]], axis=1)
    out["nsaon"] = inp["nsa_out_norm"][:L]
    out["nsagb"] = inp["nsa_gate_b"][:L]
    out["cmpw1"] = np.stack([inp[k][:L].reshape(L, 32, 128, 128).transpose(0, 2, 1, 3).reshape(L, 128, 4096)
                             for k in ("nsa_cmp_w1_k", "nsa_cmp_w1_v")], axis=1)
    out["cmpw2"] = np.stack([inp["nsa_cmp_w2_k"][:L], inp["nsa_cmp_w2_v"][:L]], axis=1)
    out["cmppe"] = np.stack([inp["nsa_cmp_pe_k"][:L].transpose(0, 2, 1), inp["nsa_cmp_pe_v"][:L].transpose(0, 2, 1)], axis=1)
    return {k: np.ascontiguousarray(v, dtype=np.float32) for k, v in out.items()}


_CACHE = {}


def kernel(**inputs):
    inp = {k: np.asarray(v) for k, v in inputs.items()}
    x = inp["x"]
    B, T, _ = x.shape
    L = inp["w_in"].shape[0]
    key = (T, L)
    if key not in _CACHE:
        _CACHE[key] = build(T, L)
    nc = _CACHE[key]
    shared = {}
    shared.update(prep_weights(inp, L))
    shared.update(prep_small(inp, L))
    shared.update(make_consts(T))
    in_maps = []
    for b in range(B):
        m = dict(shared)
        m["xT"] = np.ascontiguousarray(x[b].T)
        in_maps.append(m)
    res = run_bass_kernel_spmd(nc, in_maps, core_ids=list(range(B)))
    out = np.stack([np.asarray(r["outT"]).T for r in res.results], axis=0)
    return np.ascontiguousarray(out.astype(np.float32))
```

```python
from contextlib import ExitStack
import numpy as np
import concourse.bass as bass
import concourse.mybir as mybir
from concourse.bass_utils import run_bass_kernel_spmd

F32 = mybir.dt.float32
BF16 = mybir.dt.bfloat16
AF = mybir.ActivationFunctionType
ALU = mybir.AluOpType
AX = mybir.AxisListType

D = 2048
DFF = 5632
NFF = DFF // 128
KC = D // 128
EPS = 1e-6
NTOK = 512


class Res:
    __slots__ = ("w", "r")

    def __init__(self):
        self.w = None
        self.r = {}


class Sched:
    def __init__(self, nc, es, ndma=40):
        self.nc = nc
        self.eng = {"pe": nc.tensor, "act": nc.scalar, "dve": nc.vector, "pool": nc.gpsimd, "sp": nc.sync}
        self.sem = {}
        self.cnt = {}
        self.seen = {e: {} for e in self.eng}
        for e in ("pe", "act", "dve", "pool"):
            self.sem[("E", e)] = es.enter_context(nc.semaphore("sem_" + e))
            self.cnt[e] = 0
        self.ndma = {"sp": ndma, "pool": 8, "act": 8}
        for q, n in self.ndma.items():
            for i in range(n):
                self.sem[("D", q, i)] = es.enter_context(nc.semaphore("dsem_%s%d" % (q, i)))
        self.dma_i = {"sp": 0, "pool": 0, "act": 0}
        self.dlast = {}

    def _waits(self, eng, reads, writes):
        need = {}
        seen = self.seen[eng]
        own = ("E", eng)

        def add(k, v):
            if seen.get(k, 0) >= v:
                return
            if need.get(k, 0) < v:
                need[k] = v

        for r in reads:
            if r.w is not None:
                k, v = r.w
                if not (k == own and eng == "pe"):
                    add(k, v)
        pe_own = (eng == "pe")
        for w in writes:
            if w.w is not None and not (pe_own and w.w[0] == own):
                add(*w.w)
            for k, v in w.r.items():
                if not (pe_own and k == own):
                    add(k, v)
        return need

    def _emit_waits(self, eng, need):
        e = self.eng[eng]
        for k, v in need.items():
            e.wait_ge(self.sem[k], v)
            self.seen[eng][k] = v

    def op(self, eng, fn, reads=(), writes=()):
        need = self._waits(eng, reads, writes)
        self._emit_waits(eng, need)
        ins = fn(self.eng[eng])
        self.cnt[eng] += 1
        k = ("E", eng)
        v = self.cnt[eng]
        ins.then_inc(self.sem[k], 1)
        for r in reads:
            r.r[k] = v
        for w in writes:
            w.w = (k, v)
            w.r = {}

    def dma(self, out, in_, reads=(), writes=(), q="sp"):
        i = self.dma_i[q]
        self.dma_i[q] += 1
        slot = i % self.ndma[q]
        rnd = i // self.ndma[q]
        need = self._waits(q, reads, writes)
        k = ("D", q, slot)
        if rnd > 0 and self.seen[q].get(k, 0) < 16 * rnd:
            need[k] = max(need.get(k, 0), 16 * rnd)
        self._emit_waits(q, need)
        ins = self.eng[q].dma_start(out=out, in_=in_)
        v = 16 * (rnd + 1)
        ins.then_inc(self.sem[k], 16)
        self.dlast[k] = v
        for r in reads:
            r.r[k] = v
        for w in writes:
            w.w = (k, v)
            w.r = {}

    def barrier(self, include_pool=False):
        for eng in self.eng:
            need = {}
            for e2 in ("pe", "act", "dve", "pool"):
                k = ("E", e2)
                if e2 != eng and self.cnt[e2] > self.seen[eng].get(k, 0):
                    need[k] = self.cnt[e2]
            for k, v in self.dlast.items():
                if k[1] == "pool" and not include_pool:
                    continue
                if v > self.seen[eng].get(k, 0):
                    need[k] = v
            self._emit_waits(eng, need)


class Rot:
    def __init__(self, items):
        self.items = items
        self.i = 0

    def next(self):
        it = self.items[self.i % len(self.items)]
        self.i += 1
        return it


def mm_group(s, out, pairs, reads, writes, **kw):
    n = len(pairs)

    def fn(e):
        ins = None
        for i, (a, b) in enumerate(pairs):
            ins = e.matmul(out, a, b, start=(i == 0), stop=(i == n - 1), **kw)
        return ins

    s.op("pe", fn, reads=reads, writes=writes)


class Prog:
    def __init__(self, T, L, debug=()):
        self.T = T
        self.L = L
        self.debug = set(debug)
        self.nc = bass.Bass("TRN2", target_bir_lowering=False)
        self.dram = {}
        self.res = {}

    def din(self, name, shape, dt=F32):
        t = self.nc.dram_tensor(name, list(shape), dt, kind="ExternalInput")
        self.dram[name] = t
        return t

    def dout(self, name, shape, dt=F32):
        t = self.nc.dram_tensor(name, list(shape), dt, kind="ExternalOutput")
        self.dram[name] = t
        return t

    def dscr(self, name, shape, dt=F32):
        kind = "ExternalOutput" if name in self.debug else "Internal"
        t = self.nc.dram_tensor(name, list(shape), dt, kind=kind)
        self.dram[name] = t
        return t

    def R(self, *key):
        r = self.res.get(key)
        if r is None:
            r = Res()
            self.res[key] = r
        return r


WIN_CM = 17
WIN_TM = 7


def build(T, L, debug=(), stop_after=None, only_mix=None, skip_tt0=False, stage=99):
    P = Prog(T, L, debug)
    nc = P.nc
    NT = T // NTOK
    es = ExitStack()
    with es:
        s = Sched(nc, es)
        xT_in = P.din("xT", [D, T])
        outT = P.dout("outT", [D, T])
        gains = P.din("gains", [128, L * 3 * KC])
        w32 = {}
        w16 = {}
        wspec = {
            "wgu1": (NFF, 2 * KC * 128), "wd1": (KC, NFF * 128),
            "wgu2": (NFF, 2 * KC * 128), "wd2": (KC, NFF * 128),
            "wincm": (WIN_CM, KC * 128), "wintm": (WIN_TM, KC * 512), "wout": (KC, KC * 128),
            "cmpw1": (2, 4096), "cmpw2": (2, 128), "cmppe": (2, 32),
        }
        for nm, (npc, el) in wspec.items():
            w32[nm] = P.din(nm, [L, npc, 128, el])
            w16[nm] = P.dscr(nm + "_bf", [L, npc, 128, el], BF16)
        xT = P.dscr("xT_s", [D, T])
        mixT = P.dscr("mixT", [D, T], BF16)
        cm_rows = {"lru": 1024, "kvc": 512, "gqk": 512}
        lruT = P.dscr("lruT", [1024, T])
        kvcT = P.dscr("kvcT", [512, T], BF16)
        gqkT = P.dscr("gqkT", [512, T])
        gaT = P.dscr("gaT", [16, T], BF16)
        q_tm = P.dscr("q_tm", [T, 1024])
        ksw_tm = P.dscr("ksw_tm", [T, 1024])
        gkg_tm = P.dscr("gkg_tm", [T, 512])
        gv_tm = P.dscr("gv_tm", [T, 512], BF16)
        gg_tm = P.dscr("gg_tm", [T, 512])

        lruv = P.din("lruv", [L, 128, 4, 9])
        lrug = P.din("lrug", [L, 2, 4, 128, 128])
        glaw = P.din("glaw", [L, 17, 256])
        glan = P.din("glan", [L, 128])
        c128_d = P.din("c128", [128, 6, 128])
        NCBp = 256
        cmaskT = P.din("cmaskT", [256, T])
        Efull = P.din("Efull", [64, T])
        ropecs = P.din("ropecs", [T, 32])
        fmvm = P.din("fmvm", [T, 128])
        ovl = P.din("ovl", [256, 64])
        nsan = P.din("nsan", [L, 4, 128])
        nsaon = P.din("nsaon", [L, 1024])
        nsagb = P.din("nsagb", [L, 24])
        cast_order = ["wgu1", "wd1", "wincm", "wintm", "cmpw1", "cmpw2", "cmppe", "wout", "wgu2", "wd2"]

        def cast_jobs(l):
            jobs = []
            for nm in cast_order:
                npc, el = wspec[nm]
                grp = max(1, (1 << 20) // (128 * el))
                for p0 in range(0, npc, grp):
                    jobs.append((nm, l, p0, min(npc, p0 + grp)))
            return jobs

        def cast_job(job):
            nm, l, p0, p1 = job
            src = w32[nm].ap()[l, p0:p1].rearrange("c p e -> (c p) e")
            dst = w16[nm].ap()[l, p0:p1].rearrange("c p e -> (c p) e")
            s.dma(dst, src, writes=[P.R("w", nm, l, pc) for pc in range(p0, p1)], q="pool")

        def cast_layer(l):
            for job in cast_jobs(l):
                cast_job(job)

        def cast_layer_old(l):
            for nm, (npc, el) in wspec.items():
                grp = max(1, (1 << 20) // (128 * el))
                for p0 in range(0, npc, grp):
                    p1 = min(npc, p0 + grp)
                    src = w32[nm].ap()[l, p0:p1].rearrange("c p e -> (c p) e")
                    dst = w16[nm].ap()[l, p0:p1].rearrange("c p e -> (c p) e")
                    ws = [P.R("w", nm, l, pc) for pc in range(p0, p1)]
                    s.dma(dst, src, writes=ws, q="pool")

        cmask_bf = P.dscr("cmask_bf", [256, T], BF16)
        Efull_bf = P.dscr("Efull_bf", [64, T], BF16)
        ovl_bf = P.dscr("ovl_bf", [256, 64], BF16)
        s.dma(cmask_bf.ap(), cmaskT.ap(), writes=[P.R("cmask_bf")], q="pool")
        s.dma(Efull_bf.ap(), Efull.ap(), writes=[P.R("Efull_bf")], q="pool")
        s.dma(ovl_bf.ap(), ovl.ap(), writes=[P.R("ovl_bf")], q="pool")
        cast_layer(0)

        def sb(name, shape, dt):
            return es.enter_context(nc.sbuf_tensor(name, list(shape), dt))

        psall = es.enter_context(nc.psum_tensor("psall", [128, 4096], F32))
        psb = psall.bitcast(BF16)
        psum = [psall[:, i * 512:(i + 1) * 512] for i in range(8)]
        psR = [P.R("ps", i) for i in range(8)]
        ones32 = sb("ones32", [128, 128], F32)
        gains_sb = sb("gains_sb", [128, L * 3 * KC], F32)
        s.op("dve", lambda e: e.memset(ones32[:], 1.0), writes=[P.R("ones32")])
        s.dma(gains_sb[:], gains.ap(), writes=[P.R("gains")])
        cst = sb("cst", [128, 4], F32)
        s.op("dve", lambda e: e.memset(cst[:, 0:1], 1.0), writes=[P.R("cst")])
        s.op("dve", lambda e: e.memset(cst[:, 1:2], EPS), writes=[P.R("cst")])
        c128 = sb("c128_sb", [128, 6, 128], F32)
        s.dma(c128[:], c128_d.ap(), writes=[P.R("c128")])
        identb = sb("identb", [128, 128], BF16)
        s.op("dve", lambda e: e.tensor_copy(out=identb[:], in_=c128[:, 0, :]), reads=[P.R("c128")], writes=[P.R("identb")])

        g = dict(locals())
        orchestrate(P, s, g)
        s.barrier(include_pool=True)
    return nc


def orchestrate(P, s, g):
    T, L = P.T, P.L
    NT = T // NTOK
    stop_after = g.get("stop_after")

    def body0(f):
        for t in range(NT):
            f["load_x"](g["xT_in"], t, "xT_in")
            f["ffn"](0, 1, False, True)
            f["proj"](0, t, True)
            f["store_x"](g["xT"] if stop_after != "tt0" else g["outT"], t, "xT")
    if not g.get("skip_tt0"):
        tt_phase(P, s, g, body0, "a")
        s.barrier()
    if stop_after == "tt0":
        return
    for l in range(L):
        mix_phase(P, s, g, l)
        s.barrier()
        if stop_after == "mix%d" % l:
            return

        def body(f, l=l):
            for t in range(NT):
                f["load_x"](g["xT"], t, "xT")
                f["wout"](l, t, True)
                f["ffn"](l, 2, True, l + 1 < L)
                if l + 1 < L:
                    f["ffn"](l + 1, 1, True, True)
                    f["proj"](l + 1, t, True)
                    f["store_x"](g["xT"], t, "xT")
                else:
                    f["store_x"](g["outT"], t, "outT")
        tt_phase(P, s, g, body, "b%d" % l)
        s.barrier()


def mix_phase(P, s, g, l):
    only = g.get("only_mix")
    if only is None or "lru" in only:
        lru_mix(P, s, g, l)
        s.barrier()
    if only is None or "gla" in only:
        gla_mix(P, s, g, l)
        s.barrier()
    if only is None or "nsa" in only:
        nsa_mix(P, s, g, l)
        s.barrier()


def gelu2(s, dst, src, tmp, rsrc, rtmp, rdst):
    s.op("dve", lambda e: e.tensor_tensor(out=tmp, in0=src, in1=src, op=ALU.mult), reads=[rsrc], writes=[rtmp])
    s.op("dve", lambda e: e.tensor_scalar(out=tmp, in0=tmp, scalar1=0.044715, scalar2=1.0, op0=ALU.mult, op1=ALU.add),
         reads=[rtmp], writes=[rtmp])
    s.op("dve", lambda e: e.tensor_tensor(out=tmp, in0=tmp, in1=src, op=ALU.mult), reads=[rtmp, rsrc], writes=[rtmp])
    s.op("act", lambda e: e.activation(out=tmp, in_=tmp, func=AF.Tanh, scale=0.7978845608028654), reads=[rtmp], writes=[rtmp])
    s.op("dve", lambda e: e.scalar_tensor_tensor(out=dst, in0=tmp, scalar=1.0, in1=src, op0=ALU.add, op1=ALU.mult),
         reads=[rtmp, rsrc], writes=[rdst])


def lru_mix(P, s, g, l):
    nc = P.nc
    T = P.T
    NTT = T // 512
    psum, ones32, cst = g["psum"], g["ones32"], g["cst"]
    lruT, mixT = g["lruT"], g["mixT"]
    R = P.R
    with ExitStack() as ms:
        def sb(name, shape, dt):
            return ms.enter_context(nc.sbuf_tensor("lru_%s_%d" % (name, l), list(shape), dt))
        lv = sb("lv", [128, 4, 9], F32)
        gw = sb("gw", [128, 2, 4, 128], F32)
        c12 = sb("c12", [128, 2, 4], F32)
        u_sb = sb("u", [128, 4, 515], F32)
        y_sb = sb("y", [128, 4, 512], F32)
        xc = sb("xc", [128, 4, 512], F32)
        r_sb = sb("r", [128, 4, 512], F32)
        i_sb = sb("i", [128, 4, 512], F32)
        a_sb = sb("a", [128, 4, 512], F32)
        m_sb = sb("m", [128, 4, 512], F32)
        h_sb = sb("h", [128, 4, 512], F32)
        gl = sb("gl", [128, 4, 512], F32)
        tmp = sb("tmp", [128, 4, 512], F32)
        ya = sb("ya", [128, 4, 512], F32)
        sq = sb("sq", [128, 2, 512], F32)
        rstd = sb("rstd", [128, 512], F32)
        outb = sb("outb", [128, 4, 512], BF16)
        hprev = sb("hprev", [128, 4], F32)
        s.dma(lv[:], g["lruv"].ap()[l], writes=[R("lv")])
        s.dma(gw[:], g["lrug"].ap()[l].rearrange("a c p o -> p a c o"), writes=[R("gw")])
        s.op("act", lambda e: e.activation(out=c12[:, 0, :], in_=lv[:, :, 7], func=AF.Exp, scale=-1.0), reads=[R("lv")], writes=[R("c12")])
        s.op("act", lambda e: e.activation(out=c12[:, 0, :], in_=c12[:, 0, :], func=AF.Ln, bias=cst[:, 0:1]), reads=[R("c12"), R("cst")], writes=[R("c12")])
        s.op("dve", lambda e: e.tensor_scalar(out=c12[:, 1, :], in0=c12[:, 0, :], scalar1=-16.0, scalar2=None, op0=ALU.mult), reads=[R("c12")], writes=[R("c12")])
        s.op("dve", lambda e: e.tensor_scalar(out=c12[:, 0, :], in0=c12[:, 0, :], scalar1=-8.0, scalar2=None, op0=ALU.mult), reads=[R("c12")], writes=[R("c12")])
        sqrot = Rot([(0, R("lsq", 0)), (1, R("lsq", 1))])
        C4 = range(4)
        for tt in range(NTT):
            t0 = tt * 512
            for c in C4:
                rows = slice(c * 128, (c + 1) * 128)
                uR, yR = R("lu", c), R("ly", c)
                if tt == 0:
                    s.op("dve", lambda e: e.memset(u_sb[:, c, 0:3], 0.0), writes=[uR])
                    s.dma(u_sb[:, c, 3:515], lruT.ap()[rows, 0:512], reads=[R("lruT", 0)], writes=[uR])
                else:
                    s.dma(u_sb[:, c, :], lruT.ap()[rows, t0 - 3:t0 + 512], reads=[R("lruT", tt - 1), R("lruT", tt)], writes=[uR])
                s.dma(y_sb[:, c, :], lruT.ap()[512 + c * 128:512 + (c + 1) * 128, t0:t0 + 512], reads=[R("lruT", tt)], writes=[yR])
            for c in C4:
                s.op("dve", lambda e: e.tensor_scalar(out=xc[:, c, :], in0=u_sb[:, c, 3:515], scalar1=lv[:, c, 3:4], scalar2=lv[:, c, 4:5],
                                                      op0=ALU.mult, op1=ALU.add), reads=[R("lu", c), R("lv")], writes=[R("lxc", c)])
            for k in (2, 1, 0):
                for c in C4:
                    s.op("dve", lambda e: e.scalar_tensor_tensor(out=xc[:, c, :], in0=u_sb[:, c, k:k + 512], scalar=lv[:, c, k:k + 1],
                                                                  in1=xc[:, c, :], op0=ALU.mult, op1=ALU.add), reads=[R("lu", c), R("lxc", c)], writes=[R("lxc", c)])
            for c in C4:
                for a, (dst, nm, bcol) in enumerate(((r_sb, "lr", 5), (i_sb, "li", 6))):
                    bk = a + 2 * (c % 2)
                    pr = g["psR"][bk]
                    s.op("pe", lambda e: e.matmul(psum[bk][:], gw[:, a, c, :], xc[:, c, :], start=True, stop=True), reads=[R("gw"), R("lxc", c)], writes=[pr])
                    s.op("act", lambda e: e.activation(out=dst[:, c, :], in_=psum[bk][:], func=AF.Sigmoid, bias=lv[:, c, bcol:bcol + 1]),
                         reads=[pr, R("lv")], writes=[R(nm, c)])
            for c in C4:
                s.op("act", lambda e: e.activation(out=a_sb[:, c, :], in_=r_sb[:, c, :], func=AF.Exp, scale=c12[:, 0, c:c + 1]), reads=[R("lr", c), R("c12")], writes=[R("la", c)])
                s.op("act", lambda e: e.activation(out=m_sb[:, c, :], in_=r_sb[:, c, :], func=AF.Exp, scale=c12[:, 1, c:c + 1]), reads=[R("lr", c), R("c12")], writes=[R("lm", c)])
            for c in C4:
                s.op("act", lambda e: e.activation(out=m_sb[:, c, :], in_=m_sb[:, c, :], func=AF.Sqrt, scale=-1.0, bias=cst[:, 0:1]), reads=[R("lm", c), R("cst")], writes=[R("lm", c)])
            for c in C4:
                s.op("dve", lambda e: e.tensor_tensor(out=m_sb[:, c, :], in0=m_sb[:, c, :], in1=i_sb[:, c, :], op=ALU.mult), reads=[R("lm", c), R("li", c)], writes=[R("lm", c)])
            for c in C4:
                s.op("dve", lambda e: e.tensor_tensor(out=m_sb[:, c, :], in0=m_sb[:, c, :], in1=xc[:, c, :], op=ALU.mult), reads=[R("lm", c), R("lxc", c)], writes=[R("lm", c)])
            for c in C4:
                init = 0.0 if tt == 0 else hprev[:, c:c + 1]
                s.op("dve", lambda e: e.tensor_tensor_scan(out=h_sb[:, c, :], data0=a_sb[:, c, :], data1=m_sb[:, c, :], initial=init,
                                                           op0=ALU.mult, op1=ALU.add), reads=[R("la", c), R("lm", c), R("lhp", c)], writes=[R("lh", c)])
            for c in C4:
                s.op("dve", lambda e: e.tensor_copy(out=hprev[:, c:c + 1], in_=h_sb[:, c, 511:512]), reads=[R("lh", c)], writes=[R("lhp", c)])
            for c in C4:
                s.op("dve", lambda e: e.tensor_tensor(out=tmp[:, c, :], in0=y_sb[:, c, :], in1=y_sb[:, c, :], op=ALU.mult), reads=[R("ly", c)], writes=[R("ltmp", c)])
            for c in C4:
                s.op("dve", lambda e: e.tensor_scalar(out=tmp[:, c, :], in0=tmp[:, c, :], scalar1=0.044715, scalar2=1.0, op0=ALU.mult, op1=ALU.add),
                     reads=[R("ltmp", c)], writes=[R("ltmp", c)])
            for c in C4:
                s.op("dve", lambda e: e.tensor_tensor(out=tmp[:, c, :], in0=tmp[:, c, :], in1=y_sb[:, c, :], op=ALU.mult), reads=[R("ltmp", c), R("ly", c)], writes=[R("ltmp", c)])
            for c in C4:
                s.op("act", lambda e: e.activation(out=tmp[:, c, :], in_=tmp[:, c, :], func=AF.Tanh, scale=0.7978845608028654), reads=[R("ltmp", c)], writes=[R("ltmp", c)])
            for c in C4:
                s.op("dve", lambda e: e.scalar_tensor_tensor(out=gl[:, c, :], in0=tmp[:, c, :], scalar=1.0, in1=y_sb[:, c, :], op0=ALU.add, op1=ALU.mult),
                     reads=[R("ltmp", c), R("ly", c)], writes=[R("lgl", c)])
            for c in C4:
                s.op("dve", lambda e: e.scalar_tensor_tensor(out=ya[:, c, :], in0=gl[:, c, :], scalar=0.5, in1=h_sb[:, c, :], op0=ALU.mult, op1=ALU.mult),
                     reads=[R("lgl", c), R("lh", c)], writes=[R("lya", c)])
            for c in C4:
                qi, qR = sqrot.next()
                s.op("act", lambda e: e.activation(out=sq[:, qi, :], in_=ya[:, c, :], func=AF.Square), reads=[R("lya", c)], writes=[qR])
                s.op("pe", lambda e: e.matmul(psum[6][:], ones32[:], sq[:, qi, :], start=(c == 0), stop=(c == 3)), reads=[qR, R("ones32")], writes=[g["psR"][6]])
            s.op("act", lambda e: e.activation(out=rstd[:], in_=psum[6][:], func=AF.Sqrt, scale=1.0 / 512, bias=cst[:, 1:2]),
                 reads=[g["psR"][6], R("cst")], writes=[R("lrstd")])
            s.op("dve", lambda e: e.reciprocal(out=rstd[:], in_=rstd[:]), reads=[R("lrstd")], writes=[R("lrstd")])
            for c in C4:
                s.op("dve", lambda e: e.scalar_tensor_tensor(out=outb[:, c, :], in0=ya[:, c, :], scalar=lv[:, c, 8:9], in1=rstd[:], op0=ALU.mult, op1=ALU.mult),
                     reads=[R("lya", c), R("lrstd"), R("lv")], writes=[R("loutb", c)])
                s.dma(mixT.ap()[c * 128:(c + 1) * 128, t0:t0 + 512], outb[:, c, :], reads=[R("loutb", c)], writes=[R("mixT", tt)], q="act")


def gla_mix(P, s, g, l):
    nc = P.nc
    T = P.T
    NG = T // 512
    psum, psb, psR, ones32, cst, c128 = g["psum"], g["psb"], g["psR"], g["ones32"], g["cst"], g["c128"]
    identb = g["identb"]
    R = P.R
    with ExitStack() as ms:
        def sb(name, shape, dt):
            return ms.enter_context(nc.sbuf_tensor("gla_%s_%d" % (name, l), list(shape), dt))
        aw32 = sb("aw32", [17, 256], F32)
        aw = sb("aw", [17, 256], BF16)
        gout = sb("gout", [128, 128], F32)
        qT = sb("qT", [64, 4, 512], F32)
        kT = sb("kT", [64, 4, 512], F32)
        gaT = sb("gaT", [17, 512], BF16)
        ktm = sb("ktm", [128, 4, 256], F32)
        vtm = sb("vtm", [128, 4, 512], BF16)
        ggt = sb("ggt", [128, 4, 512], F32)
        lsp = sb("lsp", [128, 256], F32)
        ekb = sb("ekb", [128, 256], F32)
        kp = sb("kp", [128, 2, 256], BF16)
        eb = sb("eb", [64, 2, 4, 128], F32)
        enb = sb("enb", [64, 4, 128], F32)
        qt_b = sb("qtb", [64, 2, 4, 128], BF16)
        kt_b = sb("ktb", [64, 2, 4, 128], BF16)
        atm = sb("atm", [128, 2, 4, 128], BF16)
        S32 = sb("S32", [64, 4, 128], F32)
        Sb = sb("Sb", [64, 4, 128], BF16)
        osq = sb("osq", [128, 4, 128], F32)
        on = sb("on", [128, 4, 128], F32)
        ssum = sb("ssum", [128, 4], F32)
        sgg = sb("sgg", [128, 512], F32)
        yc = sb("yc", [128, 512], BF16)
        stg = sb("stg", [128, 4, 512], BF16)
        s.dma(aw32[:], g["glaw"].ap()[l], writes=[R("aw32")])
        s.op("dve", lambda e: e.tensor_copy(out=aw[:], in_=aw32[:]), reads=[R("aw32")], writes=[R("aw")])
        s.dma(gout[:], g["glan"].ap()[l:l + 1, :].to_broadcast([128, 128]), writes=[R("gout")])
        s.op("dve", lambda e: e.memset(gaT[:], 1.0), writes=[R("gaT")])
        s.op("dve", lambda e: e.memset(S32[:], 0.0), writes=[R("S32")])
        s.op("dve", lambda e: e.memset(Sb[:], 0.0), writes=[R("Sb")])
        for gi in range(NG):
            tok = slice(gi * 512, (gi + 1) * 512)
            s.dma(qT[:], g["gqkT"].ap()[0:256, tok].rearrange("(a p) n -> p a n", p=64), reads=[R("gqkT", gi)], writes=[R("gqT")])
            s.dma(kT[:], g["gqkT"].ap()[256:512, tok].rearrange("(a p) n -> p a n", p=64), reads=[R("gqkT", gi)], writes=[R("gkT")])
            s.dma(gaT[0:16, :], g["gaT"].ap()[:, tok], reads=[R("gaT", gi)], writes=[R("gaT")])
            s.dma(ktm[:], g["gkg_tm"].ap()[tok, 0:256].rearrange("(c p) n -> p c n", p=128), reads=[R("gkg_tm", gi)], writes=[R("gktm")])
            s.dma(vtm[:], g["gv_tm"].ap()[tok, :].rearrange("(c p) n -> p c n", p=128), reads=[R("gv_tm", gi)], writes=[R("gvtm")])
            s.dma(ggt[:], g["gg_tm"].ap()[tok, :].rearrange("(c p) n -> p c n", p=128), reads=[R("gg_tm", gi)], writes=[R("gggt")])
            def s1(cc):
                pb = cc % 2
                ct = slice(cc * 128, (cc + 1) * 128)
                s.op("pe", lambda e: e.matmul(psum[0][:, 0:256], gaT[:, ct], aw[:], start=True, stop=True), reads=[R("gaT"), R("aw")], writes=[psR[0]])
                s.op("act", lambda e: e.activation(out=lsp[:], in_=psum[0][:, 0:256], func=AF.Exp, scale=-1.0), reads=[psR[0]], writes=[R("lsp")])
                s.op("act", lambda e: e.activation(out=lsp[:], in_=lsp[:], func=AF.Ln, bias=cst[:, 0:1]), reads=[R("lsp"), R("cst")], writes=[R("lsp")])
                s.op("pe", lambda e: e.matmul(psum[1][:, 0:256], c128[:, 2, :], lsp[:], start=True, stop=True), reads=[R("lsp"), R("c128")], writes=[psR[1]])
                for a in range(4):
                    s.op("pe", lambda e: e.matmul(psum[2][0:64, a * 128:(a + 1) * 128], lsp[:, a * 64:(a + 1) * 64], c128[:, 1, :], start=True, stop=True),
                         reads=[R("lsp"), R("c128")], writes=[psR[2]])
                s.op("act", lambda e: e.activation(out=ekb[:], in_=psum[1][:, 0:256], func=AF.Exp), reads=[psR[1]], writes=[R("ekb")])
                s.op("dve", lambda e: e.tensor_tensor(out=kp[:, pb, :], in0=ktm[:, cc, :], in1=ekb[:], op=ALU.mult), reads=[R("gktm"), R("ekb")], writes=[R("kp", pb)])
                s.op("act", lambda e: e.activation(out=eb[:, pb].rearrange("p a n -> p (a n)"), in_=psum[2][0:64, :], func=AF.Exp), reads=[psR[2]], writes=[R("eb", pb)])
                s.op("act", lambda e: e.activation(out=enb[:].rearrange("p a n -> p (a n)"), in_=psum[2][0:64, :], func=AF.Exp, scale=-1.0), reads=[psR[2]], writes=[R("enb")])
                s.op("dve", lambda e: e.scalar_tensor_tensor(out=qt_b[:, pb], in0=qT[:, :, ct], scalar=0.125, in1=eb[:, pb], op0=ALU.mult, op1=ALU.mult),
                     reads=[R("gqT"), R("eb", pb)], writes=[R("qtb", pb)])
                s.op("dve", lambda e: e.tensor_tensor(out=kt_b[:, pb], in0=kT[:, :, ct], in1=enb[:], op=ALU.mult), reads=[R("gkT"), R("enb")], writes=[R("ktb", pb)])
                def at_fn(e):
                    ins = None
                    for h in range(4):
                        ins = e.matmul(psum[3][:, h * 128:(h + 1) * 128], kt_b[:, pb, h, :], qt_b[:, pb, h, :], start=True, stop=True)
                    return ins
                s.op("pe", at_fn, reads=[R("ktb", pb), R("qtb", pb)], writes=[psR[3]])
                s.op("dve", lambda e: e.tensor_tensor(out=atm[:, pb], in0=psum[3][:].rearrange("p (h n) -> p h n", h=4),
                                                      in1=c128[:, 3, :].unsqueeze(1).to_broadcast([128, 4, 128]), op=ALU.mult),
                     reads=[psR[3], R("c128")], writes=[R("atm", pb)])
            def s2(cc):
                pb = cc % 2
                ct = slice(cc * 128, (cc + 1) * 128)
                def o_fn(e):
                    ins = None
                    for h in range(4):
                        e.matmul(psum[4][:, h * 128:(h + 1) * 128], qt_b[:, pb, h, :], Sb[:, h, :], start=True, stop=False)
                        ins = e.matmul(psum[4][:, h * 128:(h + 1) * 128], atm[:, pb, h, :], vtm[:, cc, h * 128:(h + 1) * 128], start=False, stop=True)
                    return ins
                s.op("pe", o_fn, reads=[R("qtb", pb), R("Sb"), R("atm", pb), R("gvtm")], writes=[psR[4]])
                def kv_fn(e):
                    ins = None
                    for h in range(4):
                        ins = e.matmul(psum[5][0:64, h * 128:(h + 1) * 128], kp[:, pb, h * 64:(h + 1) * 64], vtm[:, cc, h * 128:(h + 1) * 128], start=True, stop=True)
                    return ins
                s.op("pe", kv_fn, reads=[R("kp", pb), R("gvtm")], writes=[psR[5]])
                s.op("dve", lambda e: e.tensor_tensor(out=S32[:], in0=S32[:], in1=eb[:, pb, :, 127:128].to_broadcast([64, 4, 128]), op=ALU.mult),
                     reads=[R("S32"), R("eb", pb)], writes=[R("S32")])
                s.op("dve", lambda e: e.tensor_tensor(out=S32[:], in0=S32[:], in1=psum[5][0:64, :].rearrange("p (h n) -> p h n", h=4), op=ALU.add),
                     reads=[R("S32"), psR[5]], writes=[R("S32")])
                s.op("act", lambda e: e.activation(out=Sb[:], in_=S32[:], func=AF.Copy), reads=[R("S32")], writes=[R("Sb")])
                o3 = psum[4][:].rearrange("p (h n) -> p h n", h=4)
                s.op("act", lambda e: e.activation(out=osq[:], in_=o3, func=AF.Square), reads=[psR[4]], writes=[R("osq")])
                s.op("dve", lambda e: e.tensor_reduce(out=ssum[:], in_=osq[:], axis=AX.X, op=ALU.add), reads=[R("osq")], writes=[R("ssum")])
                s.op("act", lambda e: e.activation(out=ssum[:], in_=ssum[:], func=AF.Sqrt, scale=1.0 / 128, bias=cst[:, 1:2]), reads=[R("ssum"), R("cst")], writes=[R("ssum")])
                s.op("dve", lambda e: e.reciprocal(out=ssum[:], in_=ssum[:]), reads=[R("ssum")], writes=[R("ssum")])
                s.op("dve", lambda e: e.tensor_tensor(out=on[:], in0=o3, in1=ssum[:].unsqueeze(2).to_broadcast([128, 4, 128]), op=ALU.mult),
                     reads=[psR[4], R("ssum")], writes=[R("on")])
                s.op("dve", lambda e: e.tensor_tensor(out=on[:], in0=on[:], in1=gout[:].unsqueeze(1).to_broadcast([128, 4, 128]), op=ALU.mult),
                     reads=[R("on"), R("gout")], writes=[R("on")])
                s.op("act", lambda e: e.activation(out=sgg[:], in_=ggt[:, cc, :], func=AF.Silu), reads=[R("gggt")], writes=[R("sgg")])
                s.op("dve", lambda e: e.tensor_tensor(out=yc[:], in0=on[:].rearrange("p h n -> p (h n)"), in1=sgg[:], op=ALU.mult),
                     reads=[R("on"), R("sgg")], writes=[R("yc")])
                def tr_fn(e):
                    ins = None
                    for h in range(4):
                        ins = e.transpose(psb[:, 6 * 1024 + h * 128:6 * 1024 + (h + 1) * 128], yc[:, h * 128:(h + 1) * 128], identb[:])
                    return ins
                s.op("pe", tr_fn, reads=[R("yc"), R("identb")], writes=[psR[6]])
                s.op("act", lambda e: e.activation(out=stg[:, :, ct], in_=psb[:, 6 * 1024:6 * 1024 + 512].rearrange("p (h n) -> p h n", h=4), func=AF.Copy),
                     reads=[psR[6]], writes=[R("gstg")])
            s1(0)
            for cc in range(4):
                if cc + 1 < 4:
                    s1(cc + 1)
                s2(cc)
            s.dma(g["mixT"].ap()[1536:2048, tok].rearrange("(h p) n -> p h n", p=128), stg[:], reads=[R("gstg")], writes=[R("mixT", gi)], q="act")


def nsa_mix(P, s, g, l):
    nc = P.nc
    T = P.T
    NQ = T // 128
    NCB = (T - 32) // 16 + 1
    SCALE = 128 ** -0.5
    psum, psb, psall, psR, ones32, cst, c128, identb = (g["psum"], g["psb"], g["psall"], g["psR"], g["ones32"], g["cst"],
                                                        g["c128"], g["identb"])
    w16 = g["w16"]
    R = P.R
    with ExitStack() as ms:
        def sbuf(name, shape, dt, st=ms):
            return st.enter_context(nc.sbuf_tensor("nsa_%s_%d" % (name, l), list(shape), dt))
        ksT = sbuf("ksT", [128, 2, T], BF16)
        kwT = sbuf("kwT", [128, 2, T], BF16)
        vsa = sbuf("vsa", [128, NQ, 2, 129], BF16)
        vwa = sbuf("vwa", [128, NQ, 2, 129], BF16)
        cmask = sbuf("cmask", [128, 2, T], BF16)
        Efull = sbuf("Efull", [64, T], BF16)
        cs_sb = sbuf("cs", [128, NQ, 32], F32)
        fmvm = sbuf("fmvm", [128, NQ, 128], F32)
        gates = sbuf("gates", [128, NQ, 24], F32)
        outn = sbuf("outn", [128, 1024], F32)
        nrm_bc = sbuf("nrm_bc", [128, 4, 128], F32)
        kcg = sbuf("kcg", [128, 1], F32)
        gb_bc = sbuf("gb_bc", [128, 24], F32)
        kcn = sbuf("kcn", [128, 2, 256], BF16)
        vca = sbuf("vca", [128, 2, 2, 193], BF16)
        negc4 = sbuf("negc4", [128, 4, 128], BF16)
        negu4 = sbuf("negu4", [128, 4, 128], BF16)
        w2 = sbuf("w2", [128, 2, 128], BF16)
        pe_bf = sbuf("pe_bf", [128, 2, 32], BF16)

        s.dma(cmask[:], g["cmask_bf"].ap().rearrange("(c p) t -> p c t", p=128), reads=[R("cmask_bf")], writes=[R("cmask")])
        s.dma(Efull[:], g["Efull_bf"].ap(), reads=[R("Efull_bf")], writes=[R("Efull")])
        s.dma(cs_sb[:], g["ropecs"].ap().rearrange("(i p) c -> p i c", p=128), writes=[R("cs")])
        s.dma(fmvm[:], g["fmvm"].ap().rearrange("(i p) c -> p i c", p=128), writes=[R("fmvm")])
        s.dma(outn[:], g["nsaon"].ap()[l:l + 1, :].to_broadcast([128, 1024]), writes=[R("outn")])
        for k in range(4):
            s.dma(nrm_bc[:, k, :], g["nsan"].ap()[l, k:k + 1, :].to_broadcast([128, 128]), writes=[R("nrm_bc")])
        s.dma(kcg[:], g["nsan"].ap()[l, 1, :].rearrange("(p o) -> p o", o=1), writes=[R("kcg")])
        s.dma(gb_bc[:], g["nsagb"].ap()[l:l + 1, :].to_broadcast([128, 24]), writes=[R("gb_bc")])
        s.op("dve", lambda e: e.memset(kcn[:], 0.0), writes=[R("kcn")])
        s.op("dve", lambda e: e.memset(vca[:], 0.0), writes=[R("vca")])
        s.op("dve", lambda e: e.memset(vca[:, :, :, 128:129], 1.0), writes=[R("vca")])
        for kv in range(2):
            s.dma(vca[:, :, kv, 129:193], g["ovl_bf"].ap().rearrange("(c p) s -> p c s", p=128), reads=[R("vca"), R("ovl_bf")], writes=[R("vca")])
        s.op("dve", lambda e: e.memset(vsa[:, :, :, 128:129], 1.0), writes=[R("vsa1")])
        s.op("dve", lambda e: e.memset(vwa[:, :, :, 128:129], 1.0), writes=[R("vwa1")])
        s.op("dve", lambda e: e.tensor_copy(out=negc4[:], in_=c128[:, 4, :].unsqueeze(1).to_broadcast([128, 4, 128])), reads=[R("c128")], writes=[R("negc4")])
        s.op("dve", lambda e: e.tensor_copy(out=negu4[:], in_=c128[:, 5, :].unsqueeze(1).to_broadcast([128, 4, 128])), reads=[R("c128")], writes=[R("negu4")])
        s.dma(w2[:], w16["cmpw2"].ap()[l].rearrange("k p o -> p k o"), reads=[R("w", "cmpw2", l, 0), R("w", "cmpw2", l, 1)], writes=[R("w2")])
        s.dma(pe_bf[:], w16["cmppe"].ap()[l].rearrange("k p o -> p k o"), reads=[R("w", "cmppe", l, 0), R("w", "cmppe", l, 1)], writes=[R("pe_bf")])

        with ExitStack() as cs_:
            xT_sb = sbuf("xT", [128, 4, T], BF16, cs_)
            w1 = sbuf("w1", [128, 2, 4096], BF16, cs_)
            hid = sbuf("hid", [128, 4, 256], BF16, cs_)
            hx = sbuf("hx", [128, 256], F32, cs_)
            htmp = sbuf("htmp", [128, 256], F32, cs_)
            hg = sbuf("hg", [128, 256], F32, cs_)
            cvec = sbuf("cvec", [128, 2], F32, cs_)
            ksq = sbuf("ksq", [128, 256], F32, cs_)
            krs = sbuf("krs", [128, 256], F32, cs_)
            s.dma(xT_sb[:], g["kvcT"].ap().rearrange("(a p) t -> p a t", p=128), reads=[R("kvcT", t) for t in range(T // NTOK)], writes=[R("nxT")])
            s.dma(w1[:], w16["cmpw1"].ap()[l].rearrange("k p o -> p k o"), reads=[R("w", "cmpw1", l, 0), R("w", "cmpw1", l, 1)], writes=[R("nw1")])
            s.op("dve", lambda e: e.memset(hid[:], 0.0), writes=[R("nhid")])
            for kvi in range(2):
                mm_group(s, psum[7][:, 0:1], [(w1[:, kvi, ll * 128:(ll + 1) * 128], pe_bf[:, kvi, ll:ll + 1]) for ll in range(32)],
                         reads=[R("nw1"), R("pe_bf")], writes=[psR[7]])
                s.op("dve", lambda e: e.tensor_copy(out=cvec[:, kvi:kvi + 1], in_=psum[7][:, 0:1]), reads=[psR[7]], writes=[R("ncvec")])
                for hd in range(2):
                    a = kvi * 2 + hd
                    mm_group(s, psum[0][:, 0:NCB], [(w1[:, kvi, ll * 128:(ll + 1) * 128], xT_sb[:, a, ll:ll + 16 * (NCB - 1) + 1:16]) for ll in range(32)],
                             reads=[R("nw1"), R("nxT")], writes=[psR[0]])
                    s.op("dve", lambda e: e.tensor_scalar(out=hx[:, 0:NCB], in0=psum[0][:, 0:NCB], scalar1=cvec[:, kvi:kvi + 1], scalar2=None, op0=ALU.add),
                         reads=[psR[0], R("ncvec")], writes=[R("nhx")])
                    gelu2(s, hg[:, 0:NCB], hx[:, 0:NCB], htmp[:, 0:NCB], R("nhx"), R("nhtmp"), R("nhg"))
                    s.op("act", lambda e: e.activation(out=hid[:, a, 0:NCB], in_=hg[:, 0:NCB], func=AF.Copy, scale=0.5), reads=[R("nhg")], writes=[R("nhid")])
                    if kvi == 0:
                        s.op("pe", lambda e: e.matmul(psum[1][:, 0:NCB], w2[:, 0, :], hid[:, a, 0:NCB], start=True, stop=True), reads=[R("w2"), R("nhid")], writes=[psR[1]])
                        s.op("act", lambda e: e.activation(out=ksq[:, 0:NCB], in_=psum[1][:, 0:NCB], func=AF.Square), reads=[psR[1]], writes=[R("nksq")])
                        s.op("pe", lambda e: e.matmul(psum[2][:, 0:NCB], ones32[:], ksq[:, 0:NCB], start=True, stop=True), reads=[R("nksq"), R("ones32")], writes=[psR[2]])
                        s.op("act", lambda e: e.activation(out=krs[:, 0:NCB], in_=psum[2][:, 0:NCB], func=AF.Sqrt, scale=1.0 / 128, bias=cst[:, 1:2]),
                             reads=[psR[2], R("cst")], writes=[R("nkrs")])
                        s.op("dve", lambda e: e.reciprocal(out=krs[:, 0:NCB], in_=krs[:, 0:NCB]), reads=[R("nkrs")], writes=[R("nkrs")])
                        s.op("dve", lambda e: e.scalar_tensor_tensor(out=kcn[:, hd, 0:NCB], in0=psum[1][:, 0:NCB], scalar=kcg[:, 0:1], in1=krs[:, 0:NCB],
                                                                      op0=ALU.mult, op1=ALU.mult), reads=[psR[1], R("kcg"), R("nkrs")], writes=[R("kcn")])
                    else:
                        for c in range((NCB + 127) // 128):
                            s.op("pe", lambda e: e.matmul(psum[3][:, 0:128], hid[:, a, c * 128:(c + 1) * 128], w2[:, 1, :], start=True, stop=True),
                                 reads=[R("w2"), R("nhid")], writes=[psR[3]])
                            s.op("act", lambda e: e.activation(out=vca[:, c, hd, 0:128], in_=psum[3][:, 0:128], func=AF.Copy), reads=[psR[3]], writes=[R("vca")])
            s.barrier()

        q_in = sbuf("q_in", [128, 8, 128], F32)
        ksw_in = sbuf("ksw_in", [128, 8, 128], F32)
        gl_in = sbuf("gl_in", [128, 24], F32)
        sqb = sbuf("sqb", [128, 8, 128], F32)
        xn = sbuf("xn", [128, 8, 128], F32)
        ss = sbuf("ss", [128, 8], F32)
        rt = sbuf("rt", [128, 4, 8, 16], F32)
        qc_bf = sbuf("qc_bf", [128, 8, 128], BF16)
        qr_bf = sbuf("qr_bf", [128, 8, 128], BF16)
        kr_bf = sbuf("kr_bf", [128, 4, 128], BF16)
        qcT = sbuf("qcT", [128, 8, 128], BF16)
        qrT = sbuf("qrT", [128, 8, 128], BF16)
        pT = sbuf("pT", [128, 3, 512], BF16)
        yb = sbuf("yb", [128, 8, 128], F32)
        tmpo = sbuf("tmpo", [128, 4, 128], F32)
        tmpu = sbuf("tmpu", [128, 4, 64], F32)
        imp = sbuf("imp", [128, 64], F32)
        imp2 = sbuf("imp2", [128, 64], F32)
        m8 = sbuf("m8", [128, 2, 8], F32)
        negsel = sbuf("negsel", [128, 2, 64], F32)
        sqy = sbuf("sqy", [128, 8, 128], F32)
        nsT4 = sbuf("nsT4", [64, 2, 4, 128], BF16)
        rden = sbuf("rden", [128, 3, 4], F32)
        coef = sbuf("coef", [128, 3, 4], F32)
        ss1 = sbuf("ss1", [128, 2], F32)
        yn = sbuf("yn", [128, 8, 128], BF16)
        stg = sbuf("stg", [128, 8, 512], BF16)
        pTrot = Rot([(k, R("npT", k)) for k in range(3)])
        SC = Rot([(0, psR[0]), (1, psR[1])])
        OA = Rot([(2, [psR[2], psR[3]]), (4, [psR[4], psR[5]])])

        def oview(b):
            return psall[:, b * 512:(b + 2) * 512].rearrange("p (g c) -> p g c", g=4)

        def norm_rope(i, src, H, gain, out_c, out_r, rsrc, rout):
            s.op("dve", lambda e: e.tensor_tensor(out=sqb[:, 0:H, :], in0=src, in1=src, op=ALU.mult), reads=[rsrc], writes=[R("nsqb")])
            s.op("dve", lambda e: e.tensor_reduce(out=ss[:, 0:H], in_=sqb[:, 0:H, :], axis=AX.X, op=ALU.add), reads=[R("nsqb")], writes=[R("nss")])
            s.op("act", lambda e: e.activation(out=ss[:, 0:H], in_=ss[:, 0:H], func=AF.Sqrt, scale=1.0 / 128, bias=cst[:, 1:2]), reads=[R("nss"), R("cst")], writes=[R("nss")])
            s.op("dve", lambda e: e.reciprocal(out=ss[:, 0:H], in_=ss[:, 0:H]), reads=[R("nss")], writes=[R("nss")])
            s.op("dve", lambda e: e.tensor_tensor(out=xn[:, 0:H, :], in0=src, in1=ss[:, 0:H].unsqueeze(2).to_broadcast([128, H, 128]), op=ALU.mult),
                 reads=[rsrc, R("nss")], writes=[R("nxn")])
            s.op("dve", lambda e: e.tensor_tensor(out=xn[:, 0:H, :], in0=xn[:, 0:H, :], in1=gain.unsqueeze(1).to_broadcast([128, H, 128]), op=ALU.mult),
                 reads=[R("nxn"), R("nrm_bc")], writes=[R("nxn")])
            if out_c is not None:
                s.op("act", lambda e: e.activation(out=out_c, in_=xn[:, 0:H, :], func=AF.Copy), reads=[R("nxn")], writes=[rout])
            cosb = cs_sb[:, i, 0:16].unsqueeze(1).to_broadcast([128, H, 16])
            sinb = cs_sb[:, i, 16:32].unsqueeze(1).to_broadcast([128, H, 16])
            x1 = xn[:, 0:H, 0:16]
            x2 = xn[:, 0:H, 16:32]
            for k, (a, b) in enumerate(((x1, cosb), (x2, sinb), (x2, cosb), (x1, sinb))):
                s.op("dve", lambda e: e.tensor_tensor(out=rt[:, k, 0:H, :], in0=a, in1=b, op=ALU.mult), reads=[R("nxn"), R("cs")], writes=[R("nrt")])
            s.op("dve", lambda e: e.tensor_tensor(out=out_r[:, :, 0:16], in0=rt[:, 0, 0:H, :], in1=rt[:, 1, 0:H, :], op=ALU.subtract), reads=[R("nrt")], writes=[rout])
            s.op("dve", lambda e: e.tensor_tensor(out=out_r[:, :, 16:32], in0=rt[:, 2, 0:H, :], in1=rt[:, 3, 0:H, :], op=ALU.add), reads=[R("nrt")], writes=[rout])
            s.op("act", lambda e: e.activation(out=out_r[:, :, 32:128], in_=xn[:, 0:H, 32:128], func=AF.Copy), reads=[R("nxn")], writes=[rout])

        def transposes(bank, srcs, rsrc):
            def fn(e):
                ins = None
                for k, a in enumerate(srcs):
                    ins = e.transpose(psb[:, bank * 1024 + k * 128:bank * 1024 + (k + 1) * 128], a, identb[:])
                return ins
            s.op("pe", fn, reads=rsrc + [R("identb")], writes=[psR[bank]])

        def branch(i, kv, kind):
            if kind == "sel":
                js = list(range(0, i + 1))
                kT_, va, bi = ksT, vsa, 1
            else:
                js = list(range(max(0, i - 4), i + 1))
                kT_, va, bi = kwT, vwa, 2
            ob, oR = OA.next()
            ov = oview(ob)
            q4 = qrT[:, kv * 4:(kv + 1) * 4, :].rearrange("p g n -> p (g n)")
            def score(j):
                si, sR = SC.next()
                pairs = [(kT_[:, kv, j * 128:(j + 1) * 128], q4)]
                rd = [R("nkT", kind, j), R("nqrT")]
                if kind == "sel":
                    pairs.append((Efull[:, j * 128:(j + 1) * 128], nsT4[:, kv, :, :].rearrange("p g n -> p (g n)")))
                    rd += [R("Efull"), R("nnsT4", kv)]
                if j == i:
                    pairs.append((identb[:], negc4[:].rearrange("p g n -> p (g n)")))
                    rd += [R("identb"), R("negc4")]
                if kind == "win" and j == i - 4:
                    pairs.append((identb[:], negu4[:].rearrange("p g n -> p (g n)")))
                    rd += [R("identb"), R("negu4")]
                mm_group(s, psum[si][:], pairs, reads=rd, writes=[sR])
                return si, sR

            def finish(j, si, sR):
                pi, pR = pTrot.next()
                s.op("act", lambda e: e.activation(out=pT[:, pi, :], in_=psum[si][:], func=AF.Exp, scale=SCALE), reads=[sR], writes=[pR])

                def pv(e):
                    ins = None
                    for gq in range(4):
                        ins = e.matmul(ov[:, gq, 0:129], pT[:, pi, gq * 128:(gq + 1) * 128], va[:, j, kv, :], start=(j == js[0] and gq % 2 == 0), stop=(j == js[-1] and gq % 2 == 1))
                    return ins
                s.op("pe", pv, reads=[pR, R("nva", kind, j), R("vsa1" if kind == "sel" else "vwa1")], writes=oR)

            nxt = score(js[0])
            for idx, j in enumerate(js):
                cur = nxt
                if idx + 1 < len(js):
                    nxt = score(js[idx + 1])
                finish(j, *cur)
            s.op("dve", lambda e: e.reciprocal(out=rden[:, bi, :], in_=ov[:, :, 128]), reads=oR, writes=[R("nrden", bi)])
            s.op("dve", lambda e: e.tensor_tensor(out=coef[:, bi, :], in0=gates[:, i, kv * 12 + bi:kv * 12 + 12:3], in1=rden[:, bi, :], op=ALU.mult),
                 reads=[R("ngates", i), R("nrden", bi)], writes=[R("ncoef", bi)])
            s.op("dve", lambda e: e.tensor_tensor(out=tmpo[:], in0=ov[:, :, 0:128], in1=coef[:, bi, :].unsqueeze(2).to_broadcast([128, 4, 128]), op=ALU.mult),
                 reads=oR + [R("ncoef", bi)], writes=[R("ntmpo")])
            s.op("dve", lambda e: e.tensor_tensor(out=yb[:, kv * 4:(kv + 1) * 4, :], in0=yb[:, kv * 4:(kv + 1) * 4, :], in1=tmpo[:], op=ALU.add),
                 reads=[R("ntmpo"), R("nyb")], writes=[R("nyb")])

        def prep_dve(i):
            rows = slice(i * 128, (i + 1) * 128)
            tt_i = i // 4
            s.dma(q_in[:].rearrange("p h d -> p (h d)"), g["q_tm"].ap()[rows, :], reads=[R("q_tm", tt_i)], writes=[R("nq_in")])
            s.dma(ksw_in[:].rearrange("p h d -> p (h d)"), g["ksw_tm"].ap()[rows, :], reads=[R("ksw_tm", tt_i)], writes=[R("nksw_in")])
            s.dma(gl_in[:], g["gkg_tm"].ap()[rows, 256:280], reads=[R("gkg_tm", tt_i)], writes=[R("ngl_in")])
            s.op("dve", lambda e: e.tensor_tensor(out=gl_in[:], in0=gl_in[:], in1=gb_bc[:], op=ALU.add), reads=[R("ngl_in"), R("gb_bc")], writes=[R("ngl_in")])
            s.op("act", lambda e: e.activation(out=gates[:, i, :], in_=gl_in[:], func=AF.Sigmoid), reads=[R("ngl_in")], writes=[R("ngates", i)])
            norm_rope(i, q_in[:], 8, nrm_bc[:, 0, :], qc_bf[:], qr_bf[:], R("nq_in"), R("nqbf"))
            norm_rope(i, ksw_in[:, 0:2, :], 2, nrm_bc[:, 2, :], None, kr_bf[:, 0:2, :], R("nksw_in"), R("nkrbf"))
            norm_rope(i, ksw_in[:, 4:6, :], 2, nrm_bc[:, 3, :], None, kr_bf[:, 2:4, :], R("nksw_in"), R("nkrbf"))
            s.op("dve", lambda e: e.tensor_copy(out=vsa[:, i, :, 0:128], in_=ksw_in[:, 2:4, :]), reads=[R("nksw_in")], writes=[R("nva", "sel", i)])
            s.op("dve", lambda e: e.tensor_copy(out=vwa[:, i, :, 0:128], in_=ksw_in[:, 6:8, :]), reads=[R("nksw_in")], writes=[R("nva", "win", i)])

        def prep_pe(i):
            rows = slice(i * 128, (i + 1) * 128)
            transposes(6, [qc_bf[:, h, :] for h in range(8)], [R("nqbf")])
            s.op("act", lambda e: e.activation(out=qcT[:].rearrange("p h n -> p (h n)"), in_=psb[:, 6 * 1024:7 * 1024], func=AF.Copy), reads=[psR[6]], writes=[R("nqcT")])
            transposes(7, [qr_bf[:, h, :] for h in range(8)], [R("nqbf")])
            s.op("dve", lambda e: e.tensor_copy(out=qrT[:].rearrange("p h n -> p (h n)"), in_=psb[:, 7 * 1024:8 * 1024]), reads=[psR[7]], writes=[R("nqrT")])
            transposes(6, [kr_bf[:, h, :] for h in range(4)], [R("nkrbf")])
            s.op("act", lambda e: e.activation(out=ksT[:, :, rows], in_=psb[:, 6 * 1024:6 * 1024 + 256].rearrange("p (h n) -> p h n", h=2), func=AF.Copy),
                 reads=[psR[6]], writes=[R("nkT", "sel", i)])
            s.op("act", lambda e: e.activation(out=kwT[:, :, rows], in_=psb[:, 6 * 1024 + 256:6 * 1024 + 512].rearrange("p (h n) -> p h n", h=2), func=AF.Copy),
                 reads=[psR[6]], writes=[R("nkT", "win", i)])

        def cmp_topk(i, kv):
            rows = slice(i * 128, (i + 1) * 128)
            n_max = min(NCB - 1, 8 * i + 6)
            ncc = n_max // 128 + 1
            ob, oR = OA.next()
            ov = oview(ob)
            q4c = qcT[:, kv * 4:(kv + 1) * 4, :].rearrange("p g n -> p (g n)")
            for c in range(ncc):
                si, sR = SC.next()
                s.op("pe", lambda e: e.matmul(psum[si][:], kcn[:, kv, c * 128:(c + 1) * 128], q4c, start=True, stop=True), reads=[R("kcn"), R("nqcT")], writes=[sR])
                pi, pR = pTrot.next()
                s.op("act", lambda e: e.activation(out=pT[:, pi, :], in_=psum[si][:], func=AF.Exp, scale=SCALE), reads=[sR], writes=[pR])
                s.op("dve", lambda e: e.tensor_tensor(out=pT[:, pi, :].rearrange("p (g n) -> p g n", g=4), in0=pT[:, pi, :].rearrange("p (g n) -> p g n", g=4),
                                                      in1=cmask[:, c, rows].unsqueeze(1).to_broadcast([128, 4, 128]), op=ALU.mult),
                     reads=[pR, R("cmask")], writes=[pR])

                def pvc(e):
                    ins = None
                    for gq in range(4):
                        ins = e.matmul(ov[:, gq, 0:193], pT[:, pi, gq * 128:(gq + 1) * 128], vca[:, c, kv, :], start=(c == 0 and gq % 2 == 0), stop=(c == ncc - 1 and gq % 2 == 1))
                    return ins
                s.op("pe", pvc, reads=[pR, R("vca")], writes=oR)
            s.op("dve", lambda e: e.tensor_scalar(out=rden[:, 0, :], in0=ov[:, :, 128], scalar1=1e-30, scalar2=None, op0=ALU.max), reads=oR, writes=[R("nrden", 0)])
            s.op("dve", lambda e: e.reciprocal(out=rden[:, 0, :], in_=rden[:, 0, :]), reads=[R("nrden", 0)], writes=[R("nrden", 0)])
            s.op("dve", lambda e: e.tensor_tensor(out=tmpu[:], in0=ov[:, :, 129:193], in1=rden[:, 0, :].unsqueeze(2).to_broadcast([128, 4, 64]), op=ALU.mult),
                 reads=oR + [R("nrden", 0)], writes=[R("ntmpu")])
            s.op("dve", lambda e: e.tensor_reduce(out=imp[:], in_=tmpu[:].rearrange("p g s -> p s g"), axis=AX.X, op=ALU.add), reads=[R("ntmpu")], writes=[R("nimp")])
            s.op("dve", lambda e: e.tensor_tensor(out=coef[:, 0, :], in0=gates[:, i, kv * 12:kv * 12 + 12:3], in1=rden[:, 0, :], op=ALU.mult),
                 reads=[R("ngates", i), R("nrden", 0)], writes=[R("ncoef", 0)])
            s.op("dve", lambda e: e.tensor_tensor(out=yb[:, kv * 4:(kv + 1) * 4, :], in0=ov[:, :, 0:128], in1=coef[:, 0, :].unsqueeze(2).to_broadcast([128, 4, 128]), op=ALU.mult),
                 reads=oR + [R("ncoef", 0)], writes=[R("nyb")])
            s.op("dve", lambda e: e.tensor_tensor(out=imp[:], in0=imp[:], in1=fmvm[:, i, 64:128], op=ALU.mult), reads=[R("nimp"), R("fmvm")], writes=[R("nimp")])
            s.op("dve", lambda e: e.tensor_tensor(out=imp[:], in0=imp[:], in1=fmvm[:, i, 0:64], op=ALU.add), reads=[R("nimp"), R("fmvm")], writes=[R("nimp")])
            s.op("dve", lambda e: e.max(out=m8[:, 0, :], in_=imp[:]), reads=[R("nimp")], writes=[R("nm8")])
            s.op("dve", lambda e: e.match_replace(out=imp2[:], in_to_replace=m8[:, 0, :], in_values=imp[:], imm_value=-3.0e38), reads=[R("nimp"), R("nm8")], writes=[R("nimp2")])
            s.op("dve", lambda e: e.max(out=m8[:, 1, :], in_=imp2[:]), reads=[R("nimp2")], writes=[R("nm8")])
            s.op("dve", lambda e: e.tensor_scalar(out=negsel[:, kv, :], in0=imp[:], scalar1=m8[:, 1, 7:8], scalar2=1.0, op0=ALU.is_ge, op1=ALU.subtract),
                 reads=[R("nimp"), R("nm8")], writes=[R("nnegsel", kv)])

        def sel_mask_T(kv):
            s.op("pe", lambda e: e.transpose(psum[7][0:64, 0:128], negsel[:, kv, :], c128[:, 0, :]), reads=[R("nnegsel", kv), R("c128")], writes=[psR[7]])
            s.op("act", lambda e: e.activation(out=nsT4[:, kv, :, :], in_=psum[7][0:64, 0:128].unsqueeze(1).to_broadcast([64, 4, 128]), func=AF.Copy),
                 reads=[psR[7]], writes=[R("nnsT4", kv)])

        def out_dve(i):
            s.op("dve", lambda e: e.tensor_tensor(out=sqy[:], in0=yb[:], in1=yb[:], op=ALU.mult), reads=[R("nyb")], writes=[R("nsqy")])
            s.op("dve", lambda e: e.tensor_reduce(out=ss1[:, 0:1], in_=sqy[:].rearrange("p h d -> p (h d)"), axis=AX.X, op=ALU.add), reads=[R("nsqy")], writes=[R("nss1")])
            s.op("act", lambda e: e.activation(out=ss1[:, 0:1], in_=ss1[:, 0:1], func=AF.Sqrt, scale=1.0 / 1024, bias=cst[:, 1:2]), reads=[R("nss1"), R("cst")], writes=[R("nss1")])
            s.op("dve", lambda e: e.reciprocal(out=ss1[:, 0:1], in_=ss1[:, 0:1]), reads=[R("nss1")], writes=[R("nss1")])
            s.op("dve", lambda e: e.scalar_tensor_tensor(out=yn[:].rearrange("p h d -> p (h d)"), in0=yb[:].rearrange("p h d -> p (h d)"), scalar=ss1[:, 0:1], in1=outn[:],
                                                          op0=ALU.mult, op1=ALU.mult), reads=[R("nyb"), R("nss1"), R("outn")], writes=[R("nyn")])

        def out_pe(i):
            tt_i = i // 4
            transposes(6, [yn[:, h, :] for h in range(8)], [R("nyn")])
            ci = i % 4
            s.op("act", lambda e: e.activation(out=stg[:, :, ci * 128:(ci + 1) * 128], in_=psb[:, 6 * 1024:7 * 1024].rearrange("p (h n) -> p h n", h=8), func=AF.Copy),
                 reads=[psR[6]], writes=[R("nstg")])
            if ci == 3:
                tok = slice(tt_i * 512, (tt_i + 1) * 512)
                s.dma(g["mixT"].ap()[512:1536, tok].rearrange("(h p) n -> p h n", p=128), stg[:], reads=[R("nstg")], writes=[R("mixT", tt_i)], q="act")

        jobs = g["cast_jobs"](l + 1) if l + 1 < P.L else []
        per_tile = (len(jobs) + NQ - 1) // NQ if jobs else 0
        prep_dve(0)
        prep_pe(0)
        for i in range(NQ):
            for job in jobs[i * per_tile:(i + 1) * per_tile]:
                g["cast_job"](job)
            cmp_topk(i, 0)
            if i > 0:
                out_pe(i - 1)
            cmp_topk(i, 1)
            if i + 1 < NQ:
                prep_dve(i + 1)
            for kv in range(2):
                branch(i, kv, "win")
                sel_mask_T(kv)
                branch(i, kv, "sel")
            out_dve(i)
            if i + 1 < NQ:
                prep_pe(i + 1)
        out_pe(NQ - 1)


def tt_phase(P, s, g, body, sfx):
    nc = P.nc
    T, L = P.T, P.L
    NT = T // NTOK
    psum, psR = g["psum"], g["psR"]
    w16 = g["w16"]
    ones32, gains_sb = g["ones32"], g["gains_sb"]

    with ExitStack() as ts:
        def sb(name, shape, dt):
            return ts.enter_context(nc.sbuf_tensor(name + sfx, list(shape), dt))

        x_sb = sb("x_sb", [128, KC, NTOK], F32)
        h_sb = sb("h_sb", [128, KC, NTOK], BF16)
        act_sb = sb("act_sb", [128, NFF, NTOK], BF16)
        wbuf = sb("wbuf", [128, 4, 8192], BF16)
        sq_sb = sb("sq_sb", [128, 2, NTOK], F32)
        rstd_sb = sb("rstd_sb", [128, NTOK], F32)
        sg_sb = sb("sg_sb", [128, 2, NTOK], F32)
        stg_sb = sb("stg_sb", [128, 3, NTOK], F32)
        stgb_sb = sb("stgb_sb", [128, 3, NTOK], BF16)
        xR = [P.R("x_sb", i) for i in range(KC)]
        hRs = [P.R("h_sb", i) for i in range(KC)]
        actR = [P.R("act_sb", i) for i in range(NFF)]
        wrot = Rot([(i, P.R("wbuf", i)) for i in range(4)])
        sqrot = Rot([(i, P.R("sq", i)) for i in range(2)])
        sgrot = Rot([(i, P.R("sg", i)) for i in range(2)])
        stgrot = Rot([(i, P.R("stg", i)) for i in range(3)])
        stgbrot = Rot([(i, P.R("stgb", i)) for i in range(3)])
        rstdR = P.R("rstd")
        PG = Rot([(0, psR[0]), (1, psR[1])])
        PU = Rot([(2, psR[2]), (3, psR[3])])
        PO = Rot([(4, psR[4]), (5, psR[5])])
        PSTAT = (6, psR[6])

        def load_w(nm, l, pc, nel):
            bi, bR = wrot.next()
            s.dma(wbuf[:, bi, 0:nel], w16[nm].ap()[l, pc], reads=[P.R("w", nm, l, pc)], writes=[bR])
            return bi, bR

        def stats_act(kc):
            qi, qR = sqrot.next()
            s.op("act", lambda e: e.activation(out=sq_sb[:, qi, :], in_=x_sb[:, kc, :], func=AF.Square),
                 reads=[xR[kc]], writes=[qR])
            return kc, qi, qR

        def stats_pe(job):
            kc, qi, qR = job
            pi, pR = PSTAT
            s.op("pe", lambda e: e.matmul(psum[pi][:], ones32[:], sq_sb[:, qi, :], start=(kc == 0), stop=(kc == KC - 1)),
                 reads=[qR, P.R("ones32")], writes=[pR])

        def norm(l, which, pre=False):
            pi, pR = PSTAT
            if not pre:
                for kc in range(KC):
                    stats_pe(stats_act(kc))
            s.op("act", lambda e: e.activation(out=rstd_sb[:], in_=psum[pi][:], func=AF.Sqrt, scale=1.0 / D, bias=eps_sb[:, 0:1]),
                 reads=[pR, P.R("eps")], writes=[rstdR])
            s.op("dve", lambda e: e.reciprocal(out=rstd_sb[:], in_=rstd_sb[:]), reads=[rstdR], writes=[rstdR])
            gbase = (l * 3 + which) * KC
            for kc in range(KC):
                s.op("dve", lambda e: e.scalar_tensor_tensor(out=h_sb[:, kc, :], in0=x_sb[:, kc, :],
                                                              scalar=gains_sb[:, gbase + kc:gbase + kc + 1], in1=rstd_sb[:],
                                                              op0=ALU.mult, op1=ALU.mult),
                     reads=[xR[kc], rstdR, P.R("gains")], writes=[hRs[kc]])

        eps_sb = sb("eps_sb", [128, 1], F32)
        s.op("dve", lambda e: e.memset(eps_sb[:], EPS), writes=[P.R("eps")])

        def ffn(l, which, pre=False, nxt=False):
            nm_gu = "wgu%d" % which
            nm_d = "wd%d" % which
            norm(l, 0 if which == 1 else 2, pre)
            for f in range(NFF):
                bi, bR = load_w(nm_gu, l, f, 2 * KC * 128)
                gi, gR = PG.next()
                ui, uR = PU.next()
                mm_group(s, psum[gi][:], [(wbuf[:, bi, kc * 128:(kc + 1) * 128], h_sb[:, kc, :]) for kc in range(KC)],
                         reads=[bR] + hRs, writes=[gR])
                mm_group(s, psum[ui][:], [(wbuf[:, bi, (KC + kc) * 128:(KC + kc + 1) * 128], h_sb[:, kc, :]) for kc in range(KC)],
                         reads=[bR] + hRs, writes=[uR])
                si, sR = sgrot.next()
                s.op("act", lambda e: e.activation(out=sg_sb[:, si, :], in_=psum[gi][:], func=AF.Silu), reads=[gR], writes=[sR])
                s.op("dve", lambda e: e.tensor_tensor(out=act_sb[:, f, :], in0=sg_sb[:, si, :], in1=psum[ui][:], op=ALU.mult),
                     reads=[sR, uR], writes=[actR[f]])
            pend = None
            for m in range(KC):
                bi, bR = load_w(nm_d, l, m, NFF * 128)
                oi, oR = PO.next()
                mm_group(s, psum[oi][:], [(wbuf[:, bi, fc * 128:(fc + 1) * 128], act_sb[:, fc, :]) for fc in range(NFF)],
                         reads=[bR] + actR, writes=[oR])
                if pend is not None:
                    stats_pe(pend)
                    pend = None
                s.op("dve", lambda e: e.scalar_tensor_tensor(out=x_sb[:, m, :], in0=psum[oi][:], scalar=0.5, in1=x_sb[:, m, :],
                                                              op0=ALU.mult, op1=ALU.add),
                     reads=[oR, xR[m]], writes=[xR[m]])
                if nxt:
                    pend = stats_act(m)
            if pend is not None:
                stats_pe(pend)

        def wout(l, t, nxt=False):
            tok = slice(t * NTOK, (t + 1) * NTOK)
            s.dma(h_sb[:], g["mixT"].ap()[:, tok].rearrange("(kc p) n -> p kc n", p=128),
                  reads=[P.R("mixT", t)], writes=hRs)
            pend = None
            for m in range(KC):
                bi, bR = load_w("wout", l, m, KC * 128)
                oi, oR = PO.next()
                mm_group(s, psum[oi][:], [(wbuf[:, bi, kc * 128:(kc + 1) * 128], h_sb[:, kc, :]) for kc in range(KC)],
                         reads=[bR] + hRs, writes=[oR])
                if pend is not None:
                    stats_pe(pend)
                    pend = None
                s.op("dve", lambda e: e.tensor_tensor(out=x_sb[:, m, :], in0=psum[oi][:], in1=x_sb[:, m, :], op=ALU.add),
                     reads=[oR, xR[m]], writes=[xR[m]])
                if nxt:
                    pend = stats_act(m)
            if pend is not None:
                stats_pe(pend)

        cm_dst = ([("lruT", 128 * i, 128, F32) for i in range(8)] + [("kvcT", 128 * i, 128, BF16) for i in range(4)]
                  + [("gqkT", 128 * i, 128, F32) for i in range(4)] + [("gaT", 0, 16, BF16)])
        tm_dst = [("q_tm", 0, F32), ("q_tm", 512, F32), ("ksw_tm", 0, F32), ("ksw_tm", 512, F32), ("gkg_tm", 0, F32),
                  ("gv_tm", 0, BF16), ("gg_tm", 0, F32)]

        def proj(l, t, pre=False):
            tok = slice(t * NTOK, (t + 1) * NTOK)
            norm(l, 1, pre)
            pieces = [("wincm", c, KC * 128) for c in range(WIN_CM)] + [("wintm", pc, KC * 512) for pc in range(WIN_TM)]
            loaded = {}

            def ensure(k):
                if k < len(pieces) and k not in loaded:
                    loaded[k] = load_w(pieces[k][0], l, pieces[k][1], pieces[k][2])
            ensure(0)
            ensure(1)
            for c in range(WIN_CM):
                ensure(c + 2)
                bi, bR = loaded[c]
                name, r0, nr, dt = cm_dst[c]
                oi, oR = PO.next()
                mm_group(s, psum[oi][0:nr, :], [(wbuf[:, bi, kc * 128:kc * 128 + nr], h_sb[:, kc, :]) for kc in range(KC)],
                         reads=[bR] + hRs, writes=[oR])
                if dt == F32:
                    gi, gR = stgrot.next()
                    dst_sb = stg_sb[0:nr, gi, :]
                else:
                    gi, gR = stgbrot.next()
                    dst_sb = stgb_sb[0:nr, gi, :]
                s.op("act", lambda e: e.activation(out=dst_sb, in_=psum[oi][0:nr, :], func=AF.Copy), reads=[oR], writes=[gR])
                s.dma(g[name].ap()[r0:r0 + nr, tok], dst_sb, reads=[gR], writes=[P.R(name, t)], q="act")
            for pc in range(WIN_TM):
                ensure(WIN_CM + pc + 2)
                bi, bR = loaded[WIN_CM + pc]
                name, c0, dt = tm_dst[pc]
                for sub in range(NTOK // 128):
                    oi, oR = PO.next()
                    mm_group(s, psum[oi][:], [(h_sb[:, kc, sub * 128:(sub + 1) * 128], wbuf[:, bi, kc * 512:(kc + 1) * 512])
                                               for kc in range(KC)], reads=[bR] + hRs, writes=[oR])
                    if dt == F32:
                        gi, gR = stgrot.next()
                        dst_sb = stg_sb[:, gi, :]
                    else:
                        gi, gR = stgbrot.next()
                        dst_sb = stgb_sb[:, gi, :]
                    eng = "act" if sub % 2 == 0 else "dve"
                    if eng == "act":
                        s.op("act", lambda e: e.activation(out=dst_sb, in_=psum[oi][:], func=AF.Copy), reads=[oR], writes=[gR])
                    else:
                        s.op("dve", lambda e: e.tensor_copy(out=dst_sb, in_=psum[oi][:]), reads=[oR], writes=[gR])
                    r0 = t * NTOK + sub * 128
                    s.dma(g[name].ap()[r0:r0 + 128, c0:c0 + 512], dst_sb, reads=[gR], writes=[P.R(name, t)], q="act")

        def load_x(src, t, srcname):
            tok = slice(t * NTOK, (t + 1) * NTOK)
            for kc in range(KC):
                s.dma(x_sb[:, kc, :], src.ap()[kc * 128:(kc + 1) * 128, tok], reads=[P.R(srcname, t, kc)], writes=[xR[kc]])

        def store_x(dst, t, dstname):
            tok = slice(t * NTOK, (t + 1) * NTOK)
            for kc in range(KC):
                s.dma(dst.ap()[kc * 128:(kc + 1) * 128, tok], x_sb[:, kc, :], reads=[xR[kc]], writes=[P.R(dstname, t, kc)], q="act")

        body(dict(load_x=load_x, store_x=store_x, ffn=ffn, proj=proj, wout=wout))


CM_COLS = ([list(range(128 * i, 128 * (i + 1))) for i in range(8)]
           + [list(range(2048 + 128 * i, 2048 + 128 * (i + 1))) for i in range(4)]
           + [list(range(3608 + 128 * i, 3608 + 128 * (i + 1))) for i in range(4)]
           + [list(range(5144, 5160)) + [-1] * 112])
TM_COLS = [list(range(1024, 1536)), list(range(1536, 2048)), list(range(2560, 3072)), list(range(3072, 3584)),
           list(range(3864, 4120)) + list(range(3584, 3608)) + [-1] * 232,
           list(range(4120, 4632)), list(range(4632, 5144))]


def _tile_cols(W, cols):
    cols = np.asarray(cols)
    Wz = np.concatenate([W, np.zeros((W.shape[0], 1), W.dtype)], axis=1)
    sel = Wz[:, cols]
    return sel.reshape(KC, 128, len(cols)).transpose(1, 0, 2)


def prep_weights(inp, L):
    out = {}
    g = np.stack([inp["ffn1_norm"][:L], inp["mix_norm"][:L], inp["ffn2_norm"][:L]], axis=1)
    out["gains"] = np.ascontiguousarray(g.reshape(L, 3, KC, 128).transpose(3, 0, 1, 2).reshape(128, L * 3 * KC))
    ffn_w = {1: (inp["ffn1_w_gate"], inp["ffn1_w_up"], inp["ffn1_w_down"]),
             2: (inp["ffn2_w_gate"], inp["ffn2_w_up"], inp["ffn2_w_down"])}
    for which in (1, 2):
        wg = ffn_w[which][0][:L].reshape(L, KC, 128, NFF, 128)
        wu = ffn_w[which][1][:L].reshape(L, KC, 128, NFF, 128)
        gu = np.stack([wg, wu], axis=1)
        out["wgu%d" % which] = np.ascontiguousarray(gu.transpose(0, 4, 3, 1, 2, 5)).reshape(L, NFF, 128, 2 * KC * 128)
        wd = ffn_w[which][2][:L].reshape(L, NFF, 128, KC, 128)
        out["wd%d" % which] = np.ascontiguousarray(wd.transpose(0, 3, 2, 1, 4)).reshape(L, KC, 128, NFF * 128)
    win = inp["w_in"][:L]
    out["wincm"] = np.stack([np.stack([_tile_cols(win[l], c) for c in CM_COLS]) for l in range(L)]).reshape(L, WIN_CM, 128, KC * 128)
    out["wintm"] = np.stack([np.stack([_tile_cols(win[l], c) for c in TM_COLS]) for l in range(L)]).reshape(L, WIN_TM, 128, KC * 512)
    wo = inp["w_out"][:L].reshape(L, KC, 128, KC, 128)
    out["wout"] = np.ascontiguousarray(wo.transpose(0, 3, 2, 1, 4)).reshape(L, KC, 128, KC * 128)
    return {k: np.ascontiguousarray(v, dtype=np.float32) for k, v in out.items()}


def make_consts(T):
    c = np.zeros((128, 6, 128), np.float32)
    i = np.arange(128)
    c[:, 0, :] = np.eye(128)
    c[:, 1, :] = np.where(i[:, None] <= i[None, :], -1.0 / 16.0, 0.0)
    c[:, 2, :] = np.where(i[:, None] > i[None, :], -1.0 / 16.0, 0.0)
    c[:, 3, :] = np.where(i[:, None] <= i[None, :], 1.0, 0.0)
    c[:, 4, :] = np.where(i[:, None] <= i[None, :], 0.0, -30000.0)
    c[:, 5, :] = np.where(i[:, None] > i[None, :], 0.0, -30000.0)
    out = {"c128": c}
    NCB = (T - 32) // 16 + 1
    NS = T // 64
    t = np.arange(T)
    n = np.arange(256)
    out["cmaskT"] = ((n[:, None] < NCB) & (16 * n[:, None] + 31 <= t[None, :])).astype(np.float32)
    sidx = np.arange(64)
    out["Efull"] = np.where((t[None, :] // 64) == sidx[:, None], 30000.0, 0.0).astype(np.float32)
    inv = (np.float32(1.0) / (np.float32(500000.0) ** (np.arange(0, 32, 2, dtype=np.float32) / np.float32(32)))).astype(np.float32)
    ang = (t.astype(np.float32)[:, None] * inv[None, :]).astype(np.float32)
    out["ropecs"] = np.concatenate([np.cos(ang), np.sin(ang)], axis=1).astype(np.float32)
    cur = t // 64
    forced = (sidx[None, :] == 0) | (sidx[None, :] == cur[:, None]) | (sidx[None, :] == cur[:, None] - 1)
    valid = (sidx[None, :] * 64 <= t[:, None]) & (sidx[None, :] < NS)
    forced = forced & valid
    fm = np.where(forced, 1e30, np.where(valid, 0.0, -1e30))
    vm = (valid & ~forced).astype(np.float32)
    out["fmvm"] = np.concatenate([fm, vm], axis=1).astype(np.float32)
    cstart = n * 16
    sstart = sidx * 64
    ov = (n[:, None] < NCB) & (sidx[None, :] < NS) & (cstart[:, None] < sstart[None, :] + 64) & (cstart[:, None] + 32 > sstart[None, :])
    out["ovl"] = ov.astype(np.float32)
    return out


def prep_small(inp, L):
    out = {}
    lv = np.zeros((L, 128, 4, 9), np.float32)

    def cp(v):
        return v.reshape(L, 4, 128).transpose(0, 2, 1)
    for k in range(4):
        lv[..., k] = cp(inp["lru_conv_w"][:L, k])
    lv[..., 4] = cp(inp["lru_conv_b"][:L])
    lv[..., 5] = cp(inp["lru_gate_a_b"][:L])
    lv[..., 6] = cp(inp["lru_gate_x_b"][:L])
    lv[..., 7] = cp(inp["lru_lambda"][:L])
    lv[..., 8] = cp(inp["lru_out_norm"][:L])
    out["lruv"] = lv
    gw = np.zeros((L, 2, 4, 128, 128), np.float32)
    for a, nm in enumerate(("lru_gate_a_w", "lru_gate_x_w")):
        w = inp[nm][:L]
        for c in range(4):
            gw[:, a, c, 0:64, 0:64] = w[:, 2 * c]
            gw[:, a, c, 64:128, 64:128] = w[:, 2 * c + 1]
    out["lrug"] = gw
    out["glaw"] = np.concatenate([inp["gla_a_w2"][:L], inp["gla_a_b"][:L, None, :]], axis=1)
    out["glan"] = inp["gla_out_norm"][:L]
    out["nsan"] = np.stack([inp["nsa_q_norm"][:L], inp["nsa_k_cmp_norm"][:L], inp["nsa_k_sel_norm"][:L], inp["nsa_k_win_norm"][:L]], axis=1)
    out["nsaon"] = inp["nsa_out_norm"][:L]
    out["nsagb"] = inp["nsa_gate_b"][:L]
    out["cmpw1"] = np.stack([inp[k][:L].reshape(L, 32, 128, 128).transpose(0, 2, 1, 3).reshape(L, 128, 4096)
                             for k in ("nsa_cmp_w1_k", "nsa_cmp_w1_v")], axis=1)
    out["cmpw2"] = np.stack([inp["nsa_cmp_w2_k"][:L], inp["nsa_cmp_w2_v"][:L]], axis=1)
    out["cmppe"] = np.stack([inp["nsa_cmp_pe_k"][:L].transpose(0, 2, 1), inp["nsa_cmp_pe_v"][:L].transpose(0, 2, 1)], axis=1)
    return {k: np.ascontiguousarray(v, dtype=np.float32) for k, v in out.items()}


_CACHE = {}


def kernel(**inputs):
    inp = {k: np.asarray(v) for k, v in inputs.items()}
    x = inp["x"]
    B, T, _ = x.shape
    L = inp["w_in"].shape[0]
    key = (T, L)
    if key not in _CACHE:
        _CACHE[key] = build(T, L)
    nc = _CACHE[key]
    shared = {}
    shared.update(prep_weights(inp, L))
    shared.update(prep_small(inp, L))
    shared.update(make_consts(T))
    in_maps = []
    for b in range(B):
        m = dict(shared)
        m["xT"] = np.ascontiguousarray(x[b].T)
        in_maps.append(m)
    res = run_bass_kernel_spmd(nc, in_maps, core_ids=list(range(B)))
    out = np.stack([np.asarray(r["outT"]).T for r in res.results], axis=0)
    return np.ascontiguousarray(out.astype(np.float32))
```
